# Optimizing a Trainium2 kernel written in Bass

```python
import jax, jax.numpy as jnp
from jax import lax
import numpy as np

D_MODEL = 1024
BATCH = 4
SEQ = 4096
DEPTH = 2

N_MIXERS = 2
HEAD_SIZE = 64
N_HEADS = D_MODEL // HEAD_SIZE
DECAY_LORA = 64
ICLR_LORA = 64
GATE_LORA = 160
GN_EPS = 64e-5
LRU_WIDTH = D_MODEL
LRU_BLOCK = 256
LRU_BLOCKS = LRU_WIDTH // LRU_BLOCK
CONV_WIDTH = 4
LRU_C = 8.0
D_FF = 7 * D_MODEL // 2
N_EXPERTS = 8
TOP_K = 2
NORM_EPS = 1e-6
N_RWKV = (DEPTH + 1) // 2
N_LRU = DEPTH // 2
N_DENSE = (DEPTH + 1) // 2
N_MOE = DEPTH // 2

kernel_name = 'hybrid_rwkv7_rglru_moe_adaln'


def rmsnorm(x, g):
    xf = x.astype(jnp.float32)
    y = xf * lax.rsqrt(jnp.mean(xf * xf, axis=-1, keepdims=True) + NORM_EPS)
    return (y * g.astype(jnp.float32)).astype(x.dtype)


def modulate(x, g, shift, scale):
    return rmsnorm(x, g) * (1.0 + scale[:, None, :]) + shift[:, None, :]


def causal_shift(x):
    return jnp.pad(x, ((0, 0), (1, 0), (0, 0)))[:, :-1]


def wkv7_scan(r, w, k, v, a_in, b_in):
    B, T, H, N = r.shape
    xs = tuple(jnp.moveaxis(t, 1, 0) for t in (r, w, k, v, a_in, b_in))

    def step(S, inp):
        r_t, w_t, k_t, v_t, a_t, b_t = inp
        sa = jnp.einsum('bhvk,bhk->bhv', S, a_t)
        S = S * w_t[:, :, None, :] + sa[..., None] * b_t[:, :, None, :] + v_t[..., None] * k_t[:, :, None, :]
        return S, jnp.einsum('bhvk,bhk->bhv', S, r_t)

    S0 = jnp.zeros((B, H, N, N), jnp.float32)
    _, ys = lax.scan(step, S0, xs)
    return jnp.moveaxis(ys, 0, 1)


def rwkv7_time_mix(h, mu, w_rkv, w_o, w0, w1, w2, a0, a1, a2, g1, g2, k_k, k_a, r_k, gn_w, gn_b):
    B, T, C = h.shape
    f32 = jnp.float32
    xx = causal_shift(h) - h
    xm = h[None] + xx[None] * mu[:, None, None, :]
    r, k, v = jnp.einsum('pbtc,pcd->pbtd', xm[:3], w_rkv)
    xw, xa, xg = xm[3], xm[4], xm[5]
    w_log = -jax.nn.softplus(-(w0 + jnp.tanh(xw @ w1) @ w2).astype(f32)) - 0.5
    decay = jnp.exp(-jnp.exp(w_log))
    a = jax.nn.sigmoid((a0 + (xa @ a1) @ a2).astype(f32))
    g = jax.nn.sigmoid(xg @ g1) @ g2

    def heads(t):
        return t.astype(f32).reshape(B, T, N_HEADS, HEAD_SIZE)

    r_h, k_h, v_h, a_h, w_h = heads(r), heads(k), heads(v), heads(a), heads(decay)
    kk = k_h * k_k.reshape(N_HEADS, HEAD_SIZE)
    kk = kk / jnp.maximum(jnp.linalg.norm(kk, axis=-1, keepdims=True), 1e-12)
    k_h = k_h * (1.0 + (a_h - 1.0) * k_a.reshape(N_HEADS, HEAD_SIZE))
    y = wkv7_scan(r_h, w_h, k_h, v_h, -kk, kk * a_h)
    mean = jnp.mean(y, axis=-1, keepdims=True)
    var = jnp.mean(jnp.square(y - mean), axis=-1, keepdims=True)
    y = ((y - mean) * lax.rsqrt(var + GN_EPS)).reshape(B, T, C) * gn_w + gn_b
    bonus = jnp.sum(r_h * k_h * r_k, axis=-1, keepdims=True) * v_h
    y = (y + bonus.reshape(B, T, C)) * g
    return y.astype(h.dtype) @ w_o


def rglru_block(h, w_in, conv_w, conv_b, w_gates, b_gates, lam, w_out):
    B, T, _ = h.shape
    f32 = jnp.float32
    xb, gb = jnp.split(h @ w_in, 2, axis=-1)
    gate = jax.nn.gelu(gb)
    xb = lax.conv_general_dilated(xb, conv_w[:, None, :], window_strides=(1,),
                                  padding=[(CONV_WIDTH - 1, 0)],
                                  dimension_numbers=('NWC', 'WIO', 'NWC'),
                                  feature_group_count=LRU_WIDTH) + conv_b
    xh = xb.reshape(B, T, LRU_BLOCKS, LRU_BLOCK)
    gates = jnp.einsum('btnh,nhg->btng', xh, w_gates) + b_gates
    r_t, i_t = jnp.split(jax.nn.sigmoid(gates.astype(f32)), 2, axis=-1)
    r_t = r_t.reshape(B, T, LRU_WIDTH)
    i_t = i_t.reshape(B, T, LRU_WIDTH)
    log_a = -LRU_C * r_t * jax.nn.softplus(-lam.astype(f32))
    a_t = jnp.exp(log_a)
    b_t = jnp.sqrt(-jnp.expm1(2.0 * log_a)) * (i_t * xb.astype(f32))

    def combine(c1, c2):
        return c1[0] * c2[0], c2[0] * c1[1] + c2[1]

    _, hs = lax.associative_scan(combine, (a_t, b_t), axis=1)
    return (hs.astype(h.dtype) * gate) @ w_out


def swiglu(h, w_gu, w_d):
    g, u = jnp.split(h @ w_gu, 2, axis=-1)
    return (jax.nn.silu(g) * u) @ w_d


def moe_swiglu(h, w_router, b_router, w_gu, w_d):
    B, T, D = h.shape
    t = h.reshape(B * T, D)
    logits = (t @ w_router + b_router).astype(jnp.float32)
    top_v, top_i = lax.top_k(logits, TOP_K)
    probs = jax.nn.softmax(top_v, axis=-1)
    comb = jnp.einsum('nk,nke->ne', probs, jax.nn.one_hot(top_i, N_EXPERTS, dtype=jnp.float32))
    out = jnp.zeros_like(t)
    for e in range(N_EXPERTS):
        out = out + comb[:, e, None].astype(t.dtype) * swiglu(t, w_gu[e], w_d[e])
    return out.reshape(B, T, D)


def setup_inputs(seed: int = 0) -> dict:
    key = jax.random.key(seed)
    counter = [0]

    def nxt():
        counter[0] += 1
        return jax.random.fold_in(key, counter[0])

    def nrm(shape, scale):
        return jax.random.normal(nxt(), shape, jnp.float32) * scale

    def unif(shape, lo, hi):
        return jax.random.uniform(nxt(), shape, jnp.float32, lo, hi)

    D, F, E, W = D_MODEL, D_FF, N_EXPERTS, LRU_WIDTH
    NR, NL, ND, NM = N_RWKV, N_LRU, N_DENSE, N_MOE
    u = unif((NL, W), 0.9, 0.999)
    a_base = u ** (1.0 / LRU_C)
    lam = jnp.log(a_base) - jnp.log1p(-a_base)
    return {
        'x': nrm((BATCH, SEQ, D), 1.0),
        'c': nrm((BATCH, D), 1.0),
        'ada_w': nrm((DEPTH, D, 6 * D), 0.1 * D ** -0.5),
        'ada_b': nrm((DEPTH, 6 * D), 0.01),
        'norm_g': 1.0 + nrm((DEPTH, 2, D), 0.05),
        'final_g': 1.0 + nrm((D,), 0.05),
        'rwkv_mu': unif((NR, 6, D), 0.0, 1.0),
        'rwkv_w_rkv': nrm((NR, 3, D, D), D ** -0.5),
        'rwkv_w_o': nrm((NR, D, D), D ** -0.5),
        'rwkv_w0': unif((NR, D), -6.0, -1.0),
        'rwkv_w1': nrm((NR, D, DECAY_LORA), 0.1 * D ** -0.5),
        'rwkv_w2': nrm((NR, DECAY_LORA, D), 0.1 * DECAY_LORA ** -0.5),
        'rwkv_a0': nrm((NR, D), 0.1),
        'rwkv_a1': nrm((NR, D, ICLR_LORA), D ** -0.5),
        'rwkv_a2': nrm((NR, ICLR_LORA, D), 0.1 * ICLR_LORA ** -0.5),
        'rwkv_g1': nrm((NR, D, GATE_LORA), D ** -0.5),
        'rwkv_g2': nrm((NR, GATE_LORA, D), GATE_LORA ** -0.5),
        'rwkv_k_k': 0.85 + nrm((NR, D), 0.05),
        'rwkv_k_a': 1.0 + nrm((NR, D), 0.05),
        'rwkv_r_k': nrm((NR, N_HEADS, HEAD_SIZE), 0.1),
        'rwkv_gn_w': 1.0 + nrm((NR, D), 0.05),
        'rwkv_gn_b': nrm((NR, D), 0.01),
        'lru_w_in': nrm((NL, D, 2 * W), D ** -0.5),
        'lru_conv_w': nrm((NL, CONV_WIDTH, W), CONV_WIDTH ** -0.5),
        'lru_conv_b': nrm((NL, W), 0.01),
        'lru_w_gates': nrm((NL, LRU_BLOCKS, LRU_BLOCK, 2 * LRU_BLOCK), LRU_BLOCK ** -0.5),
        'lru_b_gates': nrm((NL, LRU_BLOCKS, 2 * LRU_BLOCK), 0.01),
        'lru_lam': lam,
        'lru_w_out': nrm((NL, W, D), W ** -0.5),
        'ffn_w_gu': nrm((ND, D, 2 * F), D ** -0.5),
        'ffn_w_d': nrm((ND, F, D), F ** -0.5),
        'moe_w_router': nrm((NM, D, E), D ** -0.5),
        'moe_b_router': nrm((NM, E), 0.01),
        'moe_w_gu': nrm((NM, E, D, 2 * F), D ** -0.5),
        'moe_w_d': nrm((NM, E, F, D), F ** -0.5),
    }


def reference(x, c, ada_w, ada_b, norm_g, final_g,
              rwkv_mu, rwkv_w_rkv, rwkv_w_o, rwkv_w0, rwkv_w1, rwkv_w2, rwkv_a0, rwkv_a1, rwkv_a2,
              rwkv_g1, rwkv_g2, rwkv_k_k, rwkv_k_a, rwkv_r_k, rwkv_gn_w, rwkv_gn_b,
              lru_w_in, lru_conv_w, lru_conv_b, lru_w_gates, lru_b_gates, lru_lam, lru_w_out,
              ffn_w_gu, ffn_w_d, moe_w_router, moe_b_router, moe_w_gu, moe_w_d):
    cond = jax.nn.silu(c)
    for i in range(DEPTH):
        j = i // N_MIXERS
        mod = cond @ ada_w[i] + ada_b[i]
        sh1, sc1, gt1, sh2, sc2, gt2 = jnp.split(mod, 6, axis=-1)
        h = modulate(x, norm_g[i, 0], sh1, sc1)
        if i % N_MIXERS == 0:
            y = rwkv7_time_mix(h, rwkv_mu[j], rwkv_w_rkv[j], rwkv_w_o[j], rwkv_w0[j], rwkv_w1[j], rwkv_w2[j],
                               rwkv_a0[j], rwkv_a1[j], rwkv_a2[j], rwkv_g1[j], rwkv_g2[j], rwkv_k_k[j],
                               rwkv_k_a[j], rwkv_r_k[j], rwkv_gn_w[j], rwkv_gn_b[j])
        else:
            y = rglru_block(h, lru_w_in[j], lru_conv_w[j], lru_conv_b[j], lru_w_gates[j], lru_b_gates[j],
                            lru_lam[j], lru_w_out[j])
        x = x + ((1.0 + gt1)[:, None, :] * y).astype(x.dtype)
        h = modulate(x, norm_g[i, 1], sh2, sc2)
        if i % 2 == 0:
            y = swiglu(h, ffn_w_gu[i // 2], ffn_w_d[i // 2])
        else:
            y = moe_swiglu(h, moe_w_router[i // 2], moe_b_router[i // 2], moe_w_gu[i // 2], moe_w_d[i // 2])
        x = x + ((1.0 + gt2)[:, None, :] * y).astype(x.dtype)
    return rmsnorm(x, final_g)
```

```python
import contextlib
import numpy as np
import concourse.bass as bass
import concourse.mybir as mybir

F32 = mybir.dt.float32
BF16 = mybir.dt.bfloat16
ALU = mybir.AluOpType
AF = mybir.ActivationFunctionType
AX = mybir.AxisListType

ENGS = ("pe", "act", "dve", "pool", "sp")
DMA_RING = 8


class T:
    __slots__ = ("ap", "w", "r", "name")

    def __init__(self, ap, name=""):
        self.ap = ap
        self.w = None
        self.r = []
        self.name = name

    def __getitem__(self, idx):
        return self.ap[idx]


class TV:
    def __init__(self, parent, ap, name=""):
        self.parent = parent
        self.ap = ap
        self.name = name

    @property
    def w(self):
        return self.parent.w

    @w.setter
    def w(self, v):
        self.parent.w = v

    @property
    def r(self):
        return self.parent.r

    @r.setter
    def r(self, v):
        self.parent.r = v


class Op:
    __slots__ = ("eng", "fn", "deps", "signal", "count", "is_dma", "dma_idx", "idx")

    def __init__(self, eng, fn, is_dma):
        self.eng = eng
        self.fn = fn
        self.deps = []
        self.signal = False
        self.count = 0
        self.is_dma = is_dma
        self.dma_idx = -1
        self.idx = -1


class Prog:
    def __init__(self, nc):
        self.nc = nc
        self.ops = []
        self.stack = contextlib.ExitStack()
        self.n_alloc = 0
        self.arena = None
        self.arena_off = 0
        self.arena_size = 0
        self.fence = {}
        self.last_op = {}
        self.recent_dma = {e: [] for e in ENGS}

    def arena_init(self, nbytes):
        self.arena_size = nbytes
        self.arena = self.stack.enter_context(self.nc.sbuf_tensor("arena", [128, nbytes // 2], BF16))
        self.arena_off = 0

    def arena_reset(self):
        self.arena_off = 0

    def barrier(self):
        deps = [o for o in self.last_op.values()]
        for e in ENGS:
            deps.extend(self.recent_dma[e])
        for e in ENGS:
            self.fence[e] = list(deps)

    def sb(self, shape, dtype, name=None):
        if self.arena is not None:
            esize = 4 if dtype == F32 else 2
            n = 1
            for d_ in shape[1:]:
                n *= d_
            off = (self.arena_off + 63) // 64 * 64
            self.arena_off = off + n * esize
            assert self.arena_off <= self.arena_size, ("SBUF arena overflow", name, self.arena_off)
            ap = self.arena[0:shape[0], off // 2:(off + n * esize) // 2]
            if dtype == F32:
                ap = ap.bitcast(F32)
            if len(shape) == 3:
                ap = ap.rearrange("p (a b) -> p a b", a=shape[1])
            elif len(shape) == 4:
                ap = ap.rearrange("p (a b c) -> p a b c", a=shape[1], b=shape[2])
            return ap
        self.n_alloc += 1
        name = f"sb{self.n_alloc}_{name or ''}"
        h = self.stack.enter_context(self.nc.sbuf_tensor(name, list(shape), dtype))
        return h

    def ps(self, shape, dtype, name=None):
        self.n_alloc += 1
        name = f"ps{self.n_alloc}_{name or ''}"
        h = self.stack.enter_context(self.nc.psum_tensor(name, list(shape), dtype))
        return h

    def tile(self, shape, dtype, name=None):
        h = self.sb(shape, dtype, name)
        return T(h[:] if False else h, name or "")

    def op(self, eng, fn, reads=(), writes=(), dma=False):
        o = Op(eng, fn, dma)
        o.idx = len(self.ops)
        deps = []
        for t in reads:
            if t.w is not None:
                deps.append((t.w, 0))
        for t in writes:
            if t.w is not None:
                deps.append((t.w, 0))
            for r in t.r:
                deps.append((r, 1))
        if self.fence.get(eng):
            for d in self.fence[eng]:
                deps.append((d, 0))
            self.fence[eng] = None
        seen = set()
        for d, war in deps:
            if d.idx in seen:
                continue
            if (not d.is_dma) and d.eng == eng and (eng == "pe" or war):
                continue
            seen.add(d.idx)
            o.deps.append(d)
        for t in reads:
            if not dma:
                t.r = [x for x in t.r if x.is_dma or x.eng != eng]
            t.r.append(o)
        for t in writes:
            t.w = o
            t.r = []
        self.ops.append(o)
        if dma:
            self.recent_dma[eng] = (self.recent_dma[eng] + [o])[-DMA_RING:]
        else:
            self.last_op[eng] = o
        return o

    def emit(self):
        nc = self.nc
        ops = self.ops
        for o in ops:
            for d in o.deps:
                d.signal = True
        cnt = {e: 0 for e in ENGS}
        dcnt = {e: 0 for e in ENGS}
        for o in ops:
            if o.is_dma:
                o.dma_idx = dcnt[o.eng]
                dcnt[o.eng] += 1
            elif o.signal:
                cnt[o.eng] += 1
                o.count = cnt[o.eng]
        sems = {}
        for e in ENGS:
            sems[e] = self.stack.enter_context(nc.semaphore(f"s_{e}"))
        dsems = {}
        for e in ENGS:
            if dcnt[e] > 0:
                dsems[e] = [self.stack.enter_context(nc.semaphore(f"d_{e}{i}")) for i in range(DMA_RING)]
        per_eng = {e: [o for o in ops if o.eng == e] for e in ENGS}
        self.stats = {e: (len(per_eng[e]), cnt[e], dcnt[e]) for e in ENGS}

        def sem_target(d):
            if d.is_dma:
                return dsems[d.eng][d.dma_idx % DMA_RING], 16 * (d.dma_idx // DMA_RING + 1)
            return sems[d.eng], d.count

        def run_engine(e, eng):
            waited = {}
            nwait = 0
            for o in per_eng[e]:
                need = {}
                for d in o.deps:
                    s, v = sem_target(d)
                    key = id(s)
                    if waited.get(key, 0) >= v:
                        continue
                    if key not in need or need[key][1] < v:
                        need[key] = (s, v)
                if o.is_dma and o.dma_idx >= DMA_RING:
                    s = dsems[e][o.dma_idx % DMA_RING]
                    v = 16 * (o.dma_idx // DMA_RING)
                    key = id(s)
                    if waited.get(key, 0) < v and (key not in need or need[key][1] < v):
                        need[key] = (s, v)
                for key, (s, v) in need.items():
                    eng.wait_ge(s, v)
                    waited[key] = v
                    nwait += 1
                ins = o.fn(eng)
                if o.is_dma:
                    s, _ = sem_target(o)
                    ins.then_inc(s, 16)
                elif o.signal:
                    ins.then_inc(sems[e], 1)
            if e == "sp":
                for q in ENGS:
                    for i in range(min(DMA_RING, dcnt[q])):
                        n = (dcnt[q] - 1 - i) // DMA_RING + 1
                        eng.wait_ge(dsems[q][i], 16 * n)
            return nwait

        block = self.stack.enter_context(nc.Block())
        self.nwaits = {}

        if per_eng["pe"]:
            @block.tensor
            def _(eng):
                self.nwaits["pe"] = run_engine("pe", eng)
        if per_eng["act"]:
            @block.scalar
            def _(eng):
                self.nwaits["act"] = run_engine("act", eng)
        if per_eng["dve"]:
            @block.vector
            def _(eng):
                self.nwaits["dve"] = run_engine("dve", eng)
        if per_eng["pool"]:
            @block.gpsimd
            def _(eng):
                self.nwaits["pool"] = run_engine("pool", eng)
        if True:
            @block.sync
            def _(eng):
                self.nwaits["sp"] = run_engine("sp", eng)

    def close(self):
        self.stack.close()

    def I(self, eng, method, reads, writes, *args, **kw):
        return self.op(eng, lambda e: getattr(e, method)(*args, **kw), list(reads), list(writes))

    def dma(self, eng, out_t, out_ap, in_t, in_ap, **kw):
        reads = [in_t] if in_t is not None else []
        writes = [out_t] if out_t is not None else []
        return self.op(eng, lambda e: e.dma_start(out=out_ap, in_=in_ap, **kw), reads, writes, dma=True)

    def mm(self, out_t, out_ap, lhsT_t, lhsT_ap, rhs_t, rhs_ap, start=True, stop=True, extra_reads=(), **kw):
        return self.op("pe", lambda e: e.matmul(out_ap, lhsT_ap, rhs_ap, start=start, stop=stop, **kw),
                       [lhsT_t, rhs_t] + list(extra_reads), [out_t])


from concourse.bass_utils import run_bass_kernel_spmd

D = 1024
JC = 8
NTOK = 2048
FF = 3584
FC = 28
NE = 8
NORM_EPS = 1e-6


def pack_vec(v):
    v = np.asarray(v, np.float32).reshape(-1)
    n = v.shape[0] // 128
    return np.ascontiguousarray(v.reshape(n, 128).T)


def pack_vecs(named):
    cols = {}
    arrs = []
    off = 0
    for k, v in named:
        a = pack_vec(v)
        cols[k] = (off, a.shape[1])
        arrs.append(a)
        off += a.shape[1]
    return np.ascontiguousarray(np.concatenate(arrs, axis=1)), cols


class Ring:
    def __init__(self, tiles):
        self.tiles = tiles
        self.i = 0

    def next(self):
        t = self.tiles[self.i % len(self.tiles)]
        self.i += 1
        return t


def new_prog():
    nc = bass.Bass("TRN2", target_bir_lowering=False)
    return nc, Prog(nc)


def emit_mod(P, nc, cT_d, adaw_d, nvec, vecs, adab_col, scratch_h, ps_bank):
    cT = P.tile([128, 8], F32, "cT")
    P.dma("sp", cT, cT.ap[:], None, cT_d)
    sc_bf = P.tile([128, 8], BF16, "sc_bf")
    P.op("act", lambda e: e.activation(sc_bf.ap[:], cT.ap[:], AF.Silu), [cT], [sc_bf])
    ncols = nvec * D
    aw = T(scratch_h, "adaw_sb")
    src = adaw_d.rearrange("(kc p) n -> p kc n", p=128)
    for v in range(nvec):
        P.dma("pool", aw, aw.ap[:, :, v * D:(v + 1) * D], None, src[:, :, v * D:(v + 1) * D])
    noc = nvec * 8
    for oc in range(noc):
        for kc in range(8):
            P.mm(ps_bank, ps_bank.ap[:, oc:oc + 1], aw, aw.ap[:, kc, oc * 128:(oc + 1) * 128],
                 sc_bf, sc_bf.ap[:, kc:kc + 1], start=(kc == 0), stop=(kc == 7))
    modv = P.tile([128, noc], F32, "modv")
    P.op("dve", lambda e: e.tensor_tensor(modv.ap[:], ps_bank.ap[:, 0:noc], vecs.ap[:, adab_col:adab_col + noc], ALU.add),
         [ps_bank, vecs], [modv])
    return modv


def emit_norm(P, x_t, x_ap, N, gm_t, gm_ap, sh_t, sh_ap, out_t, out_ap, ones_bf, ps_ss, sq_t, rt_t, tmp_t, eps_t, out_extra=()):
    for j in range(8):
        P.op("act", (lambda j: lambda e: e.activation(sq_t.ap[:, j, 0:N], x_ap[:, j, :], AF.Square))(j), [x_t], [sq_t])
    for j in range(8):
        P.mm(ps_ss, ps_ss.ap[:, 0:N], ones_bf, ones_bf.ap[:], sq_t, sq_t.ap[:, j, 0:N], start=(j == 0), stop=(j == 7))
    P.op("act", lambda e: e.activation(rt_t.ap[:, 0:N], ps_ss.ap[:, 0:N], AF.Sqrt, bias=eps_t.ap[:, 0:1], scale=1.0 / D),
         [ps_ss, eps_t], [rt_t])
    P.op("dve", lambda e: e.reciprocal(rt_t.ap[:, 0:N], rt_t.ap[:, 0:N]), [rt_t], [rt_t])
    for j in range(8):
        P.op("dve", (lambda j: lambda e: e.scalar_tensor_tensor(tmp_t.ap[:, j, 0:N], x_ap[:, j, :], gm_ap[:, j:j + 1],
                                                                rt_t.ap[:, 0:N], ALU.mult, ALU.mult))(j),
             [x_t, gm_t, rt_t], [tmp_t])
        if sh_t is not None:
            P.op("act", (lambda j: lambda e: e.activation(out_ap[:, j, :], tmp_t.ap[:, j, 0:N], AF.Identity,
                                                          bias=sh_ap[:, j:j + 1], scale=1.0))(j),
                 [tmp_t, sh_t], [out_t] + list(out_extra))
        else:
            P.op("act", (lambda j: lambda e: e.copy(out_ap[:, j, :], tmp_t.ap[:, j, 0:N]))(j), [tmp_t], [out_t])


DEBUG = False


def emit_ffn(P, nc, pbank, xT_v, yT_v, ntok, moe, pre, cT_d):
    E = NE if moe else 1
    ST = 1024
    NV = 8 + 24 + (8 if moe else 0)
    vecs_d = nc.dram_tensor(pre + "vecs", [128, NV], F32, kind="ExternalInput").ap()
    adaw_d = nc.dram_tensor(pre + "adaw", [D, 3 * D], F32, kind="ExternalInput").ap()
    wgu_d = nc.dram_tensor(pre + "wgu", [E, D, 2 * FF], F32, kind="ExternalInput").ap()
    wd_d = nc.dram_tensor(pre + "wd", [E, FF, D], F32, kind="ExternalInput").ap()
    if moe:
        ident_d = nc.dram_tensor(pre + "ident", [128, 128], F32, kind="ExternalInput").ap()
        wr_d = nc.dram_tensor(pre + "wr", [D, NE], F32, kind="ExternalInput").ap()
        br_d = nc.dram_tensor(pre + "br", [128, NE], F32, kind="ExternalInput").ap()

    vecs = P.tile([128, NV], F32, "vecs")
    P.dma("sp", vecs, vecs.ap[:], None, vecs_d)
    ones_bf = P.tile([128, 128], BF16, "ones")
    P.op("dve", lambda e: e.memset(ones_bf.ap[:], 1.0), [], [ones_bf])
    eps_t = P.tile([128, 1], F32, "eps")
    P.op("dve", lambda e: e.memset(eps_t.ap[:], NORM_EPS), [], [eps_t])
    act_h = P.sb([128, FC, ST], BF16, "act")
    act = [T(act_h[:, fc, :], f"act{fc}") for fc in range(FC)]
    banks = [T(pbank[i], f"bank{i}") for i in range(8)]
    aw_view = act_h[:, 0:24, :].rearrange("p a b -> p (a b)").rearrange("p (k n) -> p k n", k=8)
    modv = emit_mod(P, nc, cT_d, adaw_d, 3, vecs, 8, aw_view, banks[7])
    gm = P.tile([128, 8], F32, "gm")
    P.op("dve", lambda e: e.scalar_tensor_tensor(gm.ap[:], modv.ap[:, 8:16], 1.0, vecs.ap[:, 0:8], ALU.add, ALU.mult),
         [modv, vecs], [gm])
    g2p = P.tile([128, 8], F32, "g2p")
    P.op("dve", lambda e: e.tensor_scalar(g2p.ap[:], modv.ap[:, 16:24], 1.0, None, ALU.add), [modv], [g2p])

    x_h = P.sb([128, 8, ST], F32, "x")
    xall = T(x_h, "xall")
    h_bf = P.tile([128, 8, ST], BF16, "h_bf")
    sq_t = P.tile([128, 8, 512], BF16, "sq")
    rt_t = P.tile([128, 512], F32, "rt")
    tmp_t = P.tile([128, 8, 512], F32, "tmp")
    wgu_ring = Ring([P.tile([128, 8, 2, 256], BF16, f"wgu{i}") for i in range(2)])
    wd_ring = Ring([P.tile([128, FC, 128], BF16, f"wd{i}") for i in range(2)])
    sg_ring = Ring([P.tile([128, 512], F32, f"sg{i}") for i in range(2)])
    psg_ring = Ring([banks[0], banks[1]])
    psu_ring = Ring([banks[2], banks[3]])
    pso_ring = Ring([banks[4], banks[5]])
    ps_ss = banks[6]
    if moe:
        ident = P.tile([128, 128], F32, "ident")
        P.dma("sp", ident, ident.ap[:], None, ident_d)
        wr = P.tile([128, 8, NE], F32, "wr")
        P.dma("sp", wr, wr.ap[:], None, wr_d.rearrange("(kc p) e -> p kc e", p=128))
        br = P.tile([128, NE], F32, "br")
        P.dma("sp", br, br.ap[:], None, br_d)
        comb = P.tile([128, ST // 128, NE], F32, "comb")
        lg = P.tile([128, NE], F32, "lg")
        m1 = P.tile([128, 1], F32, "m1")
        m2 = P.tile([128, 1], F32, "m2")
        eq1 = P.tile([128, NE], F32, "eq1")
        lg2 = P.tile([128, NE], F32, "lg2")
        eq2 = P.tile([128, NE], F32, "eq2")
        p1 = P.tile([128, 1], F32, "p1")
        p2 = P.tile([128, 1], F32, "p2")
        rep_ring = Ring([P.tile([128, 128], F32, f"rep{i}") for i in range(2)])
        cbc_ring = Ring([P.tile([128, ST], F32, f"cbc{i}") for i in range(2)])
        tmp2_ring = Ring([P.tile([128, 512], F32, f"tmp2{i}") for i in range(2)])
        ps_misc = banks[7]

    def loads_wgu(e, g2):
        t = wgu_ring.next()
        src = wgu_d[e].rearrange("(kc p) n -> p kc n", p=128)
        P.dma("pool", t, t.ap[:, :, 0, :], None, src[:, :, g2 * 256:(g2 + 1) * 256])
        P.dma("pool", t, t.ap[:, :, 1, :], None, src[:, :, FF + g2 * 256:FF + (g2 + 1) * 256])
        return t

    def load_wd(e, d):
        t = wd_ring.next()
        src = wd_d[e].rearrange("(fc p) n -> p fc n", p=128)
        P.dma("pool", t, t.ap[:], None, src[:, :, d * 128:(d + 1) * 128])
        return t

    if moe:
        hf = T(act_h[:, 0:16, :].rearrange("p a b -> p (a b)").bitcast(F32).rearrange("p (j n) -> p j n", j=8), "hf")
    for st in range(ntok // ST):
        c0 = st * ST
        P.dma("sp", xall, x_h[:], None, xT_v[:, :, c0:c0 + ST])
        for tt in range(ST // 512):
            cs = slice(tt * 512, (tt + 1) * 512)
            if not moe:
                emit_norm(P, xall, x_h[:, :, cs], 512, gm, gm.ap, modv, modv.ap[:, 0:8], h_bf, h_bf.ap[:, :, cs],
                          ones_bf, ps_ss, sq_t, rt_t, tmp_t, eps_t)
            else:
                emit_norm(P, xall, x_h[:, :, cs], 512, gm, gm.ap, modv, modv.ap[:, 0:8], hf, hf.ap[:, :, 0:512],
                          ones_bf, ps_ss, sq_t, rt_t, tmp_t, eps_t, out_extra=act[0:16])
                for j in range(8):
                    P.I("dve", "tensor_copy", [hf], [h_bf], h_bf.ap[:, j, cs], hf.ap[:, j, 0:512])
                for b in range(4):
                    blk = tt * 4 + b
                    for kc in range(8):
                        P.mm(ps_misc, ps_misc.ap[:, 0:NE], hf, hf.ap[:, kc, b * 128:(b + 1) * 128], wr, wr.ap[:, kc, :],
                             start=(kc == 0), stop=(kc == 7))
                    P.op("dve", lambda e: e.tensor_tensor(lg.ap[:], ps_misc.ap[:, 0:NE], br.ap[:], ALU.add), [ps_misc, br], [lg])
                    P.op("dve", lambda e: e.tensor_reduce(m1.ap[:], lg.ap[:], AX.X, ALU.max), [lg], [m1])
                    P.op("dve", lambda e: e.tensor_scalar(eq1.ap[:], lg.ap[:], m1.ap[:, 0:1], None, ALU.is_equal), [lg, m1], [eq1])
                    P.op("dve", lambda e: e.scalar_tensor_tensor(lg2.ap[:], eq1.ap[:], -1e30, lg.ap[:], ALU.mult, ALU.add),
                         [eq1, lg], [lg2])
                    P.op("dve", lambda e: e.tensor_reduce(m2.ap[:], lg2.ap[:], AX.X, ALU.max), [lg2], [m2])
                    P.op("dve", lambda e: e.tensor_scalar(eq2.ap[:], lg2.ap[:], m2.ap[:, 0:1], None, ALU.is_equal), [lg2, m2], [eq2])
                    P.op("dve", lambda e: e.tensor_tensor(p2.ap[:], m1.ap[:], m2.ap[:], ALU.subtract), [m1, m2], [p2])
                    P.op("act", lambda e: e.activation(p1.ap[:], p2.ap[:], AF.Sigmoid), [p2], [p1])
                    P.op("dve", lambda e: e.tensor_scalar(p2.ap[:], p1.ap[:], -1.0, 1.0, ALU.mult, ALU.add), [p1], [p2])
                    P.op("dve", lambda e: e.tensor_scalar(eq1.ap[:], eq1.ap[:], p1.ap[:, 0:1], None, ALU.mult), [eq1, p1], [eq1])
                    P.op("dve", (lambda blk: lambda e: e.scalar_tensor_tensor(comb.ap[:, blk, :], eq2.ap[:], p2.ap[:, 0:1], eq1.ap[:],
                                                                             ALU.mult, ALU.add))(blk), [eq2, p2, eq1], [comb])
        if moe and DEBUG and st == 0:
            dbg_comb = nc.dram_tensor("dbg_comb", [128, ST // 128, NE], F32, kind="ExternalOutput").ap()
            P.dma("sp", None, dbg_comb, comb, comb.ap[:])
            dbg_h = nc.dram_tensor("dbg_h", [128, 8, ST], BF16, kind="ExternalOutput").ap()
            P.dma("sp", None, dbg_h, h_bf, h_bf.ap[:])
        for e_i in range(E):
            if moe:
                cbc = cbc_ring.next()
                for blk in range(ST // 128):
                    rep = rep_ring.next()
                    P.op("dve", (lambda rep, blk, e_i: lambda e: e.tensor_copy(rep.ap[:], comb.ap[:, blk, e_i:e_i + 1].broadcast_to([128, 128])))(rep, blk, e_i),
                         [comb], [rep])
                    half = blk // 4
                    col = (blk % 4) * 128
                    P.mm(ps_misc, ps_misc.ap[:, col:col + 128], rep, rep.ap[:], ident, ident.ap[:])
                    if blk % 4 == 3:
                        P.op("act", (lambda cbc, half: lambda e: e.copy(cbc.ap[:, half * 512:(half + 1) * 512], ps_misc.ap[:]))(cbc, half),
                             [ps_misc], [cbc])
            if moe and DEBUG and st == 0 and e_i == 0:
                dbg_cbc = nc.dram_tensor("dbg_cbc", [128, ST], F32, kind="ExternalOutput").ap()
                P.dma("sp", None, dbg_cbc, cbc, cbc.ap[:])
            for g2 in range(FC // 2):
                wt = loads_wgu(e_i, g2)
                for f in range(2):
                    fc = g2 * 2 + f
                    for tt in range(ST // 512):
                        cs = slice(tt * 512, (tt + 1) * 512)
                        psg = psg_ring.next()
                        psu = psu_ring.next()
                        for kc in range(8):
                            P.mm(psg, psg.ap[:], wt, wt.ap[:, kc, 0, f * 128:(f + 1) * 128], h_bf, h_bf.ap[:, kc, cs],
                                 start=(kc == 0), stop=(kc == 7))
                        for kc in range(8):
                            P.mm(psu, psu.ap[:], wt, wt.ap[:, kc, 1, f * 128:(f + 1) * 128], h_bf, h_bf.ap[:, kc, cs],
                                 start=(kc == 0), stop=(kc == 7))
                        sg = sg_ring.next()
                        P.op("act", (lambda sg, psg: lambda e: e.activation(sg.ap[:], psg.ap[:], AF.Silu))(sg, psg), [psg], [sg])
                        if not moe:
                            P.op("dve", (lambda sg, psu, fc, cs: lambda e: e.tensor_tensor(act_h[:, fc, cs], sg.ap[:], psu.ap[:], ALU.mult))(sg, psu, fc, cs),
                                 [sg, psu], [act[fc]])
                        else:
                            t2 = tmp2_ring.next()
                            P.op("dve", (lambda sg, psu, t2: lambda e: e.tensor_tensor(t2.ap[:], sg.ap[:], psu.ap[:], ALU.mult))(sg, psu, t2),
                                 [sg, psu], [t2])
                            P.op("dve", (lambda t2, cbc, fc, cs: lambda e: e.tensor_tensor(act_h[:, fc, cs], t2.ap[:], cbc.ap[:, cs], ALU.mult))(t2, cbc, fc, cs),
                                 [t2, cbc], [act[fc]])
            for d in range(8):
                wt = load_wd(e_i, d)
                for tt in range(ST // 512):
                    cs = slice(tt * 512, (tt + 1) * 512)
                    pso = pso_ring.next()
                    for fc in range(FC):
                        P.mm(pso, pso.ap[:], wt, wt.ap[:, fc, :], act[fc], act_h[:, fc, cs], start=(fc == 0), stop=(fc == FC - 1))
                    P.op("dve", (lambda pso, d, cs: lambda e: e.scalar_tensor_tensor(x_h[:, d, cs], pso.ap[:], g2p.ap[:, d:d + 1], x_h[:, d, cs],
                                                                                    ALU.mult, ALU.add))(pso, d, cs),
                         [pso, g2p, xall], [xall])
        if moe:
            fg_col = 32
            for tt in range(ST // 512):
                cs = slice(tt * 512, (tt + 1) * 512)
                emit_final(P, xall, x_h[:, :, cs], vecs, fg_col, ones_bf, ps_ss, sq_t, rt_t, tmp_t, eps_t)
                P.dma("sp", None, yT_v[:, :, c0 + tt * 512:c0 + (tt + 1) * 512], tmp_t, tmp_t.ap[:])
        else:
            P.dma("sp", None, yT_v[:, :, c0:c0 + ST], xall, x_h[:])


def emit_final(P, x_t, x_ap, vecs, fg_col, ones_bf, ps_ss, sq_t, rt_t, tmp_t, eps_t):
    N = 512
    for j in range(8):
        P.op("act", (lambda j: lambda e: e.activation(sq_t.ap[:, j, :], x_ap[:, j, :], AF.Square))(j), [x_t], [sq_t])
    for j in range(8):
        P.mm(ps_ss, ps_ss.ap[:], ones_bf, ones_bf.ap[:], sq_t, sq_t.ap[:, j, :], start=(j == 0), stop=(j == 7))
    P.op("act", lambda e: e.activation(rt_t.ap[:], ps_ss.ap[:], AF.Sqrt, bias=eps_t.ap[:, 0:1], scale=1.0 / D), [ps_ss, eps_t], [rt_t])
    P.op("dve", lambda e: e.reciprocal(rt_t.ap[:], rt_t.ap[:]), [rt_t], [rt_t])
    for j in range(8):
        P.op("dve", (lambda j: lambda e: e.scalar_tensor_tensor(tmp_t.ap[:, j, :], x_ap[:, j, :], vecs.ap[:, fg_col + j:fg_col + j + 1],
                                                                rt_t.ap[:], ALU.mult, ALU.mult))(j),
             [x_t, vecs, rt_t], [tmp_t])


LRU_C = 8.0
LRU_VEC_NAMES = ["ng", "adab", "cw0", "cw1", "cw2", "cw3", "cb", "bgr", "bgi", "lam", "flag"]


def emit_lru(P, nc, pbank, xT_v, yT_v, pre, cT_d):
    NT = 256
    VT = 2 * NTOK
    NV = 8 + 24 + 32 + 8 + 8 + 8 + 8 + 1
    vecs_d = nc.dram_tensor(pre + "vecs", [128, NV], F32, kind="ExternalInput").ap()
    adaw_d = nc.dram_tensor(pre + "adaw", [D, 3 * D], F32, kind="ExternalInput").ap()
    win_d = nc.dram_tensor(pre + "win", [D, 2 * D], F32, kind="ExternalInput").ap()
    wg_d = nc.dram_tensor(pre + "wg", [4, 256, 512], F32, kind="ExternalInput").ap()
    wout_d = nc.dram_tensor(pre + "wout", [D, D], F32, kind="ExternalInput").ap()
    C_NG, C_AB, C_CW, C_CB, C_BGR, C_BGI, C_LAM, C_FLAG = 0, 8, 32, 64, 72, 80, 88, 96

    vecs = P.tile([128, NV], F32, "vecs")
    P.dma("sp", vecs, vecs.ap[:], None, vecs_d)
    ones_bf = P.tile([128, 128], BF16, "ones")
    P.I("dve", "memset", [], [ones_bf], ones_bf.ap[:], 1.0)
    eps_t = P.tile([128, 1], F32, "eps")
    P.I("dve", "memset", [], [eps_t], eps_t.ap[:], NORM_EPS)
    banks = [T(pbank[i], f"bank{i}") for i in range(8)]
    scratch = P.sb([128, 8, 3 * D], BF16, "adaw_sb")
    modv = emit_mod(P, nc, cT_d, adaw_d, 3, vecs, C_AB, scratch, banks[7])
    gm = P.tile([128, 8], F32, "gm")
    P.I("dve", "scalar_tensor_tensor", [modv, vecs], [gm], gm.ap[:], modv.ap[:, 8:16], 1.0, vecs.ap[:, C_NG:C_NG + 8], ALU.add, ALU.mult)
    g1p = P.tile([128, 8], F32, "g1p")
    P.I("dve", "tensor_scalar", [modv], [g1p], g1p.ap[:], modv.ap[:, 16:24], 1.0, None, ALU.add)
    cj = P.tile([128, 8], F32, "cj")
    P.I("act", "activation", [vecs], [cj], cj.ap[:], vecs.ap[:, C_LAM:C_LAM + 8], AF.Exp, scale=-1.0)
    P.I("dve", "tensor_scalar", [cj], [cj], cj.ap[:], cj.ap[:], 1.0, None, ALU.add)
    P.I("act", "activation", [cj], [cj], cj.ap[:], cj.ap[:], AF.Ln)
    P.I("dve", "tensor_scalar", [cj], [cj], cj.ap[:], cj.ap[:], -LRU_C, None, ALU.mult)

    win = T(scratch[:, :, 0:2 * D], "win")
    wout = T(scratch[:, :, 2 * D:3 * D], "wout")
    wg = P.tile([128, 4, 2, 512], BF16, "wg")
    dummy = P.tile([128, 1], F32, "dummy")
    P.I("pool", "tensor_copy", [modv], [win, wout, dummy], dummy.ap[:], modv.ap[:, 0:1])
    src = win_d.rearrange("(kc p) n -> p kc n", p=128)
    for v in range(2):
        P.dma("pool", win, scratch[:, :, v * D:(v + 1) * D], None, src[:, :, v * D:(v + 1) * D])
    P.dma("pool", wout, scratch[:, :, 2 * D:3 * D], None, wout_d.rearrange("(kc p) n -> p kc n", p=128))
    for n in range(4):
        P.dma("pool", wg, wg.ap[:, n, :, :], None, wg_d[n].rearrange("(kc p) n -> p kc n", p=128))

    x_t = P.tile([128, 8, NT], F32, "x")
    h_bf = P.tile([128, 8, NT], BF16, "h_bf")
    sq_t = P.tile([128, 8, NT], BF16, "sq")
    rt_t = P.tile([128, NT], F32, "rt")
    tmp_t = P.tile([128, 8, NT], F32, "tmp")
    xb_sb = P.tile([128, 8, NT + 3], F32, "xb_sb")
    gate = P.tile([128, 8, NT], F32, "gate")
    xbc = P.tile([128, 8, NT], F32, "xbc")
    xbc_bf = P.tile([128, 8, NT], BF16, "xbc_bf")
    hg = P.tile([128, 8, NT], BF16, "hg")
    hprev = P.tile([128, 8], F32, "hprev")
    xo = P.tile([128, 8, NT], F32, "xo")
    w_ring = Ring([P.tile([128, NT], F32, f"wk{i}") for i in range(10)])
    xb_ring = Ring([banks[0], banks[1]])
    gb_ring = Ring([banks[2], banks[3]])
    r_ring = Ring([banks[4]])
    i_ring = Ring([banks[5]])
    ps_ss = banks[6]
    o_ring = Ring([banks[6], banks[7]])

    P.I("dve", "memset", [], [xb_sb], xb_sb.ap[:, :, NT:NT + 3], 0.0)
    P.I("dve", "memset", [], [hprev], hprev.ap[:], 0.0)
    fl = vecs.ap[:, C_FLAG:C_FLAG + 1]

    for tt in range(VT // NT):
        c0 = tt * NT
        own = tt >= (NTOK // NT)
        P.dma("sp", x_t, x_t.ap[:], None, xT_v[:, :, c0:c0 + NT])
        emit_norm(P, x_t, x_t.ap[:], NT, gm, gm.ap, modv, modv.ap[:, 0:8], h_bf, h_bf.ap[:], ones_bf, ps_ss, sq_t, rt_t, tmp_t, eps_t)
        if tt == NTOK // NT:
            P.I("dve", "tensor_scalar", [xb_sb, vecs], [xb_sb], xb_sb.ap[:, :, 0:3], xb_sb.ap[:, :, NT:NT + 3], fl, None, ALU.mult)
            P.I("dve", "tensor_scalar", [hprev, vecs], [hprev], hprev.ap[:], hprev.ap[:], fl, None, ALU.mult)
        else:
            P.I("dve", "tensor_copy", [xb_sb], [xb_sb], xb_sb.ap[:, :, 0:3], xb_sb.ap[:, :, NT:NT + 3])
        for j in range(8):
            xb_ps = xb_ring.next()
            gb_ps = gb_ring.next() if own else None
            for kc in range(8):
                P.mm(xb_ps, xb_ps.ap[:, 0:NT], win, scratch[:, kc, j * 128:(j + 1) * 128], h_bf, h_bf.ap[:, kc, :], start=(kc == 0), stop=(kc == 7))
            for kc in range(8 if own else 0):
                P.mm(gb_ps, gb_ps.ap[:, 0:NT], win, scratch[:, kc, D + j * 128:D + (j + 1) * 128], h_bf, h_bf.ap[:, kc, :], start=(kc == 0), stop=(kc == 7))
            P.I("act", "copy", [xb_ps], [xb_sb], xb_sb.ap[:, j, 3:NT + 3], xb_ps.ap[:, 0:NT])
            if own:
                t1 = w_ring.next()
                P.I("act", "activation", [gb_ps], [t1], t1.ap[:], gb_ps.ap[:, 0:NT], AF.Square)
                P.I("dve", "tensor_scalar", [t1], [t1], t1.ap[:], t1.ap[:], 0.044715, 1.0, ALU.mult, ALU.add)
                P.I("dve", "tensor_tensor", [t1, gb_ps], [t1], t1.ap[:], t1.ap[:], gb_ps.ap[:, 0:NT], ALU.mult)
                P.I("act", "activation", [t1], [t1], t1.ap[:], t1.ap[:], AF.Sigmoid, scale=1.5957691216057308)
                P.I("dve", "tensor_tensor", [t1, gb_ps], [gate], gate.ap[:, j, :], t1.ap[:], gb_ps.ap[:, 0:NT], ALU.mult)
            P.I("act", "activation", [xb_sb, vecs], [xbc], xbc.ap[:, j, :], xb_sb.ap[:, j, 3:NT + 3], AF.Identity,
                bias=vecs.ap[:, C_CB + j:C_CB + j + 1], scale=vecs.ap[:, C_CW + 24 + j:C_CW + 24 + j + 1])
            for i in (2, 1, 0):
                P.I("dve", "scalar_tensor_tensor", [xb_sb, vecs, xbc], [xbc], xbc.ap[:, j, :], xb_sb.ap[:, j, i:NT + i],
                    vecs.ap[:, C_CW + i * 8 + j:C_CW + i * 8 + j + 1], xbc.ap[:, j, :], ALU.mult, ALU.add)
            P.I("act", "copy", [xbc], [xbc_bf], xbc_bf.ap[:, j, :], xbc.ap[:, j, :])
        for n in range(4):
            for oc in range(2):
                j = 2 * n + oc
                r_ps = r_ring.next()
                i_ps = i_ring.next()
                for kc in range(2):
                    P.mm(r_ps, r_ps.ap[:, 0:NT], wg, wg.ap[:, n, kc, oc * 128:(oc + 1) * 128], xbc_bf, xbc_bf.ap[:, 2 * n + kc, :],
                         start=(kc == 0), stop=(kc == 1))
                for kc in range(2):
                    P.mm(i_ps, i_ps.ap[:, 0:NT], wg, wg.ap[:, n, kc, 256 + oc * 128:256 + (oc + 1) * 128], xbc_bf, xbc_bf.ap[:, 2 * n + kc, :],
                         start=(kc == 0), stop=(kc == 1))
                a_t = w_ring.next()
                b_t = w_ring.next()
                i_t = w_ring.next()
                P.I("act", "activation", [r_ps, vecs], [a_t], a_t.ap[:], r_ps.ap[:, 0:NT], AF.Sigmoid, bias=vecs.ap[:, C_BGR + j:C_BGR + j + 1])
                P.I("act", "activation", [i_ps, vecs], [i_t], i_t.ap[:], i_ps.ap[:, 0:NT], AF.Sigmoid, bias=vecs.ap[:, C_BGI + j:C_BGI + j + 1])
                P.I("act", "activation", [a_t, cj], [a_t], a_t.ap[:], a_t.ap[:], AF.Exp, scale=cj.ap[:, j:j + 1])
                P.I("dve", "tensor_tensor", [a_t], [b_t], b_t.ap[:], a_t.ap[:], a_t.ap[:], ALU.mult)
                P.I("dve", "tensor_scalar", [b_t], [b_t], b_t.ap[:], b_t.ap[:], -1.0, 1.0, ALU.mult, ALU.add)
                P.I("act", "activation", [b_t], [b_t], b_t.ap[:], b_t.ap[:], AF.Sqrt)
                P.I("dve", "tensor_tensor", [i_t, xbc], [i_t], i_t.ap[:], i_t.ap[:], xbc.ap[:, j, :], ALU.mult)
                P.I("dve", "tensor_tensor", [b_t, i_t], [b_t], b_t.ap[:], b_t.ap[:], i_t.ap[:], ALU.mult)
                hs = w_ring.next()
                P.I("dve", "tensor_tensor_scan", [a_t, b_t, hprev], [hs], hs.ap[:], a_t.ap[:], b_t.ap[:], hprev.ap[:, j:j + 1], ALU.mult, ALU.add)
                P.I("dve", "tensor_copy", [hs], [hprev], hprev.ap[:, j:j + 1], hs.ap[:, NT - 1:NT])
                if own:
                    P.I("dve", "tensor_tensor", [hs, gate], [hg], hg.ap[:, j, :], hs.ap[:], gate.ap[:, j, :], ALU.mult)
        for jo in range(8 if own else 0):
            ps = o_ring.next()
            for kc in range(8):
                P.mm(ps, ps.ap[:, 0:NT], wout, scratch[:, kc, 2 * D + jo * 128:2 * D + (jo + 1) * 128], hg, hg.ap[:, kc, :], start=(kc == 0), stop=(kc == 7))
            P.I("dve", "scalar_tensor_tensor", [ps, g1p, x_t], [xo], xo.ap[:, jo, :], ps.ap[:, 0:NT], g1p.ap[:, jo:jo + 1], x_t.ap[:, jo, :], ALU.mult, ALU.add)
        if own:
            P.dma("sp", None, yT_v[:, :, c0 - NTOK:c0 - NTOK + NT], xo, xo.ap[:])


GN_EPS = 64e-5
EXPM05 = 0.6065306597126334


def rwkv_consts():
    p = np.arange(128)[:, None]
    q = np.arange(64)[None, :]
    ms = ((p % 64) < q).astype(np.float32)
    mi = ((p % 64) <= q).astype(np.float32)
    mt = (q < (p % 64)).astype(np.float32)
    iq = ((p % 64) == q).astype(np.float32)
    pp = np.arange(128)[None, :]
    bo = ((p // 64) == (pp // 64)).astype(np.float32)
    idn = np.eye(128, dtype=np.float32)
    t = np.arange(256)[None, :]
    mc = np.broadcast_to(((t % 64) != 0).astype(np.float32), (128, 256))
    return np.ascontiguousarray(np.concatenate([ms, mi, mt, iq, bo, bo / 64.0, idn, mc], axis=1))


RW_VECS = ["ng", "adab", "mu", "w0", "a0", "kk", "ka", "rk", "gnw", "gnb", "flag"]


class StopBuild(Exception):
    pass


RW_STOP = None
RW_DUMPS = []


def emit_rwkv(P, nc, pbank, xT_v, yT_v, pre, cT_d):
    def ckpt(name, dumps):
        return
    VT = 2 * NTOK
    NT = 256
    C = NT // 64
    NV = 8 + 24 + 48 + 8 * 7 + 1
    vecs_d = nc.dram_tensor(pre + "vecs", [128, NV], F32, kind="ExternalInput").ap()
    NCON = 64 * 4 + 128 * 3 + 256
    con_d = nc.dram_tensor(pre + "consts", [128, NCON], F32, kind="ExternalInput").ap()
    adaw_d = nc.dram_tensor(pre + "adaw", [D, 3 * D], F32, kind="ExternalInput").ap()
    wrkv_d = nc.dram_tensor(pre + "wrkv", [3, D, D], F32, kind="ExternalInput").ap()
    wo_d = nc.dram_tensor(pre + "wo", [D, D], F32, kind="ExternalInput").ap()
    w1_d = nc.dram_tensor(pre + "w1", [D, 64], F32, kind="ExternalInput").ap()
    w2_d = nc.dram_tensor(pre + "w2", [64, D], F32, kind="ExternalInput").ap()
    a1_d = nc.dram_tensor(pre + "a1", [D, 64], F32, kind="ExternalInput").ap()
    a2_d = nc.dram_tensor(pre + "a2", [64, D], F32, kind="ExternalInput").ap()
    g1_d = nc.dram_tensor(pre + "g1", [D, 160], F32, kind="ExternalInput").ap()
    g2_d = nc.dram_tensor(pre + "g2", [160, D], F32, kind="ExternalInput").ap()
    C_NG, C_AB, C_MU, C_W0, C_A0, C_KK, C_KA, C_RK, C_GNW, C_GNB, C_FLAG = 0, 8, 32, 80, 88, 96, 104, 112, 120, 128, 136

    vecs = P.tile([128, NV], F32, "vecs")
    P.dma("sp", vecs, vecs.ap[:], None, vecs_d)
    con = P.tile([128, NCON], F32, "con")
    P.dma("sp", con, con.ap[:], None, con_d)
    MS, MI, MT, IQ = (con.ap[:, 64 * i:64 * (i + 1)] for i in range(4))
    BO = con.ap[:, 256:384]
    BO64 = con.ap[:, 384:512]
    IDN = con.ap[:, 512:640]
    MC = con.ap[:, 640:896]
    conb = P.tile([128, 384], BF16, "conb")
    P.I("dve", "tensor_copy", [con], [conb], conb.ap[:, 0:128], BO)
    P.I("dve", "tensor_copy", [con], [conb], conb.ap[:, 128:256], IDN)
    P.I("dve", "memset", [], [conb], conb.ap[:, 256:384], 1.0)
    BO_bf = conb.ap[:, 0:128]
    ID_bf = conb.ap[:, 128:256]
    ones_bf = T(conb.ap[:, 256:384], "ones_v")
    ones_bf.w = conb.w
    eps_t = P.tile([128, 3], F32, "eps")
    P.I("dve", "memset", [], [eps_t], eps_t.ap[:, 0:1], NORM_EPS)
    P.I("dve", "memset", [], [eps_t], eps_t.ap[:, 1:2], 1e-24)
    P.I("dve", "memset", [], [eps_t], eps_t.ap[:, 2:3], GN_EPS)


    PB = [T(pbank[i], f"PB{i}") for i in range(8)]

    def reg(i, name):
        return TV(PB[i // 2], pbank[i // 2][:, 256 * (i % 2):256 * (i % 2 + 1)], name)
    R_r, R_k, R_v, R_zw, R_za, R_g, R_st, R_st2 = (reg(i, f"R{i}") for i in range(8))
    B6 = TV(PB[6], pbank[6][:, :], "B6")
    B7 = TV(PB[7], pbank[7][:, :], "B7")

    scratch = P.sb([128, 8, 3 * D], BF16, "wrkv_sb")
    modv = emit_mod(P, nc, cT_d, adaw_d, 3, vecs, C_AB, scratch, R_g)
    gm = P.tile([128, 8], F32, "gm")
    P.I("dve", "scalar_tensor_tensor", [modv, vecs], [gm], gm.ap[:], modv.ap[:, 8:16], 1.0, vecs.ap[:, C_NG:C_NG + 8], ALU.add, ALU.mult)
    g1p = P.tile([128, 8], F32, "g1p")
    P.I("dve", "tensor_scalar", [modv], [g1p], g1p.ap[:], modv.ap[:, 16:24], 1.0, None, ALU.add)
    omka = P.tile([128, 8], F32, "omka")
    P.I("dve", "tensor_scalar", [vecs], [omka], omka.ap[:], vecs.ap[:, C_KA:C_KA + 8], -1.0, 1.0, ALU.mult, ALU.add)

    wrkv = T(scratch, "wrkv")
    dummy = P.tile([128, 1], F32, "dummy")
    P.I("pool", "tensor_copy", [modv], [wrkv, dummy], dummy.ap[:], modv.ap[:, 0:1])
    for p_ in range(3):
        P.dma("pool", wrkv, scratch[:, :, p_ * D:(p_ + 1) * D], None, wrkv_d[p_].rearrange("(kc p) n -> p kc n", p=128))
    wo = P.tile([128, 8, D], BF16, "wo")
    P.dma("pool", wo, wo.ap[:], None, wo_d.rearrange("(kc p) n -> p kc n", p=128))
    w1 = P.tile([128, 8, 64], BF16, "w1")
    P.dma("pool", w1, w1.ap[:], None, w1_d.rearrange("(kc p) n -> p kc n", p=128))
    a1 = P.tile([128, 8, 64], BF16, "a1")
    P.dma("pool", a1, a1.ap[:], None, a1_d.rearrange("(kc p) n -> p kc n", p=128))
    g1 = P.tile([128, 8, 256], BF16, "g1")
    P.I("pool", "memset", [], [g1], g1.ap[:], 0.0)
    P.dma("pool", g1, g1.ap[:, :, 0:160], None, g1_d.rearrange("(kc p) n -> p kc n", p=128))
    w2 = P.tile([64, D], BF16, "w2")
    P.dma("pool", w2, w2.ap[:], None, w2_d)
    a2 = P.tile([64, D], BF16, "a2")
    P.dma("pool", a2, a2.ap[:], None, a2_d)
    g2a = P.tile([128, D], BF16, "g2a")
    P.dma("pool", g2a, g2a.ap[:], None, g2_d[0:128, :])
    g2b = P.tile([128, D], BF16, "g2b")
    P.I("pool", "memset", [], [g2b], g2b.ap[:], 0.0)
    P.dma("pool", g2b, g2b.ap[0:32, :], None, g2_d[128:160, :])

    ckpt("setup", [("gm", gm, gm.ap[:], [128, 8], F32)])
    x_t = P.tile([128, 8, NT], F32, "x")
    h_t = P.tile([128, 8, NT + 1], F32, "h")
    sq_t = P.tile([128, 8, NT], BF16, "sq")
    rt_t = P.tile([128, NT], F32, "rt")
    tmp_t = P.tile([128, 8, NT], F32, "tmp")
    xm = [P.tile([128, 8, NT], BF16, f"xm{p_}") for p_ in range(6)]
    lw1 = P.tile([64, NT], BF16, "lw1")
    la1 = P.tile([64, NT], BF16, "la1")
    lg1a = P.tile([128, NT], BF16, "lg1a")
    lg1b = P.tile([128, NT], BF16, "lg1b")
    yo = P.tile([128, 8, NT], BF16, "yo")
    F = {}
    for nm in ["lw", "cum", "eg", "egm", "eneg", "a", "k", "kk", "rn", "ka", "fac", "km", "v", "bonus", "g", "Y", "yc", "sq2", "rs"]:
        F[nm] = P.tile([128, NT], F32, "f_" + nm)
    kk2 = P.tile([128, NT], BF16, "kk2")
    rkb = P.tile([128, NT], BF16, "rkb")
    AR = P.tile([128, C, 128], BF16, "AR")
    BKr = P.tile([128, C, 128], BF16, "BKr")
    bd_names = ["A_bd", "B_bd", "K_bd", "Bb_bd", "Kb_bd", "V_bd", "N_bd", "NT_bd", "T_bd", "AhT_bd", "M1T_bd", "P_bd", "W2T_bd", "TV_bd"]
    BD = {}
    for nm in bd_names:
        BD[nm] = P.tile([128, C, 128], BF16, nm)
        P.I("pool", "memset", [], [BD[nm]], BD[nm].ap[:], 0.0)
    Xt = P.tile([128, C, 128], BF16, "Xt")
    RH = {}
    for nm in ["TBr", "TKr", "TVr", "Nr", "NTr", "Tr", "Arb", "Ark", "Rhat", "M2"]:
        RH[nm] = P.tile([128, C, 64], BF16, nm)
    diagG = P.tile([128, C, 64], F32, "diagG")
    tmpP = P.tile([128, C, 64], BF16, "tmpP")
    tmpW = P.tile([128, C, 64], BF16, "tmpW")
    St_r = [P.tile([128, 64], BF16, f"St_r{j}") for j in range(8)]
    St_bd = [P.tile([128, 128], BF16, f"St_bd{j}") for j in range(8)]
    HS = (slice(0, 64), slice(64, 128))

    def halves(eng, method, reads, writes, out_fn, in_fns, *extra):
        for hh in range(2):
            hs = HS[hh]
            e_, m_ = eng, method
            if eng == "dve" and method == "tensor_copy" and hh == 1:
                e_, m_ = "act", "copy"
            P.I(e_, m_, reads, writes, out_fn(hs, hh), *[f(hs, hh) for f in in_fns], *extra)

    for j in range(8):
        P.I("pool", "memset", [], [St_bd[j]], St_bd[j].ap[:], 0.0)
        P.I("pool", "memset", [], [St_r[j]], St_r[j].ap[:], 0.0)
    P.I("dve", "memset", [], [h_t], h_t.ap[:, :, NT:NT + 1], 0.0)
    fl = vecs.ap[:, C_FLAG:C_FLAG + 1]

    v3 = lambda ap: ap.rearrange("p (c n) -> p c n", c=C)
    ckpt("init", [("hfull", h_t, h_t.ap[:], [128, 8, NT + 1], F32), ("Stbd0", St_bd[0], St_bd[0].ap[:], [128, 128], BF16)])

    for tt in range(VT // NT):
        c0 = tt * NT
        P.dma("sp", x_t, x_t.ap[:], None, xT_v[:, :, c0:c0 + NT])
        if tt == NTOK // NT:
            P.I("dve", "tensor_scalar", [h_t, vecs], [h_t], h_t.ap[:, :, 0:1], h_t.ap[:, :, NT:NT + 1], fl, None, ALU.mult)
            for j in range(8):
                P.I("dve", "tensor_scalar", [St_r[j], vecs], [St_r[j]], St_r[j].ap[:], St_r[j].ap[:], fl, None, ALU.mult)
                P.I("dve", "tensor_scalar", [St_bd[j], vecs], [St_bd[j]], St_bd[j].ap[:], St_bd[j].ap[:], fl, None, ALU.mult)
        else:
            P.I("dve", "tensor_copy", [h_t], [h_t], h_t.ap[:, :, 0:1], h_t.ap[:, :, NT:NT + 1])
        emit_norm(P, x_t, x_t.ap[:], NT, gm, gm.ap, modv, modv.ap[:, 0:8], h_t, h_t.ap[:, :, 1:NT + 1], ones_bf, R_st, sq_t, rt_t, tmp_t, eps_t)
        P.I("dve", "tensor_tensor", [h_t], [tmp_t], tmp_t.ap[:], h_t.ap[:, :, 0:NT], h_t.ap[:, :, 1:NT + 1], ALU.subtract)
        ckpt("norm", [("hfull", h_t, h_t.ap[:], [128, 8, NT + 1], F32), ("xx", tmp_t, tmp_t.ap[:], [128, 8, NT], F32)])
        for p_ in range(6):
            for j in range(8):
                eng = "dve"
                P.I(eng, "scalar_tensor_tensor", [tmp_t, vecs, h_t], [xm[p_]], xm[p_].ap[:, j, :], tmp_t.ap[:, j, :],
                    vecs.ap[:, C_MU + p_ * 8 + j:C_MU + p_ * 8 + j + 1], h_t.ap[:, j, 1:NT + 1], ALU.mult, ALU.add)
        ckpt("xm", [("xm0", xm[0], xm[0].ap[:], [128, 8, NT], BF16), ("xm5", xm[5], xm[5].ap[:], [128, 8, NT], BF16)])
        for kc in range(8):
            P.mm(R_r, pbank[0][0:64, 0:NT], w1, w1.ap[:, kc, :], xm[3], xm[3].ap[:, kc, :], start=(kc == 0), stop=(kc == 7))
        ckpt("l0", [("gm", gm, gm.ap[:], [128, 8], F32)])
        P.I("act", "activation", [R_r], [lw1], lw1.ap[:], pbank[0][0:64, 0:NT], AF.Tanh)
        ckpt("l1", [("gm", gm, gm.ap[:], [128, 8], F32)])
        for kc in range(8):
            P.mm(R_k, pbank[0][0:64, 256:256 + NT], a1, a1.ap[:, kc, :], xm[4], xm[4].ap[:, kc, :], start=(kc == 0), stop=(kc == 7))
        P.I("act", "copy", [R_k], [la1], la1.ap[:], pbank[0][0:64, 256:256 + NT])
        ckpt("l2", [("gm", gm, gm.ap[:], [128, 8], F32)])
        for kc in range(8):
            P.mm(R_v, R_v.ap[:], g1, g1.ap[:, kc, 0:128], xm[5], xm[5].ap[:, kc, :], start=(kc == 0), stop=(kc == 7))
        P.I("act", "activation", [R_v], [lg1a], lg1a.ap[:], R_v.ap[:], AF.Sigmoid)
        ckpt("l3", [("gm", gm, gm.ap[:], [128, 8], F32)])
        for kc in range(8):
            P.mm(R_zw, R_zw.ap[:], g1, g1.ap[:, kc, 128:256], xm[5], xm[5].ap[:, kc, :], start=(kc == 0), stop=(kc == 7))
        P.I("act", "activation", [R_zw], [lg1b], lg1b.ap[:], R_zw.ap[:], AF.Sigmoid)

        ckpt("lora", [("gm", gm, gm.ap[:], [128, 8], F32)])
        for j in range(8):
            js = slice(j * 128, (j + 1) * 128)
            vj = lambda col: vecs.ap[:, col + j:col + j + 1]
            for (R_, p_) in ((R_r, 0), (R_k, 1), (R_v, 2)):
                for kc in range(8):
                    P.mm(R_, R_.ap[:], wrkv, scratch[:, kc, p_ * D + j * 128:p_ * D + (j + 1) * 128], xm[p_], xm[p_].ap[:, kc, :],
                         start=(kc == 0), stop=(kc == 7))
            P.mm(R_zw, R_zw.ap[:], w2, w2.ap[:, js], lw1, lw1.ap[:])
            P.mm(R_za, R_za.ap[:], a2, a2.ap[:, js], la1, la1.ap[:])
            P.mm(R_g, R_g.ap[:], g2a, g2a.ap[:, js], lg1a, lg1a.ap[:], start=True, stop=False)
            P.mm(R_g, R_g.ap[:], g2b, g2b.ap[:, js], lg1b, lg1b.ap[:], start=False, stop=True)
            P.I("act", "activation", [R_zw, vecs], [F["lw"]], F["lw"].ap[:], R_zw.ap[:], AF.Sigmoid, bias=vj(C_W0))
            P.I("act", "activation", [R_za, vecs], [F["a"]], F["a"].ap[:], R_za.ap[:], AF.Sigmoid, bias=vj(C_A0))
            P.I("act", "copy", [R_k], [F["k"]], F["k"].ap[:], R_k.ap[:])
            P.I("act", "activation", [R_k, vecs], [kk2], kk2.ap[:], R_k.ap[:], AF.Square, scale=vj(C_KK))
            P.mm(R_st, R_st.ap[:], conb, BO_bf, kk2, kk2.ap[:])
            P.I("dve", "tensor_scalar", [F["lw"]], [F["lw"]], F["lw"].ap[:], F["lw"].ap[:], -EXPM05, None, ALU.mult)
            P.I("dve", "tensor_tensor_scan", [con, F["lw"]], [F["cum"]], F["cum"].ap[:], MC, F["lw"].ap[:], 0.0, ALU.mult, ALU.add)
            P.I("dve", "tensor_tensor", [F["cum"], F["lw"]], [F["egm"]], F["egm"].ap[:], F["cum"].ap[:], F["lw"].ap[:], ALU.subtract)
            P.I("dve", "tensor_scalar", [F["k"], vecs], [F["kk"]], F["kk"].ap[:], F["k"].ap[:], vj(C_KK), None, ALU.mult)
            P.I("act", "activation", [F["cum"]], [F["eg"]], F["eg"].ap[:], F["cum"].ap[:], AF.Exp)
            P.I("act", "activation", [F["cum"]], [F["eneg"]], F["eneg"].ap[:], F["cum"].ap[:], AF.Exp, scale=-1.0)
            P.I("act", "activation", [F["egm"]], [F["egm"]], F["egm"].ap[:], F["egm"].ap[:], AF.Exp)
            P.I("act", "activation", [R_st, eps_t], [F["rn"]], F["rn"].ap[:], R_st.ap[:], AF.Ln, bias=eps_t.ap[:, 1:2])
            P.I("act", "activation", [F["rn"]], [F["rn"]], F["rn"].ap[:], F["rn"].ap[:], AF.Exp, scale=-0.5)
            gL = v3(F["eg"].ap[:])[:, :, 63:64]
            gLb = gL.broadcast_to([128, C, 64])
            P.I("dve", "tensor_tensor", [F["kk"], F["rn"]], [F["kk"]], F["kk"].ap[:], F["kk"].ap[:], F["rn"].ap[:], ALU.mult)
            P.I("dve", "scalar_tensor_tensor", [F["kk"], F["egm"]], [AR], AR.ap[:, :, 0:64], v3(F["kk"].ap[:]), -1.0, v3(F["egm"].ap[:]), ALU.mult, ALU.mult)
            P.I("dve", "tensor_tensor", [R_r, F["eg"]], [AR], AR.ap[:, :, 64:128], v3(R_r.ap[:]), v3(F["eg"].ap[:]), ALU.mult)
            halves("dve", "tensor_copy", [AR], [BD["A_bd"]], lambda hs, hh: BD["A_bd"].ap[hs, :, 64 * hh:64 * hh + 64], [lambda hs, hh: AR.ap[hs, :, 0:64]])
            P.I("dve", "tensor_tensor", [F["kk"], F["a"]], [F["ka"]], F["ka"].ap[:], F["kk"].ap[:], F["a"].ap[:], ALU.mult)
            P.I("dve", "tensor_tensor", [F["ka"], F["eneg"]], [BKr], BKr.ap[:, :, 0:64], v3(F["ka"].ap[:]), v3(F["eneg"].ap[:]), ALU.mult)
            halves("dve", "tensor_copy", [BKr], [BD["B_bd"]], lambda hs, hh: BD["B_bd"].ap[hs, :, 64 * hh:64 * hh + 64], [lambda hs, hh: BKr.ap[hs, :, 0:64]])
            halves("dve", "tensor_tensor", [BKr, F["eg"]], [BD["Bb_bd"]], lambda hs, hh: BD["Bb_bd"].ap[hs, :, 64 * hh:64 * hh + 64],
                   [lambda hs, hh: BKr.ap[hs, :, 0:64], lambda hs, hh: gLb[hs]], ALU.mult)
            P.I("dve", "tensor_scalar", [F["a"], vecs, omka], [F["fac"]], F["fac"].ap[:], F["a"].ap[:], vj(C_KA), omka.ap[:, j:j + 1], ALU.mult, ALU.add)
            P.I("dve", "tensor_tensor", [F["k"], F["fac"]], [F["km"]], F["km"].ap[:], F["k"].ap[:], F["fac"].ap[:], ALU.mult)
            P.I("dve", "tensor_tensor", [F["km"], F["eneg"]], [BKr], BKr.ap[:, :, 64:128], v3(F["km"].ap[:]), v3(F["eneg"].ap[:]), ALU.mult)
            halves("dve", "tensor_copy", [BKr], [BD["K_bd"]], lambda hs, hh: BD["K_bd"].ap[hs, :, 64 * hh:64 * hh + 64], [lambda hs, hh: BKr.ap[hs, :, 64:128]])
            halves("dve", "tensor_tensor", [BKr, F["eg"]], [BD["Kb_bd"]], lambda hs, hh: BD["Kb_bd"].ap[hs, :, 64 * hh:64 * hh + 64],
                   [lambda hs, hh: BKr.ap[hs, :, 64:128], lambda hs, hh: gLb[hs]], ALU.mult)
            P.I("dve", "scalar_tensor_tensor", [R_r, vecs, F["km"]], [rkb], rkb.ap[:], R_r.ap[:], vj(C_RK), F["km"].ap[:], ALU.mult, ALU.mult)
            P.mm(R_st2, R_st2.ap[:], conb, BO_bf, rkb, rkb.ap[:])
            P.I("act", "copy", [R_v], [F["v"]], F["v"].ap[:], R_v.ap[:])
            P.I("dve", "tensor_tensor", [R_st2, F["v"]], [F["bonus"]], F["bonus"].ap[:], R_st2.ap[:], F["v"].ap[:], ALU.mult)
            halves("dve", "tensor_copy", [F["v"]], [BD["V_bd"]], lambda hs, hh: BD["V_bd"].ap[hs, :, 64 * hh:64 * hh + 64], [lambda hs, hh: v3(F["v"].ap[:])[hs]])
            P.I("act", "copy", [R_g], [F["g"]], F["g"].ap[:], R_g.ap[:])
            P.I("dve", "tensor_tensor", [con, F["eg"]], [diagG], diagG.ap[:], IQ.unsqueeze(1).broadcast_to([128, C, 64]), gLb, ALU.mult)

            ckpt("B", [("AR", AR, AR.ap[:], [128, C, 128], BF16), ("BKr", BKr, BKr.ap[:], [128, C, 128], BF16), ("cum", F["cum"], F["cum"].ap[:], [128, NT], F32),
                       ("bonus", F["bonus"], F["bonus"].ap[:], [128, NT], F32), ("Vbd", BD["V_bd"], BD["V_bd"].ap[:], [128, C, 128], BF16),
                       ("Bbbd", BD["Bb_bd"], BD["Bb_bd"].ap[:], [128, C, 128], BF16), ("diagG", diagG, diagG.ap[:], [128, C, 64], F32)])
            tpf = [TV(PB[4], pbank[4][:, :].rearrange("p (c n) -> p c n", c=C), "tpA"), TV(PB[5], pbank[5][:, :].rearrange("p (c n) -> p c n", c=C), "tpB")]

            def do_tp(i_, nm):
                tp = tpf[i_ % 2]
                for c in range(C):
                    P.mm(tp, tp.ap[:, c, :], BD[nm], BD[nm].ap[:, c, :], conb, ID_bf)
                return tp
            tp = do_tp(0, "A_bd")
            ckpt("T0", [("gm", gm, gm.ap[:], [128, 8], F32)])
            halves("act", "copy", [tp], [Xt], lambda hs, hh: Xt.ap[hs, :, 0:64], [lambda hs, hh: tp.ap[hs, :, 64 * hh:64 * hh + 64]])
            ckpt("T1", [("Xt", Xt, Xt.ap[:], [128, C, 128], BF16)])
            tp = do_tp(1, "Bb_bd")
            halves("act", "copy", [tp], [RH["TBr"]], lambda hs, hh: RH["TBr"].ap[hs], [lambda hs, hh: tp.ap[hs, :, 64 * hh:64 * hh + 64]])
            tp = do_tp(2, "Kb_bd")
            halves("act", "copy", [tp], [RH["TKr"]], lambda hs, hh: RH["TKr"].ap[hs], [lambda hs, hh: tp.ap[hs, :, 64 * hh:64 * hh + 64]])
            tp = do_tp(3, "V_bd")
            halves("act", "copy", [tp], [RH["TVr"]], lambda hs, hh: RH["TVr"].ap[hs], [lambda hs, hh: tp.ap[hs, :, 64 * hh:64 * hh + 64]])
            P.I("act", "copy", [tp], [BD["TV_bd"]], BD["TV_bd"].ap[:], tp.ap[:])
            ckpt("T", [("Xt", Xt, Xt.ap[:], [128, C, 128], BF16), ("TVbd", BD["TV_bd"], BD["TV_bd"].ap[:], [128, C, 128], BF16)])
            b6 = B6.ap[:].rearrange("p (c n) -> p c n", c=C)
            b7 = B7.ap[:].rearrange("p (c n) -> p c n", c=C)
            zw3 = v3(R_zw.ap[:])
            for c in range(C):
                P.mm(B6, b6[:, c, :], BD["B_bd"], BD["B_bd"].ap[:, c, :], AR, AR.ap[:, c, :])
            for c in range(C):
                P.mm(R_zw, zw3[:, c, :], BD["K_bd"], BD["K_bd"].ap[:, c, :], AR, AR.ap[:, c, 64:128])
            for c in range(C):
                P.mm(B7, b7[:, c, :], BD["A_bd"], BD["A_bd"].ap[:, c, :], BKr, BKr.ap[:, c, :])
            MSb = MS.unsqueeze(1).broadcast_to([128, C, 64])
            MIb = MI.unsqueeze(1).broadcast_to([128, C, 64])
            MTb = MT.unsqueeze(1).broadcast_to([128, C, 64])
            IQb = IQ.unsqueeze(1).broadcast_to([128, C, 64])
            P.I("dve", "tensor_tensor", [B6, con], [RH["Nr"]], RH["Nr"].ap[:], b6[:, :, 0:64], MSb, ALU.mult)
            P.I("dve", "tensor_tensor", [B6, con], [RH["Arb"]], RH["Arb"].ap[:], b6[:, :, 64:128], MIb, ALU.mult)
            P.I("dve", "tensor_tensor", [R_zw, con], [RH["Ark"]], RH["Ark"].ap[:], zw3, MIb, ALU.mult)
            P.I("dve", "tensor_tensor", [B7, con], [RH["NTr"]], RH["NTr"].ap[:], b7[:, :, 0:64], MTb, ALU.mult)
            P.I("dve", "tensor_tensor", [B7, con], [Xt], Xt.ap[:, :, 64:128], b7[:, :, 64:128], MTb, ALU.mult)
            P.I("dve", "tensor_tensor", [RH["Nr"], con], [RH["Tr"]], RH["Tr"].ap[:], RH["Nr"].ap[:], IQb, ALU.add)
            halves("dve", "tensor_copy", [RH["Nr"]], [BD["N_bd"]], lambda hs, hh: BD["N_bd"].ap[hs, :, 64 * hh:64 * hh + 64], [lambda hs, hh: RH["Nr"].ap[hs]])
            halves("dve", "tensor_copy", [RH["NTr"]], [BD["NT_bd"]], lambda hs, hh: BD["NT_bd"].ap[hs, :, 64 * hh:64 * hh + 64], [lambda hs, hh: RH["NTr"].ap[hs]])
            ckpt("C1", [("Nr", RH["Nr"], RH["Nr"].ap[:], [128, C, 64], BF16), ("NTr", RH["NTr"], RH["NTr"].ap[:], [128, C, 64], BF16),
                        ("Xt", Xt, Xt.ap[:], [128, C, 128], BF16), ("TVbd", BD["TV_bd"], BD["TV_bd"].ap[:], [128, C, 128], BF16),
                        ("Arb", RH["Arb"], RH["Arb"].ap[:], [128, C, 64], BF16), ("Nbd", BD["N_bd"], BD["N_bd"].ap[:], [128, C, 128], BF16)])
            qa = v3(pbank[6][:, 0:256])
            qb = v3(pbank[6][:, 256:512])
            qc = v3(pbank[7][:, 0:256])
            qd = v3(pbank[7][:, 256:512])
            NLEV = 5

            def squarings(do_a):
                if do_a:
                    for c in range(C):
                        P.mm(B6, qa[:, c, :], BD["NT_bd"], BD["NT_bd"].ap[:, c, :], RH["Nr"], RH["Nr"].ap[:, c, :])
                for c in range(C):
                    P.mm(B6, qb[:, c, :], BD["N_bd"], BD["N_bd"].ap[:, c, :], RH["NTr"], RH["NTr"].ap[:, c, :])

            def evac_forms(do_a):
                if do_a:
                    P.I("act", "copy", [B6], [RH["Nr"]], RH["Nr"].ap[:], qa)
                    P.I("act", "copy", [B6], [RH["NTr"]], RH["NTr"].ap[:], qb)
                    halves("dve", "tensor_copy", [RH["Nr"]], [BD["N_bd"]], lambda hs, hh: BD["N_bd"].ap[hs, :, 64 * hh:64 * hh + 64], [lambda hs, hh: RH["Nr"].ap[hs]])
                    halves("dve", "tensor_copy", [RH["NTr"]], [BD["NT_bd"]], lambda hs, hh: BD["NT_bd"].ap[hs, :, 64 * hh:64 * hh + 64], [lambda hs, hh: RH["NTr"].ap[hs]])
                else:
                    halves("act", "copy", [B6], [BD["NT_bd"]], lambda hs, hh: BD["NT_bd"].ap[hs, :, 64 * hh:64 * hh + 64], [lambda hs, hh: qb[hs]])

            squarings(True)
            evac_forms(True)
            for lev in range(1, NLEV + 1):
                for c in range(C):
                    P.mm(B7, qc[:, c, :], BD["NT_bd"], BD["NT_bd"].ap[:, c, :], RH["Tr"], RH["Tr"].ap[:, c, :])
                if lev < NLEV:
                    squarings(lev + 1 < NLEV)
                P.I("dve", "tensor_tensor", [B7, RH["Tr"]], [RH["Tr"]], RH["Tr"].ap[:], qc, RH["Tr"].ap[:], ALU.add)
                if lev < NLEV:
                    evac_forms(lev + 1 < NLEV)
            halves("dve", "tensor_copy", [RH["Tr"]], [BD["T_bd"]], lambda hs, hh: BD["T_bd"].ap[hs, :, 64 * hh:64 * hh + 64], [lambda hs, hh: RH["Tr"].ap[hs]])
            ckpt("inv", [("Tr", RH["Tr"], RH["Tr"].ap[:], [128, C, 64], BF16)])
            for c in range(C):
                P.mm(B6, b6[:, c, :], BD["T_bd"], BD["T_bd"].ap[:, c, :], Xt, Xt.ap[:, c, :])
            halves("act", "copy", [B6], [BD["AhT_bd"]], lambda hs, hh: BD["AhT_bd"].ap[hs, :, 64 * hh:64 * hh + 64], [lambda hs, hh: b6[hs, :, 0:64]])
            halves("act", "copy", [B6], [BD["M1T_bd"]], lambda hs, hh: BD["M1T_bd"].ap[hs, :, 64 * hh:64 * hh + 64], [lambda hs, hh: b6[hs, :, 64:128]])
            for c in range(C):
                P.mm(B7, qc[:, c, :], BD["AhT_bd"], BD["AhT_bd"].ap[:, c, :], RH["Arb"], RH["Arb"].ap[:, c, :])
            for c in range(C):
                P.mm(B7, qd[:, c, :], BD["M1T_bd"], BD["M1T_bd"].ap[:, c, :], RH["Arb"], RH["Arb"].ap[:, c, :])
            P.I("dve", "tensor_tensor", [B7, AR], [RH["Rhat"]], RH["Rhat"].ap[:], qc, AR.ap[:, :, 64:128], ALU.add)
            P.I("dve", "tensor_tensor", [B7, RH["Ark"]], [RH["M2"]], RH["M2"].ap[:], qd, RH["Ark"].ap[:], ALU.add)
            for c in range(C):
                P.mm(B6, qa[:, c, :], BD["AhT_bd"], BD["AhT_bd"].ap[:, c, :], RH["TBr"], RH["TBr"].ap[:, c, :])
            for c in range(C):
                P.mm(B6, qb[:, c, :], BD["M1T_bd"], BD["M1T_bd"].ap[:, c, :], RH["TBr"], RH["TBr"].ap[:, c, :])
            P.I("dve", "tensor_tensor", [B6, diagG], [tmpP], tmpP.ap[:], qa, diagG.ap[:], ALU.add)
            P.I("dve", "tensor_tensor", [B6, RH["TKr"]], [tmpW], tmpW.ap[:], qb, RH["TKr"].ap[:], ALU.add)
            halves("dve", "tensor_copy", [tmpP], [BD["P_bd"]], lambda hs, hh: BD["P_bd"].ap[hs, :, 64 * hh:64 * hh + 64], [lambda hs, hh: tmpP.ap[hs]])
            halves("dve", "tensor_copy", [tmpW], [BD["W2T_bd"]], lambda hs, hh: BD["W2T_bd"].ap[hs, :, 64 * hh:64 * hh + 64], [lambda hs, hh: tmpW.ap[hs]])

            ckpt("C2", [("Rhat", RH["Rhat"], RH["Rhat"].ap[:], [128, C, 64], BF16), ("M2", RH["M2"], RH["M2"].ap[:], [128, C, 64], BF16),
                        ("Pbd", BD["P_bd"], BD["P_bd"].ap[:], [128, C, 128], BF16), ("W2Tbd", BD["W2T_bd"], BD["W2T_bd"].ap[:], [128, C, 128], BF16)])
            for c in range(C):
                P.mm(R_r, R_r.ap[:, c * 64:(c + 1) * 64], St_bd[j], St_bd[j].ap[:], RH["Rhat"], RH["Rhat"].ap[:, c, :], start=True, stop=False)
                P.mm(R_r, R_r.ap[:, c * 64:(c + 1) * 64], BD["TV_bd"], BD["TV_bd"].ap[:, c, :], RH["M2"], RH["M2"].ap[:, c, :], start=False, stop=True)
                P.mm(R_st, R_st.ap[:, 0:64], BD["P_bd"], BD["P_bd"].ap[:, c, :], St_r[j], St_r[j].ap[:], start=True, stop=False)
                P.mm(R_st, R_st.ap[:, 0:64], BD["W2T_bd"], BD["W2T_bd"].ap[:, c, :], RH["TVr"], RH["TVr"].ap[:, c, :], start=False, stop=True)
                P.I("act", "copy", [R_st], [St_r[j]], St_r[j].ap[:], R_st.ap[:, 0:64])
                for hh in range(2):
                    P.I("act", "copy", [R_st], [St_bd[j]], St_bd[j].ap[HS[hh], 64 * hh:64 * hh + 64], R_st.ap[HS[hh], 0:64])

            ckpt("D", [("Stbd", St_bd[j], St_bd[j].ap[:], [128, 128], BF16)])
            P.I("act", "copy", [R_r], [F["Y"]], F["Y"].ap[:], R_r.ap[:])
            P.mm(R_st2, R_st2.ap[:], con, BO64, F["Y"], F["Y"].ap[:])
            P.I("dve", "tensor_tensor", [F["Y"], R_st2], [F["yc"]], F["yc"].ap[:], F["Y"].ap[:], R_st2.ap[:], ALU.subtract)
            P.I("act", "activation", [F["yc"]], [F["sq2"]], F["sq2"].ap[:], F["yc"].ap[:], AF.Square)
            P.mm(R_k, R_k.ap[:], con, BO64, F["sq2"], F["sq2"].ap[:])
            P.I("act", "activation", [R_k, eps_t], [F["rs"]], F["rs"].ap[:], R_k.ap[:], AF.Ln, bias=eps_t.ap[:, 2:3])
            P.I("act", "activation", [F["rs"]], [F["rs"]], F["rs"].ap[:], F["rs"].ap[:], AF.Exp, scale=-0.5)
            P.I("dve", "tensor_tensor", [F["yc"], F["rs"]], [F["yc"]], F["yc"].ap[:], F["yc"].ap[:], F["rs"].ap[:], ALU.mult)
            P.I("act", "activation", [F["yc"], vecs], [F["yc"]], F["yc"].ap[:], F["yc"].ap[:], AF.Identity, bias=vj(C_GNB), scale=vj(C_GNW))
            P.I("dve", "tensor_tensor", [F["yc"], F["bonus"]], [F["yc"]], F["yc"].ap[:], F["yc"].ap[:], F["bonus"].ap[:], ALU.add)
            P.I("dve", "tensor_tensor", [F["yc"], F["g"]], [yo], yo.ap[:, j, :], F["yc"].ap[:], F["g"].ap[:], ALU.mult)

        ckpt("E", [("yo", yo, yo.ap[:], [128, 8, NT], BF16)])
        o_ring = Ring([R_v, R_za, R_g])
        for jo in range(8):
            ps = o_ring.next()
            for kc in range(8):
                P.mm(ps, ps.ap[:], wo, wo.ap[:, kc, jo * 128:(jo + 1) * 128], yo, yo.ap[:, kc, :], start=(kc == 0), stop=(kc == 7))
            P.I("dve", "scalar_tensor_tensor", [ps, g1p, x_t], [x_t], x_t.ap[:, jo, :], ps.ap[:], g1p.ap[:, jo:jo + 1], x_t.ap[:, jo, :], ALU.mult, ALU.add)
        P.dma("sp", None, yT_v[:, :, c0:c0 + NT], x_t, x_t.ap[:])


NCORES = 8
RUN_KW = {}
LAST_RES = None
ARENA_BYTES = 211968
_FUSED = []


BLOCKS = ("rw", "f0", "lr", "f1")


def build_fused():
    nc, P = new_prog()
    pbank = [P.ps([128, 512], F32, f"pb{i}") for i in range(8)]
    P.arena_init(ARENA_BYTES)
    VT = 2 * NTOK
    xT_d = nc.dram_tensor("xT", [D, VT], F32, kind="ExternalInput").ap()
    cT_d = nc.dram_tensor("cT", [128, 8], F32, kind="ExternalInput").ap()
    x1_d = nc.dram_tensor("x1_scr", [D, VT], F32, kind="Internal").ap()
    x2_d = nc.dram_tensor("x2_scr", [D, VT], F32, kind="Internal").ap()
    x3_d = nc.dram_tensor("x3_scr", [D, NTOK], F32, kind="Internal").ap()
    yT_d = nc.dram_tensor("yT", [D, NTOK], F32, kind="ExternalOutput").ap()
    fm = lambda ap: ap.rearrange("(j p) t -> p j t", p=128)
    if "rw" in BLOCKS:
        emit_rwkv(P, nc, pbank, fm(xT_d), fm(x1_d), "rw_", cT_d)
        P.barrier()
        P.arena_reset()
    if "f0" in BLOCKS:
        emit_ffn(P, nc, pbank, fm(x1_d if "rw" in BLOCKS else xT_d), fm(x2_d), VT, False, "f0_", cT_d)
        P.barrier()
        P.arena_reset()
    if "lr" in BLOCKS:
        emit_lru(P, nc, pbank, fm(x2_d if "f0" in BLOCKS else xT_d), fm(x3_d), "lr_", cT_d)
        P.barrier()
        P.arena_reset()
    if "f1" in BLOCKS:
        emit_ffn(P, nc, pbank, fm(x3_d if "lr" in BLOCKS else xT_d[:, 0:NTOK]), fm(yT_d), NTOK, True, "f1_", cT_d)
    P.emit()
    P.close()
    return nc, P


IDENT = np.eye(128, dtype=np.float32)


def kernel(x, c, ada_w, ada_b, norm_g, final_g,
           rwkv_mu, rwkv_w_rkv, rwkv_w_o, rwkv_w0, rwkv_w1, rwkv_w2, rwkv_a0, rwkv_a1, rwkv_a2,
           rwkv_g1, rwkv_g2, rwkv_k_k, rwkv_k_a, rwkv_r_k, rwkv_gn_w, rwkv_gn_b,
           lru_w_in, lru_conv_w, lru_conv_b, lru_w_gates, lru_b_gates, lru_lam, lru_w_out,
           ffn_w_gu, ffn_w_d, moe_w_router, moe_b_router, moe_w_gu, moe_w_d):
    f = lambda a: np.ascontiguousarray(np.asarray(a, dtype=np.float32))
    x, c, ada_w, ada_b, norm_g, final_g = f(x), f(c), f(ada_w), f(ada_b), f(norm_g), f(final_g)
    if not _FUSED:
        _FUSED.append(build_fused())
    nc, _ = _FUSED[0]
    B = x.shape[0]
    consts = rwkv_consts()
    bg = f(lru_b_gates)[0]
    cw = f(lru_conv_w)[0]
    shared = {
        "rw_consts": consts, "rw_adaw": f(ada_w[0][:, 0:3 * D]), "rw_wrkv": f(rwkv_w_rkv)[0], "rw_wo": f(rwkv_w_o)[0],
        "rw_w1": f(rwkv_w1)[0], "rw_w2": f(rwkv_w2)[0], "rw_a1": f(rwkv_a1)[0], "rw_a2": f(rwkv_a2)[0],
        "rw_g1": f(rwkv_g1)[0], "rw_g2": f(rwkv_g2)[0],
        "f0_vecs": pack_vecs([("ng", norm_g[0, 1]), ("adab", ada_b[0][3 * D:6 * D])])[0], "f0_adaw": f(ada_w[0][:, 3 * D:6 * D]),
        "f0_wgu": f(ffn_w_gu), "f0_wd": f(ffn_w_d),
        "lr_adaw": f(ada_w[1][:, 0:3 * D]), "lr_win": f(lru_w_in)[0], "lr_wg": f(lru_w_gates)[0], "lr_wout": f(lru_w_out)[0],
        "f1_vecs": pack_vecs([("ng", norm_g[1, 1]), ("adab", ada_b[1][3 * D:6 * D]), ("fg", final_g)])[0],
        "f1_adaw": f(ada_w[1][:, 3 * D:6 * D]), "f1_wgu": f(moe_w_gu)[0], "f1_wd": f(moe_w_d)[0], "f1_ident": IDENT,
        "f1_wr": f(moe_w_router)[0], "f1_br": f(np.broadcast_to(f(moe_b_router)[0].reshape(1, NE), (128, NE))),
    }
    in_maps = []
    for core in range(NCORES):
        b, half = core // 2, core % 2
        flag = np.full((128,), float(half), np.float32)
        xv = np.empty((D, 2 * NTOK), np.float32)
        if half == 1:
            xv[:, :] = x[b].T
        else:
            xv[:, :NTOK] = x[b, 0:NTOK].T
            xv[:, NTOK:] = x[b, 0:NTOK].T
        rw_vecs = pack_vecs([("ng", norm_g[0, 0]), ("adab", ada_b[0][0:3 * D]), ("mu", f(rwkv_mu)[0].reshape(-1)), ("w0", f(rwkv_w0)[0]),
                             ("a0", f(rwkv_a0)[0]), ("kk", f(rwkv_k_k)[0]), ("ka", f(rwkv_k_a)[0]), ("rk", f(rwkv_r_k)[0].reshape(-1)),
                             ("gnw", f(rwkv_gn_w)[0]), ("gnb", f(rwkv_gn_b)[0]), ("flag", flag)])[0]
        lr_vecs = pack_vecs([("ng", norm_g[1, 0]), ("adab", ada_b[1][0:3 * D]), ("cw0", cw[0]), ("cw1", cw[1]), ("cw2", cw[2]), ("cw3", cw[3]),
                             ("cb", f(lru_conv_b)[0]), ("bgr", bg[:, 0:256].reshape(-1)), ("bgi", bg[:, 256:512].reshape(-1)),
                             ("lam", f(lru_lam)[0]), ("flag", flag)])[0]
        m = dict(shared)
        m.update({"xT": xv, "cT": pack_vec(c[b]), "rw_vecs": rw_vecs, "lr_vecs": lr_vecs})
        in_maps.append(m)
    if len(BLOCKS) < 4:
        pre = tuple(b_ + "_" for b_ in BLOCKS)
        in_maps = [{k: v for k, v in m.items() if k in ("xT", "cT") or k.startswith(pre)} for m in in_maps]
    res = run_bass_kernel_spmd(nc, in_maps, core_ids=list(range(NCORES)), **RUN_KW)
    global LAST_RES
    LAST_RES = res
    out = np.empty((B, 2 * NTOK, D), np.float32)
    for core in range(NCORES):
        b, half = core // 2, core % 2
        out[b, half * NTOK:(half + 1) * NTOK, :] = res.results[core]["yT"].T
    return out
```

```python
import contextlib
import numpy as np
import concourse.bass as bass
import concourse.mybir as mybir

F32 = mybir.dt.float32
BF16 = mybir.dt.bfloat16
ALU = mybir.AluOpType
AF = mybir.ActivationFunctionType
AX = mybir.AxisListType

ENGS = ("pe", "act", "dve", "pool", "sp")
DMA_RING = 8


class T:
    __slots__ = ("ap", "w", "r", "name")

    def __init__(self, ap, name=""):
        self.ap = ap
        self.w = None
        self.r = []
        self.name = name

    def __getitem__(self, idx):
        return self.ap[idx]


class TV:
    def __init__(self, parent, ap, name=""):
        self.parent = parent
        self.ap = ap
        self.name = name

    @property
    def w(self):
        return self.parent.w

    @w.setter
    def w(self, v):
        self.parent.w = v

    @property
    def r(self):
        return self.parent.r

    @r.setter
    def r(self, v):
        self.parent.r = v


class Op:
    __slots__ = ("eng", "fn", "deps", "signal", "count", "is_dma", "dma_idx", "idx")

    def __init__(self, eng, fn, is_dma):
        self.eng = eng
        self.fn = fn
        self.deps = []
        self.signal = False
        self.count = 0
        self.is_dma = is_dma
        self.dma_idx = -1
        self.idx = -1


class Prog:
    def __init__(self, nc):
        self.nc = nc
        self.ops = []
        self.stack = contextlib.ExitStack()
        self.n_alloc = 0
        self.arena = None
        self.arena_off = 0
        self.arena_size = 0
        self.fence = {}
        self.last_op = {}
        self.recent_dma = {e: [] for e in ENGS}

    def arena_init(self, nbytes):
        self.arena_size = nbytes
        self.arena = self.stack.enter_context(self.nc.sbuf_tensor("arena", [128, nbytes // 2], BF16))
        self.arena_off = 0

    def arena_reset(self):
        self.arena_off = 0

    def barrier(self):
        deps = [o for o in self.last_op.values()]
        for e in ENGS:
            deps.extend(self.recent_dma[e])
        for e in ENGS:
            self.fence[e] = list(deps)

    def sb(self, shape, dtype, name=None):
        if self.arena is not None:
            esize = 4 if dtype == F32 else 2
            n = 1
            for d_ in shape[1:]:
                n *= d_
            off = (self.arena_off + 63) // 64 * 64
            self.arena_off = off + n * esize
            assert self.arena_off <= self.arena_size, ("SBUF arena overflow", name, self.arena_off)
            ap = self.arena[0:shape[0], off // 2:(off + n * esize) // 2]
            if dtype == F32:
                ap = ap.bitcast(F32)
            if len(shape) == 3:
                ap = ap.rearrange("p (a b) -> p a b", a=shape[1])
            elif len(shape) == 4:
                ap = ap.rearrange("p (a b c) -> p a b c", a=shape[1], b=shape[2])
            return ap
        self.n_alloc += 1
        name = f"sb{self.n_alloc}_{name or ''}"
        h = self.stack.enter_context(self.nc.sbuf_tensor(name, list(shape), dtype))
        return h

    def ps(self, shape, dtype, name=None):
        self.n_alloc += 1
        name = f"ps{self.n_alloc}_{name or ''}"
        h = self.stack.enter_context(self.nc.psum_tensor(name, list(shape), dtype))
        return h

    def tile(self, shape, dtype, name=None):
        h = self.sb(shape, dtype, name)
        return T(h[:] if False else h, name or "")

    def op(self, eng, fn, reads=(), writes=(), dma=False):
        o = Op(eng, fn, dma)
        o.idx = len(self.ops)
        deps = []
        for t in reads:
            if t.w is not None:
                deps.append((t.w, 0))
        for t in writes:
            if t.w is not None:
                deps.append((t.w, 0))
            for r in t.r:
                deps.append((r, 1))
        if self.fence.get(eng):
            for d in self.fence[eng]:
                deps.append((d, 0))
            self.fence[eng] = None
        seen = set()
        for d, war in deps:
            if d.idx in seen:
                continue
            if (not d.is_dma) and d.eng == eng and (eng == "pe" or war):
                continue
            seen.add(d.idx)
            o.deps.append(d)
        for t in reads:
            if not dma:
                t.r = [x for x in t.r if x.is_dma or x.eng != eng]
            t.r.append(o)
        for t in writes:
            t.w = o
            t.r = []
        self.ops.append(o)
        if dma:
            self.recent_dma[eng] = (self.recent_dma[eng] + [o])[-DMA_RING:]
        else:
            self.last_op[eng] = o
        return o

    def emit(self):
        nc = self.nc
        ops = self.ops
        for o in ops:
            for d in o.deps:
                d.signal = True
        cnt = {e: 0 for e in ENGS}
        dcnt = {e: 0 for e in ENGS}
        for o in ops:
            if o.is_dma:
                o.dma_idx = dcnt[o.eng]
                dcnt[o.eng] += 1
            elif o.signal:
                cnt[o.eng] += 1
                o.count = cnt[o.eng]
        sems = {}
        for e in ENGS:
            sems[e] = self.stack.enter_context(nc.semaphore(f"s_{e}"))
        dsems = {}
        for e in ENGS:
            if dcnt[e] > 0:
                dsems[e] = [self.stack.enter_context(nc.semaphore(f"d_{e}{i}")) for i in range(DMA_RING)]
        per_eng = {e: [o for o in ops if o.eng == e] for e in ENGS}
        self.stats = {e: (len(per_eng[e]), cnt[e], dcnt[e]) for e in ENGS}

        def sem_target(d):
            if d.is_dma:
                return dsems[d.eng][d.dma_idx % DMA_RING], 16 * (d.dma_idx // DMA_RING + 1)
            return sems[d.eng], d.count

        def run_engine(e, eng):
            waited = {}
            nwait = 0
            for o in per_eng[e]:
                need = {}
                for d in o.deps:
                    s, v = sem_target(d)
                    key = id(s)
                    if waited.get(key, 0) >= v:
                        continue
                    if key not in need or need[key][1] < v:
                        need[key] = (s, v)
                if o.is_dma and o.dma_idx >= DMA_RING:
                    s = dsems[e][o.dma_idx % DMA_RING]
                    v = 16 * (o.dma_idx // DMA_RING)
                    key = id(s)
                    if waited.get(key, 0) < v and (key not in need or need[key][1] < v):
                        need[key] = (s, v)
                for key, (s, v) in need.items():
                    eng.wait_ge(s, v)
                    waited[key] = v
                    nwait += 1
                ins = o.fn(eng)
                if o.is_dma:
                    s, _ = sem_target(o)
                    ins.then_inc(s, 16)
                elif o.signal:
                    ins.then_inc(sems[e], 1)
            if e == "sp":
                for q in ENGS:
                    for i in range(min(DMA_RING, dcnt[q])):
                        n = (dcnt[q] - 1 - i) // DMA_RING + 1
                        eng.wait_ge(dsems[q][i], 16 * n)
            return nwait

        block = self.stack.enter_context(nc.Block())
        self.nwaits = {}

        if per_eng["pe"]:
            @block.tensor
            def _(eng):
                self.nwaits["pe"] = run_engine("pe", eng)
        if per_eng["act"]:
            @block.scalar
            def _(eng):
                self.nwaits["act"] = run_engine("act", eng)
        if per_eng["dve"]:
            @block.vector
            def _(eng):
                self.nwaits["dve"] = run_engine("dve", eng)
        if per_eng["pool"]:
            @block.gpsimd
            def _(eng):
                self.nwaits["pool"] = run_engine("pool", eng)
        if True:
            @block.sync
            def _(eng):
                self.nwaits["sp"] = run_engine("sp", eng)

    def close(self):
        self.stack.close()

    def I(self, eng, method, reads, writes, *args, **kw):
        return self.op(eng, lambda e: getattr(e, method)(*args, **kw), list(reads), list(writes))

    def dma(self, eng, out_t, out_ap, in_t, in_ap, **kw):
        reads = [in_t] if in_t is not None else []
        writes = [out_t] if out_t is not None else []
        return self.op(eng, lambda e: e.dma_start(out=out_ap, in_=in_ap, **kw), reads, writes, dma=True)

    def mm(self, out_t, out_ap, lhsT_t, lhsT_ap, rhs_t, rhs_ap, start=True, stop=True, extra_reads=(), **kw):
        return self.op("pe", lambda e: e.matmul(out_ap, lhsT_ap, rhs_ap, start=start, stop=stop, **kw),
                       [lhsT_t, rhs_t] + list(extra_reads), [out_t])


from concourse.bass_utils import run_bass_kernel_spmd

D = 1024
JC = 8
NTOK = 2048
FF = 3584
FC = 28
NE = 8
NORM_EPS = 1e-6


def pack_vec(v):
    v = np.asarray(v, np.float32).reshape(-1)
    n = v.shape[0] // 128
    return np.ascontiguousarray(v.reshape(n, 128).T)


def pack_vecs(named):
    cols = {}
    arrs = []
    off = 0
    for k, v in named:
        a = pack_vec(v)
        cols[k] = (off, a.shape[1])
        arrs.append(a)
        off += a.shape[1]
    return np.ascontiguousarray(np.concatenate(arrs, axis=1)), cols


class Ring:
    def __init__(self, tiles):
        self.tiles = tiles
        self.i = 0

    def next(self):
        t = self.tiles[self.i % len(self.tiles)]
        self.i += 1
        return t


def new_prog():
    nc = bass.Bass("TRN2", target_bir_lowering=False)
    return nc, Prog(nc)


def emit_mod(P, nc, cT_d, adaw_d, nvec, vecs, adab_col, scratch_h, ps_bank):
    cT = P.tile([128, 8], F32, "cT")
    P.dma("sp", cT, cT.ap[:], None, cT_d)
    sc_bf = P.tile([128, 8], BF16, "sc_bf")
    P.op("act", lambda e: e.activation(sc_bf.ap[:], cT.ap[:], AF.Silu), [cT], [sc_bf])
    ncols = nvec * D
    aw = T(scratch_h, "adaw_sb")
    src = adaw_d.rearrange("(kc p) n -> p kc n", p=128)
    for v in range(nvec):
        P.dma("pool", aw, aw.ap[:, :, v * D:(v + 1) * D], None, src[:, :, v * D:(v + 1) * D])
    noc = nvec * 8
    for oc in range(noc):
        for kc in range(8):
            P.mm(ps_bank, ps_bank.ap[:, oc:oc + 1], aw, aw.ap[:, kc, oc * 128:(oc + 1) * 128],
                 sc_bf, sc_bf.ap[:, kc:kc + 1], start=(kc == 0), stop=(kc == 7))
    modv = P.tile([128, noc], F32, "modv")
    P.op("dve", lambda e: e.tensor_tensor(modv.ap[:], ps_bank.ap[:, 0:noc], vecs.ap[:, adab_col:adab_col + noc], ALU.add),
         [ps_bank, vecs], [modv])
    return modv


def emit_norm(P, x_t, x_ap, N, gm_t, gm_ap, sh_t, sh_ap, out_t, out_ap, ones_bf, ps_ss, sq_t, rt_t, tmp_t, eps_t, out_extra=()):
    for j in range(8):
        P.op("act", (lambda j: lambda e: e.activation(sq_t.ap[:, j, 0:N], x_ap[:, j, :], AF.Square))(j), [x_t], [sq_t])
    for j in range(8):
        P.mm(ps_ss, ps_ss.ap[:, 0:N], ones_bf, ones_bf.ap[:], sq_t, sq_t.ap[:, j, 0:N], start=(j == 0), stop=(j == 7))
    P.op("act", lambda e: e.activation(rt_t.ap[:, 0:N], ps_ss.ap[:, 0:N], AF.Sqrt, bias=eps_t.ap[:, 0:1], scale=1.0 / D),
         [ps_ss, eps_t], [rt_t])
    P.op("dve", lambda e: e.reciprocal(rt_t.ap[:, 0:N], rt_t.ap[:, 0:N]), [rt_t], [rt_t])
    for j in range(8):
        P.op("dve", (lambda j: lambda e: e.scalar_tensor_tensor(tmp_t.ap[:, j, 0:N], x_ap[:, j, :], gm_ap[:, j:j + 1],
                                                                rt_t.ap[:, 0:N], ALU.mult, ALU.mult))(j),
             [x_t, gm_t, rt_t], [tmp_t])
        if sh_t is not None:
            P.op("act", (lambda j: lambda e: e.activation(out_ap[:, j, :], tmp_t.ap[:, j, 0:N], AF.Identity,
                                                          bias=sh_ap[:, j:j + 1], scale=1.0))(j),
                 [tmp_t, sh_t], [out_t] + list(out_extra))
        else:
            P.op("act", (lambda j: lambda e: e.copy(out_ap[:, j, :], tmp_t.ap[:, j, 0:N]))(j), [tmp_t], [out_t])


DEBUG = False


def emit_ffn(P, nc, pbank, xT_v, yT_v, ntok, moe, pre, cT_d):
    E = NE if moe else 1
    ST = 1024
    NV = 8 + 24 + (8 if moe else 0)
    vecs_d = nc.dram_tensor(pre + "vecs", [128, NV], F32, kind="ExternalInput").ap()
    adaw_d = nc.dram_tensor(pre + "adaw", [D, 3 * D], F32, kind="ExternalInput").ap()
    wgu_d = nc.dram_tensor(pre + "wgu", [E, D, 2 * FF], F32, kind="ExternalInput").ap()
    wd_d = nc.dram_tensor(pre + "wd", [E, FF, D], F32, kind="ExternalInput").ap()
    if moe:
        ident_d = nc.dram_tensor(pre + "ident", [128, 128], F32, kind="ExternalInput").ap()
        wr_d = nc.dram_tensor(pre + "wr", [D, NE], F32, kind="ExternalInput").ap()
        br_d = nc.dram_tensor(pre + "br", [128, NE], F32, kind="ExternalInput").ap()

    vecs = P.tile([128, NV], F32, "vecs")
    P.dma("sp", vecs, vecs.ap[:], None, vecs_d)
    ones_bf = P.tile([128, 128], BF16, "ones")
    P.op("dve", lambda e: e.memset(ones_bf.ap[:], 1.0), [], [ones_bf])
    eps_t = P.tile([128, 1], F32, "eps")
    P.op("dve", lambda e: e.memset(eps_t.ap[:], NORM_EPS), [], [eps_t])
    act_h = P.sb([128, FC, ST], BF16, "act")
    act = [T(act_h[:, fc, :], f"act{fc}") for fc in range(FC)]
    banks = [T(pbank[i], f"bank{i}") for i in range(8)]
    aw_view = act_h[:, 0:24, :].rearrange("p a b -> p (a b)").rearrange("p (k n) -> p k n", k=8)
    modv = emit_mod(P, nc, cT_d, adaw_d, 3, vecs, 8, aw_view, banks[7])
    gm = P.tile([128, 8], F32, "gm")
    P.op("dve", lambda e: e.scalar_tensor_tensor(gm.ap[:], modv.ap[:, 8:16], 1.0, vecs.ap[:, 0:8], ALU.add, ALU.mult),
         [modv, vecs], [gm])
    g2p = P.tile([128, 8], F32, "g2p")
    P.op("dve", lambda e: e.tensor_scalar(g2p.ap[:], modv.ap[:, 16:24], 1.0, None, ALU.add), [modv], [g2p])

    x_h = P.sb([128, 8, ST], F32, "x")
    xall = T(x_h, "xall")
    h_bf = P.tile([128, 8, ST], BF16, "h_bf")
    sq_t = P.tile([128, 8, 512], BF16, "sq")
    rt_t = P.tile([128, 512], F32, "rt")
    tmp_t = P.tile([128, 8, 512], F32, "tmp")
    wgu_ring = Ring([P.tile([128, 8, 2, 256], BF16, f"wgu{i}") for i in range(2)])
    wd_ring = Ring([P.tile([128, FC, 128], BF16, f"wd{i}") for i in range(2)])
    sg_ring = Ring([P.tile([128, 512], F32, f"sg{i}") for i in range(2)])
    psg_ring = Ring([banks[0], banks[1]])
    psu_ring = Ring([banks[2], banks[3]])
    pso_ring = Ring([banks[4], banks[5]])
    ps_ss = banks[6]
    if moe:
        ident = P.tile([128, 128], F32, "ident")
        P.dma("sp", ident, ident.ap[:], None, ident_d)
        wr = P.tile([128, 8, NE], F32, "wr")
        P.dma("sp", wr, wr.ap[:], None, wr_d.rearrange("(kc p) e -> p kc e", p=128))
        br = P.tile([128, NE], F32, "br")
        P.dma("sp", br, br.ap[:], None, br_d)
        comb = P.tile([128, ST // 128, NE], F32, "comb")
        lg = P.tile([128, NE], F32, "lg")
        m1 = P.tile([128, 1], F32, "m1")
        m2 = P.tile([128, 1], F32, "m2")
        eq1 = P.tile([128, NE], F32, "eq1")
        lg2 = P.tile([128, NE], F32, "lg2")
        eq2 = P.tile([128, NE], F32, "eq2")
        p1 = P.tile([128, 1], F32, "p1")
        p2 = P.tile([128, 1], F32, "p2")
        rep_ring = Ring([P.tile([128, 128], F32, f"rep{i}") for i in range(2)])
        cbc_ring = Ring([P.tile([128, ST], F32, f"cbc{i}") for i in range(2)])
        tmp2_ring = Ring([P.tile([128, 512], F32, f"tmp2{i}") for i in range(2)])
        ps_misc = banks[7]

    def loads_wgu(e, g2):
        t = wgu_ring.next()
        src = wgu_d[e].rearrange("(kc p) n -> p kc n", p=128)
        P.dma("pool", t, t.ap[:, :, 0, :], None, src[:, :, g2 * 256:(g2 + 1) * 256])
        P.dma("pool", t, t.ap[:, :, 1, :], None, src[:, :, FF + g2 * 256:FF + (g2 + 1) * 256])
        return t

    def load_wd(e, d):
        t = wd_ring.next()
        src = wd_d[e].rearrange("(fc p) n -> p fc n", p=128)
        P.dma("pool", t, t.ap[:], None, src[:, :, d * 128:(d + 1) * 128])
        return t

    if moe:
        hf = T(act_h[:, 0:16, :].rearrange("p a b -> p (a b)").bitcast(F32).rearrange("p (j n) -> p j n", j=8), "hf")
    for st in range(ntok // ST):
        c0 = st * ST
        P.dma("sp", xall, x_h[:], None, xT_v[:, :, c0:c0 + ST])
        for tt in range(ST // 512):
            cs = slice(tt * 512, (tt + 1) * 512)
            if not moe:
                emit_norm(P, xall, x_h[:, :, cs], 512, gm, gm.ap, modv, modv.ap[:, 0:8], h_bf, h_bf.ap[:, :, cs],
                          ones_bf, ps_ss, sq_t, rt_t, tmp_t, eps_t)
            else:
                emit_norm(P, xall, x_h[:, :, cs], 512, gm, gm.ap, modv, modv.ap[:, 0:8], hf, hf.ap[:, :, 0:512],
                          ones_bf, ps_ss, sq_t, rt_t, tmp_t, eps_t, out_extra=act[0:16])
                for j in range(8):
                    P.I("dve", "tensor_copy", [hf], [h_bf], h_bf.ap[:, j, cs], hf.ap[:, j, 0:512])
                for b in range(4):
                    blk = tt * 4 + b
                    for kc in range(8):
                        P.mm(ps_misc, ps_misc.ap[:, 0:NE], hf, hf.ap[:, kc, b * 128:(b + 1) * 128], wr, wr.ap[:, kc, :],
                             start=(kc == 0), stop=(kc == 7))
                    P.op("dve", lambda e: e.tensor_tensor(lg.ap[:], ps_misc.ap[:, 0:NE], br.ap[:], ALU.add), [ps_misc, br], [lg])
                    P.op("dve", lambda e: e.tensor_reduce(m1.ap[:], lg.ap[:], AX.X, ALU.max), [lg], [m1])
                    P.op("dve", lambda e: e.tensor_scalar(eq1.ap[:], lg.ap[:], m1.ap[:, 0:1], None, ALU.is_equal), [lg, m1], [eq1])
                    P.op("dve", lambda e: e.scalar_tensor_tensor(lg2.ap[:], eq1.ap[:], -1e30, lg.ap[:], ALU.mult, ALU.add),
                         [eq1, lg], [lg2])
                    P.op("dve", lambda e: e.tensor_reduce(m2.ap[:], lg2.ap[:], AX.X, ALU.max), [lg2], [m2])
                    P.op("dve", lambda e: e.tensor_scalar(eq2.ap[:], lg2.ap[:], m2.ap[:, 0:1], None, ALU.is_equal), [lg2, m2], [eq2])
                    P.op("dve", lambda e: e.tensor_tensor(p2.ap[:], m1.ap[:], m2.ap[:], ALU.subtract), [m1, m2], [p2])
                    P.op("act", lambda e: e.activation(p1.ap[:], p2.ap[:], AF.Sigmoid), [p2], [p1])
                    P.op("dve", lambda e: e.tensor_scalar(p2.ap[:], p1.ap[:], -1.0, 1.0, ALU.mult, ALU.add), [p1], [p2])
                    P.op("dve", lambda e: e.tensor_scalar(eq1.ap[:], eq1.ap[:], p1.ap[:, 0:1], None, ALU.mult), [eq1, p1], [eq1])
                    P.op("dve", (lambda blk: lambda e: e.scalar_tensor_tensor(comb.ap[:, blk, :], eq2.ap[:], p2.ap[:, 0:1], eq1.ap[:],
                                                                             ALU.mult, ALU.add))(blk), [eq2, p2, eq1], [comb])
        if moe and DEBUG and st == 0:
            dbg_comb = nc.dram_tensor("dbg_comb", [128, ST // 128, NE], F32, kind="ExternalOutput").ap()
            P.dma("sp", None, dbg_comb, comb, comb.ap[:])
            dbg_h = nc.dram_tensor("dbg_h", [128, 8, ST], BF16, kind="ExternalOutput").ap()
            P.dma("sp", None, dbg_h, h_bf, h_bf.ap[:])
        for e_i in range(E):
            if moe:
                cbc = cbc_ring.next()
                for blk in range(ST // 128):
                    rep = rep_ring.next()
                    P.op("dve", (lambda rep, blk, e_i: lambda e: e.tensor_copy(rep.ap[:], comb.ap[:, blk, e_i:e_i + 1].broadcast_to([128, 128])))(rep, blk, e_i),
                         [comb], [rep])
                    half = blk // 4
                    col = (blk % 4) * 128
                    P.mm(ps_misc, ps_misc.ap[:, col:col + 128], rep, rep.ap[:], ident, ident.ap[:])
                    if blk % 4 == 3:
                        P.op("act", (lambda cbc, half: lambda e: e.copy(cbc.ap[:, half * 512:(half + 1) * 512], ps_misc.ap[:]))(cbc, half),
                             [ps_misc], [cbc])
            if moe and DEBUG and st == 0 and e_i == 0:
                dbg_cbc = nc.dram_tensor("dbg_cbc", [128, ST], F32, kind="ExternalOutput").ap()
                P.dma("sp", None, dbg_cbc, cbc, cbc.ap[:])
            for g2 in range(FC // 2):
                wt = loads_wgu(e_i, g2)
                for f in range(2):
                    fc = g2 * 2 + f
                    for tt in range(ST // 512):
                        cs = slice(tt * 512, (tt + 1) * 512)
                        psg = psg_ring.next()
                        psu = psu_ring.next()
                        for kc in range(8):
                            P.mm(psg, psg.ap[:], wt, wt.ap[:, kc, 0, f * 128:(f + 1) * 128], h_bf, h_bf.ap[:, kc, cs],
                                 start=(kc == 0), stop=(kc == 7))
                        for kc in range(8):
                            P.mm(psu, psu.ap[:], wt, wt.ap[:, kc, 1, f * 128:(f + 1) * 128], h_bf, h_bf.ap[:, kc, cs],
                                 start=(kc == 0), stop=(kc == 7))
                        sg = sg_ring.next()
                        P.op("act", (lambda sg, psg: lambda e: e.activation(sg.ap[:], psg.ap[:], AF.Silu))(sg, psg), [psg], [sg])
                        if not moe:
                            P.op("dve", (lambda sg, psu, fc, cs: lambda e: e.tensor_tensor(act_h[:, fc, cs], sg.ap[:], psu.ap[:], ALU.mult))(sg, psu, fc, cs),
                                 [sg, psu], [act[fc]])
                        else:
                            t2 = tmp2_ring.next()
                            P.op("dve", (lambda sg, psu, t2: lambda e: e.tensor_tensor(t2.ap[:], sg.ap[:], psu.ap[:], ALU.mult))(sg, psu, t2),
                                 [sg, psu], [t2])
                            P.op("dve", (lambda t2, cbc, fc, cs: lambda e: e.tensor_tensor(act_h[:, fc, cs], t2.ap[:], cbc.ap[:, cs], ALU.mult))(t2, cbc, fc, cs),
                                 [t2, cbc], [act[fc]])
            for d in range(8):
                wt = load_wd(e_i, d)
                for tt in range(ST // 512):
                    cs = slice(tt * 512, (tt + 1) * 512)
                    pso = pso_ring.next()
                    for fc in range(FC):
                        P.mm(pso, pso.ap[:], wt, wt.ap[:, fc, :], act[fc], act_h[:, fc, cs], start=(fc == 0), stop=(fc == FC - 1))
                    P.op("dve", (lambda pso, d, cs: lambda e: e.scalar_tensor_tensor(x_h[:, d, cs], pso.ap[:], g2p.ap[:, d:d + 1], x_h[:, d, cs],
                                                                                    ALU.mult, ALU.add))(pso, d, cs),
                         [pso, g2p, xall], [xall])
        if moe:
            fg_col = 32
            for tt in range(ST // 512):
                cs = slice(tt * 512, (tt + 1) * 512)
                emit_final(P, xall, x_h[:, :, cs], vecs, fg_col, ones_bf, ps_ss, sq_t, rt_t, tmp_t, eps_t)
                P.dma("sp", None, yT_v[:, :, c0 + tt * 512:c0 + (tt + 1) * 512], tmp_t, tmp_t.ap[:])
        else:
            P.dma("sp", None, yT_v[:, :, c0:c0 + ST], xall, x_h[:])


def emit_final(P, x_t, x_ap, vecs, fg_col, ones_bf, ps_ss, sq_t, rt_t, tmp_t, eps_t):
    N = 512
    for j in range(8):
        P.op("act", (lambda j: lambda e: e.activation(sq_t.ap[:, j, :], x_ap[:, j, :], AF.Square))(j), [x_t], [sq_t])
    for j in range(8):
        P.mm(ps_ss, ps_ss.ap[:], ones_bf, ones_bf.ap[:], sq_t, sq_t.ap[:, j, :], start=(j == 0), stop=(j == 7))
    P.op("act", lambda e: e.activation(rt_t.ap[:], ps_ss.ap[:], AF.Sqrt, bias=eps_t.ap[:, 0:1], scale=1.0 / D), [ps_ss, eps_t], [rt_t])
    P.op("dve", lambda e: e.reciprocal(rt_t.ap[:], rt_t.ap[:]), [rt_t], [rt_t])
    for j in range(8):
        P.op("dve", (lambda j: lambda e: e.scalar_tensor_tensor(tmp_t.ap[:, j, :], x_ap[:, j, :], vecs.ap[:, fg_col + j:fg_col + j + 1],
                                                                rt_t.ap[:], ALU.mult, ALU.mult))(j),
             [x_t, vecs, rt_t], [tmp_t])


LRU_C = 8.0
LRU_VEC_NAMES = ["ng", "adab", "cw0", "cw1", "cw2", "cw3", "cb", "bgr", "bgi", "lam", "flag"]


def emit_lru(P, nc, pbank, xT_v, yT_v, pre, cT_d):
    NT = 256
    VT = 2 * NTOK
    NV = 8 + 24 + 32 + 8 + 8 + 8 + 8 + 1
    vecs_d = nc.dram_tensor(pre + "vecs", [128, NV], F32, kind="ExternalInput").ap()
    adaw_d = nc.dram_tensor(pre + "adaw", [D, 3 * D], F32, kind="ExternalInput").ap()
    win_d = nc.dram_tensor(pre + "win", [D, 2 * D], F32, kind="ExternalInput").ap()
    wg_d = nc.dram_tensor(pre + "wg", [4, 256, 512], F32, kind="ExternalInput").ap()
    wout_d = nc.dram_tensor(pre + "wout", [D, D], F32, kind="ExternalInput").ap()
    C_NG, C_AB, C_CW, C_CB, C_BGR, C_BGI, C_LAM, C_FLAG = 0, 8, 32, 64, 72, 80, 88, 96

    vecs = P.tile([128, NV], F32, "vecs")
    P.dma("sp", vecs, vecs.ap[:], None, vecs_d)
    ones_bf = P.tile([128, 128], BF16, "ones")
    P.I("dve", "memset", [], [ones_bf], ones_bf.ap[:], 1.0)
    eps_t = P.tile([128, 1], F32, "eps")
    P.I("dve", "memset", [], [eps_t], eps_t.ap[:], NORM_EPS)
    banks = [T(pbank[i], f"bank{i}") for i in range(8)]
    scratch = P.sb([128, 8, 3 * D], BF16, "adaw_sb")
    modv = emit_mod(P, nc, cT_d, adaw_d, 3, vecs, C_AB, scratch, banks[7])
    gm = P.tile([128, 8], F32, "gm")
    P.I("dve", "scalar_tensor_tensor", [modv, vecs], [gm], gm.ap[:], modv.ap[:, 8:16], 1.0, vecs.ap[:, C_NG:C_NG + 8], ALU.add, ALU.mult)
    g1p = P.tile([128, 8], F32, "g1p")
    P.I("dve", "tensor_scalar", [modv], [g1p], g1p.ap[:], modv.ap[:, 16:24], 1.0, None, ALU.add)
    cj = P.tile([128, 8], F32, "cj")
    P.I("act", "activation", [vecs], [cj], cj.ap[:], vecs.ap[:, C_LAM:C_LAM + 8], AF.Exp, scale=-1.0)
    P.I("dve", "tensor_scalar", [cj], [cj], cj.ap[:], cj.ap[:], 1.0, None, ALU.add)
    P.I("act", "activation", [cj], [cj], cj.ap[:], cj.ap[:], AF.Ln)
    P.I("dve", "tensor_scalar", [cj], [cj], cj.ap[:], cj.ap[:], -LRU_C, None, ALU.mult)

    win = T(scratch[:, :, 0:2 * D], "win")
    wout = T(scratch[:, :, 2 * D:3 * D], "wout")
    wg = P.tile([128, 4, 2, 512], BF16, "wg")
    dummy = P.tile([128, 1], F32, "dummy")
    P.I("pool", "tensor_copy", [modv], [win, wout, dummy], dummy.ap[:], modv.ap[:, 0:1])
    src = win_d.rearrange("(kc p) n -> p kc n", p=128)
    for v in range(2):
        P.dma("pool", win, scratch[:, :, v * D:(v + 1) * D], None, src[:, :, v * D:(v + 1) * D])
    P.dma("pool", wout, scratch[:, :, 2 * D:3 * D], None, wout_d.rearrange("(kc p) n -> p kc n", p=128))
    for n in range(4):
        P.dma("pool", wg, wg.ap[:, n, :, :], None, wg_d[n].rearrange("(kc p) n -> p kc n", p=128))

    x_t = P.tile([128, 8, NT], F32, "x")
    h_bf = P.tile([128, 8, NT], BF16, "h_bf")
    sq_t = P.tile([128, 8, NT], BF16, "sq")
    rt_t = P.tile([128, NT], F32, "rt")
    tmp_t = P.tile([128, 8, NT], F32, "tmp")
    xb_sb = P.tile([128, 8, NT + 3], F32, "xb_sb")
    gate = P.tile([128, 8, NT], F32, "gate")
    xbc = P.tile([128, 8, NT], F32, "xbc")
    xbc_bf = P.tile([128, 8, NT], BF16, "xbc_bf")
    hg = P.tile([128, 8, NT], BF16, "hg")
    hprev = P.tile([128, 8], F32, "hprev")
    xo = P.tile([128, 8, NT], F32, "xo")
    w_ring = Ring([P.tile([128, NT], F32, f"wk{i}") for i in range(10)])
    xb_ring = Ring([banks[0], banks[1]])
    gb_ring = Ring([banks[2], banks[3]])
    r_ring = Ring([banks[4]])
    i_ring = Ring([banks[5]])
    ps_ss = banks[6]
    o_ring = Ring([banks[6], banks[7]])

    P.I("dve", "memset", [], [xb_sb], xb_sb.ap[:, :, NT:NT + 3], 0.0)
    P.I("dve", "memset", [], [hprev], hprev.ap[:], 0.0)
    fl = vecs.ap[:, C_FLAG:C_FLAG + 1]

    for tt in range(VT // NT):
        c0 = tt * NT
        own = tt >= (NTOK // NT)
        P.dma("sp", x_t, x_t.ap[:], None, xT_v[:, :, c0:c0 + NT])
        emit_norm(P, x_t, x_t.ap[:], NT, gm, gm.ap, modv, modv.ap[:, 0:8], h_bf, h_bf.ap[:], ones_bf, ps_ss, sq_t, rt_t, tmp_t, eps_t)
        if tt == NTOK // NT:
            P.I("dve", "tensor_scalar", [xb_sb, vecs], [xb_sb], xb_sb.ap[:, :, 0:3], xb_sb.ap[:, :, NT:NT + 3], fl, None, ALU.mult)
            P.I("dve", "tensor_scalar", [hprev, vecs], [hprev], hprev.ap[:], hprev.ap[:], fl, None, ALU.mult)
        else:
            P.I("dve", "tensor_copy", [xb_sb], [xb_sb], xb_sb.ap[:, :, 0:3], xb_sb.ap[:, :, NT:NT + 3])
        for j in range(8):
            xb_ps = xb_ring.next()
            gb_ps = gb_ring.next() if own else None
            for kc in range(8):
                P.mm(xb_ps, xb_ps.ap[:, 0:NT], win, scratch[:, kc, j * 128:(j + 1) * 128], h_bf, h_bf.ap[:, kc, :], start=(kc == 0), stop=(kc == 7))
            for kc in range(8 if own else 0):
                P.mm(gb_ps, gb_ps.ap[:, 0:NT], win, scratch[:, kc, D + j * 128:D + (j + 1) * 128], h_bf, h_bf.ap[:, kc, :], start=(kc == 0), stop=(kc == 7))
            P.I("act", "copy", [xb_ps], [xb_sb], xb_sb.ap[:, j, 3:NT + 3], xb_ps.ap[:, 0:NT])
            if own:
                t1 = w_ring.next()
                P.I("act", "activation", [gb_ps], [t1], t1.ap[:], gb_ps.ap[:, 0:NT], AF.Square)
                P.I("dve", "tensor_scalar", [t1], [t1], t1.ap[:], t1.ap[:], 0.044715, 1.0, ALU.mult, ALU.add)
                P.I("dve", "tensor_tensor", [t1, gb_ps], [t1], t1.ap[:], t1.ap[:], gb_ps.ap[:, 0:NT], ALU.mult)
                P.I("act", "activation", [t1], [t1], t1.ap[:], t1.ap[:], AF.Sigmoid, scale=1.5957691216057308)
                P.I("dve", "tensor_tensor", [t1, gb_ps], [gate], gate.ap[:, j, :], t1.ap[:], gb_ps.ap[:, 0:NT], ALU.mult)
            P.I("act", "activation", [xb_sb, vecs], [xbc], xbc.ap[:, j, :], xb_sb.ap[:, j, 3:NT + 3], AF.Identity,
                bias=vecs.ap[:, C_CB + j:C_CB + j + 1], scale=vecs.ap[:, C_CW + 24 + j:C_CW + 24 + j + 1])
            for i in (2, 1, 0):
                P.I("dve", "scalar_tensor_tensor", [xb_sb, vecs, xbc], [xbc], xbc.ap[:, j, :], xb_sb.ap[:, j, i:NT + i],
                    vecs.ap[:, C_CW + i * 8 + j:C_CW + i * 8 + j + 1], xbc.ap[:, j, :], ALU.mult, ALU.add)
            P.I("act", "copy", [xbc], [xbc_bf], xbc_bf.ap[:, j, :], xbc.ap[:, j, :])
        for n in range(4):
            for oc in range(2):
                j = 2 * n + oc
                r_ps = r_ring.next()
                i_ps = i_ring.next()
                for kc in range(2):
                    P.mm(r_ps, r_ps.ap[:, 0:NT], wg, wg.ap[:, n, kc, oc * 128:(oc + 1) * 128], xbc_bf, xbc_bf.ap[:, 2 * n + kc, :],
                         start=(kc == 0), stop=(kc == 1))
                for kc in range(2):
                    P.mm(i_ps, i_ps.ap[:, 0:NT], wg, wg.ap[:, n, kc, 256 + oc * 128:256 + (oc + 1) * 128], xbc_bf, xbc_bf.ap[:, 2 * n + kc, :],
                         start=(kc == 0), stop=(kc == 1))
                a_t = w_ring.next()
                b_t = w_ring.next()
                i_t = w_ring.next()
                P.I("act", "activation", [r_ps, vecs], [a_t], a_t.ap[:], r_ps.ap[:, 0:NT], AF.Sigmoid, bias=vecs.ap[:, C_BGR + j:C_BGR + j + 1])
                P.I("act", "activation", [i_ps, vecs], [i_t], i_t.ap[:], i_ps.ap[:, 0:NT], AF.Sigmoid, bias=vecs.ap[:, C_BGI + j:C_BGI + j + 1])
                P.I("act", "activation", [a_t, cj], [a_t], a_t.ap[:], a_t.ap[:], AF.Exp, scale=cj.ap[:, j:j + 1])
                P.I("dve", "tensor_tensor", [a_t], [b_t], b_t.ap[:], a_t.ap[:], a_t.ap[:], ALU.mult)
                P.I("dve", "tensor_scalar", [b_t], [b_t], b_t.ap[:], b_t.ap[:], -1.0, 1.0, ALU.mult, ALU.add)
                P.I("act", "activation", [b_t], [b_t], b_t.ap[:], b_t.ap[:], AF.Sqrt)
                P.I("dve", "tensor_tensor", [i_t, xbc], [i_t], i_t.ap[:], i_t.ap[:], xbc.ap[:, j, :], ALU.mult)
                P.I("dve", "tensor_tensor", [b_t, i_t], [b_t], b_t.ap[:], b_t.ap[:], i_t.ap[:], ALU.mult)
                hs = w_ring.next()
                P.I("dve", "tensor_tensor_scan", [a_t, b_t, hprev], [hs], hs.ap[:], a_t.ap[:], b_t.ap[:], hprev.ap[:, j:j + 1], ALU.mult, ALU.add)
                P.I("dve", "tensor_copy", [hs], [hprev], hprev.ap[:, j:j + 1], hs.ap[:, NT - 1:NT])
                if own:
                    P.I("dve", "tensor_tensor", [hs, gate], [hg], hg.ap[:, j, :], hs.ap[:], gate.ap[:, j, :], ALU.mult)
        for jo in range(8 if own else 0):
            ps = o_ring.next()
            for kc in range(8):
                P.mm(ps, ps.ap[:, 0:NT], wout, scratch[:, kc, 2 * D + jo * 128:2 * D + (jo + 1) * 128], hg, hg.ap[:, kc, :], start=(kc == 0), stop=(kc == 7))
            P.I("dve", "scalar_tensor_tensor", [ps, g1p, x_t], [xo], xo.ap[:, jo, :], ps.ap[:, 0:NT], g1p.ap[:, jo:jo + 1], x_t.ap[:, jo, :], ALU.mult, ALU.add)
        if own:
            P.dma("sp", None, yT_v[:, :, c0 - NTOK:c0 - NTOK + NT], xo, xo.ap[:])


GN_EPS = 64e-5
EXPM05 = 0.6065306597126334


def rwkv_consts():
    p = np.arange(128)[:, None]
    q = np.arange(64)[None, :]
    ms = ((p % 64) < q).astype(np.float32)
    mi = ((p % 64) <= q).astype(np.float32)
    mt = (q < (p % 64)).astype(np.float32)
    iq = ((p % 64) == q).astype(np.float32)
    pp = np.arange(128)[None, :]
    bo = ((p // 64) == (pp // 64)).astype(np.float32)
    idn = np.eye(128, dtype=np.float32)
    t = np.arange(256)[None, :]
    mc = np.broadcast_to(((t % 64) != 0).astype(np.float32), (128, 256))
    return np.ascontiguousarray(np.concatenate([ms, mi, mt, iq, bo, bo / 64.0, idn, mc], axis=1))


RW_VECS = ["ng", "adab", "mu", "w0", "a0", "kk", "ka", "rk", "gnw", "gnb", "flag"]


class StopBuild(Exception):
    pass


RW_STOP = None
RW_DUMPS = []


def emit_rwkv(P, nc, pbank, xT_v, yT_v, pre, cT_d):
    def ckpt(name, dumps):
        return
    VT = 2 * NTOK
    NT = 256
    C = NT // 64
    NV = 8 + 24 + 48 + 8 * 7 + 1
    vecs_d = nc.dram_tensor(pre + "vecs", [128, NV], F32, kind="ExternalInput").ap()
    NCON = 64 * 4 + 128 * 3 + 256
    con_d = nc.dram_tensor(pre + "consts", [128, NCON], F32, kind="ExternalInput").ap()
    adaw_d = nc.dram_tensor(pre + "adaw", [D, 3 * D], F32, kind="ExternalInput").ap()
    wrkv_d = nc.dram_tensor(pre + "wrkv", [3, D, D], F32, kind="ExternalInput").ap()
    wo_d = nc.dram_tensor(pre + "wo", [D, D], F32, kind="ExternalInput").ap()
    w1_d = nc.dram_tensor(pre + "w1", [D, 64], F32, kind="ExternalInput").ap()
    w2_d = nc.dram_tensor(pre + "w2", [64, D], F32, kind="ExternalInput").ap()
    a1_d = nc.dram_tensor(pre + "a1", [D, 64], F32, kind="ExternalInput").ap()
    a2_d = nc.dram_tensor(pre + "a2", [64, D], F32, kind="ExternalInput").ap()
    g1_d = nc.dram_tensor(pre + "g1", [D, 160], F32, kind="ExternalInput").ap()
    g2_d = nc.dram_tensor(pre + "g2", [160, D], F32, kind="ExternalInput").ap()
    C_NG, C_AB, C_MU, C_W0, C_A0, C_KK, C_KA, C_RK, C_GNW, C_GNB, C_FLAG = 0, 8, 32, 80, 88, 96, 104, 112, 120, 128, 136

    vecs = P.tile([128, NV], F32, "vecs")
    P.dma("sp", vecs, vecs.ap[:], None, vecs_d)
    con = P.tile([128, NCON], F32, "con")
    P.dma("sp", con, con.ap[:], None, con_d)
    MS, MI, MT, IQ = (con.ap[:, 64 * i:64 * (i + 1)] for i in range(4))
    BO = con.ap[:, 256:384]
    BO64 = con.ap[:, 384:512]
    IDN = con.ap[:, 512:640]
    MC = con.ap[:, 640:896]
    conb = P.tile([128, 384], BF16, "conb")
    P.I("dve", "tensor_copy", [con], [conb], conb.ap[:, 0:128], BO)
    P.I("dve", "tensor_copy", [con], [conb], conb.ap[:, 128:256], IDN)
    P.I("dve", "memset", [], [conb], conb.ap[:, 256:384], 1.0)
    BO_bf = conb.ap[:, 0:128]
    ID_bf = conb.ap[:, 128:256]
    ones_bf = T(conb.ap[:, 256:384], "ones_v")
    ones_bf.w = conb.w
    eps_t = P.tile([128, 3], F32, "eps")
    P.I("dve", "memset", [], [eps_t], eps_t.ap[:, 0:1], NORM_EPS)
    P.I("dve", "memset", [], [eps_t], eps_t.ap[:, 1:2], 1e-24)
    P.I("dve", "memset", [], [eps_t], eps_t.ap[:, 2:3], GN_EPS)


    PB = [T(pbank[i], f"PB{i}") for i in range(8)]

    def reg(i, name):
        return TV(PB[i // 2], pbank[i // 2][:, 256 * (i % 2):256 * (i % 2 + 1)], name)
    R_r, R_k, R_v, R_zw, R_za, R_g, R_st, R_st2 = (reg(i, f"R{i}") for i in range(8))
    B6 = TV(PB[6], pbank[6][:, :], "B6")
    B7 = TV(PB[7], pbank[7][:, :], "B7")

    scratch = P.sb([128, 8, 3 * D], BF16, "wrkv_sb")
    modv = emit_mod(P, nc, cT_d, adaw_d, 3, vecs, C_AB, scratch, R_g)
    gm = P.tile([128, 8], F32, "gm")
    P.I("dve", "scalar_tensor_tensor", [modv, vecs], [gm], gm.ap[:], modv.ap[:, 8:16], 1.0, vecs.ap[:, C_NG:C_NG + 8], ALU.add, ALU.mult)
    g1p = P.tile([128, 8], F32, "g1p")
    P.I("dve", "tensor_scalar", [modv], [g1p], g1p.ap[:], modv.ap[:, 16:24], 1.0, None, ALU.add)
    omka = P.tile([128, 8], F32, "omka")
    P.I("dve", "tensor_scalar", [vecs], [omka], omka.ap[:], vecs.ap[:, C_KA:C_KA + 8], -1.0, 1.0, ALU.mult, ALU.add)

    wrkv = T(scratch, "wrkv")
    dummy = P.tile([128, 1], F32, "dummy")
    P.I("pool", "tensor_copy", [modv], [wrkv, dummy], dummy.ap[:], modv.ap[:, 0:1])
    for p_ in range(3):
        P.dma("pool", wrkv, scratch[:, :, p_ * D:(p_ + 1) * D], None, wrkv_d[p_].rearrange("(kc p) n -> p kc n", p=128))
    wo = P.tile([128, 8, D], BF16, "wo")
    P.dma("pool", wo, wo.ap[:], None, wo_d.rearrange("(kc p) n -> p kc n", p=128))
    w1 = P.tile([128, 8, 64], BF16, "w1")
    P.dma("pool", w1, w1.ap[:], None, w1_d.rearrange("(kc p) n -> p kc n", p=128))
    a1 = P.tile([128, 8, 64], BF16, "a1")
    P.dma("pool", a1, a1.ap[:], None, a1_d.rearrange("(kc p) n -> p kc n", p=128))
    g1 = P.tile([128, 8, 256], BF16, "g1")
    P.I("pool", "memset", [], [g1], g1.ap[:], 0.0)
    P.dma("pool", g1, g1.ap[:, :, 0:160], None, g1_d.rearrange("(kc p) n -> p kc n", p=128))
    w2 = P.tile([64, D], BF16, "w2")
    P.dma("pool", w2, w2.ap[:], None, w2_d)
    a2 = P.tile([64, D], BF16, "a2")
    P.dma("pool", a2, a2.ap[:], None, a2_d)
    g2a = P.tile([128, D], BF16, "g2a")
    P.dma("pool", g2a, g2a.ap[:], None, g2_d[0:128, :])
    g2b = P.tile([128, D], BF16, "g2b")
    P.I("pool", "memset", [], [g2b], g2b.ap[:], 0.0)
    P.dma("pool", g2b, g2b.ap[0:32, :], None, g2_d[128:160, :])

    ckpt("setup", [("gm", gm, gm.ap[:], [128, 8], F32)])
    x_t = P.tile([128, 8, NT], F32, "x")
    h_t = P.tile([128, 8, NT + 1], F32, "h")
    sq_t = P.tile([128, 8, NT], BF16, "sq")
    rt_t = P.tile([128, NT], F32, "rt")
    tmp_t = P.tile([128, 8, NT], F32, "tmp")
    xm = [P.tile([128, 8, NT], BF16, f"xm{p_}") for p_ in range(6)]
    lw1 = P.tile([64, NT], BF16, "lw1")
    la1 = P.tile([64, NT], BF16, "la1")
    lg1a = P.tile([128, NT], BF16, "lg1a")
    lg1b = P.tile([128, NT], BF16, "lg1b")
    yo = P.tile([128, 8, NT], BF16, "yo")
    F = {}
    for nm in ["lw", "cum", "eg", "egm", "eneg", "a", "k", "kk", "rn", "ka", "fac", "km", "v", "bonus", "g", "Y", "yc", "sq2", "rs"]:
        F[nm] = P.tile([128, NT], F32, "f_" + nm)
    kk2 = P.tile([128, NT], BF16, "kk2")
    rkb = P.tile([128, NT], BF16, "rkb")
    AR = P.tile([128, C, 128], BF16, "AR")
    BKr = P.tile([128, C, 128], BF16, "BKr")
    bd_names = ["A_bd", "B_bd", "K_bd", "Bb_bd", "Kb_bd", "V_bd", "N_bd", "NT_bd", "T_bd", "AhT_bd", "M1T_bd", "P_bd", "W2T_bd", "TV_bd"]
    BD = {}
    for nm in bd_names:
        BD[nm] = P.tile([128, C, 128], BF16, nm)
        P.I("pool", "memset", [], [BD[nm]], BD[nm].ap[:], 0.0)
    Xt = P.tile([128, C, 128], BF16, "Xt")
    RH = {}
    for nm in ["TBr", "TKr", "TVr", "Nr", "NTr", "Tr", "Arb", "Ark", "Rhat", "M2"]:
        RH[nm] = P.tile([128, C, 64], BF16, nm)
    diagG = P.tile([128, C, 64], F32, "diagG")
    tmpP = P.tile([128, C, 64], BF16, "tmpP")
    tmpW = P.tile([128, C, 64], BF16, "tmpW")
    St_r = [P.tile([128, 64], BF16, f"St_r{j}") for j in range(8)]
    St_bd = [P.tile([128, 128], BF16, f"St_bd{j}") for j in range(8)]
    HS = (slice(0, 64), slice(64, 128))

    def halves(eng, method, reads, writes, out_fn, in_fns, *extra):
        for hh in range(2):
            hs = HS[hh]
            e_, m_ = eng, method
            if eng == "dve" and method == "tensor_copy" and hh == 1:
                e_, m_ = "act", "copy"
            P.I(e_, m_, reads, writes, out_fn(hs, hh), *[f(hs, hh) for f in in_fns], *extra)

    for j in range(8):
        P.I("pool", "memset", [], [St_bd[j]], St_bd[j].ap[:], 0.0)
        P.I("pool", "memset", [], [St_r[j]], St_r[j].ap[:], 0.0)
    P.I("dve", "memset", [], [h_t], h_t.ap[:, :, NT:NT + 1], 0.0)
    fl = vecs.ap[:, C_FLAG:C_FLAG + 1]

    v3 = lambda ap: ap.rearrange("p (c n) -> p c n", c=C)
    ckpt("init", [("hfull", h_t, h_t.ap[:], [128, 8, NT + 1], F32), ("Stbd0", St_bd[0], St_bd[0].ap[:], [128, 128], BF16)])

    for tt in range(VT // NT):
        c0 = tt * NT
        P.dma("sp", x_t, x_t.ap[:], None, xT_v[:, :, c0:c0 + NT])
        if tt == NTOK // NT:
            P.I("dve", "tensor_scalar", [h_t, vecs], [h_t], h_t.ap[:, :, 0:1], h_t.ap[:, :, NT:NT + 1], fl, None, ALU.mult)
            for j in range(8):
                P.I("dve", "tensor_scalar", [St_r[j], vecs], [St_r[j]], St_r[j].ap[:], St_r[j].ap[:], fl, None, ALU.mult)
                P.I("dve", "tensor_scalar", [St_bd[j], vecs], [St_bd[j]], St_bd[j].ap[:], St_bd[j].ap[:], fl, None, ALU.mult)
        else:
            P.I("dve", "tensor_copy", [h_t], [h_t], h_t.ap[:, :, 0:1], h_t.ap[:, :, NT:NT + 1])
        emit_norm(P, x_t, x_t.ap[:], NT, gm, gm.ap, modv, modv.ap[:, 0:8], h_t, h_t.ap[:, :, 1:NT + 1], ones_bf, R_st, sq_t, rt_t, tmp_t, eps_t)
        P.I("dve", "tensor_tensor", [h_t], [tmp_t], tmp_t.ap[:], h_t.ap[:, :, 0:NT], h_t.ap[:, :, 1:NT + 1], ALU.subtract)
        ckpt("norm", [("hfull", h_t, h_t.ap[:], [128, 8, NT + 1], F32), ("xx", tmp_t, tmp_t.ap[:], [128, 8, NT], F32)])
        for p_ in range(6):
            for j in range(8):
                eng = "dve"
                P.I(eng, "scalar_tensor_tensor", [tmp_t, vecs, h_t], [xm[p_]], xm[p_].ap[:, j, :], tmp_t.ap[:, j, :],
                    vecs.ap[:, C_MU + p_ * 8 + j:C_MU + p_ * 8 + j + 1], h_t.ap[:, j, 1:NT + 1], ALU.mult, ALU.add)
        ckpt("xm", [("xm0", xm[0], xm[0].ap[:], [128, 8, NT], BF16), ("xm5", xm[5], xm[5].ap[:], [128, 8, NT], BF16)])
        for kc in range(8):
            P.mm(R_r, pbank[0][0:64, 0:NT], w1, w1.ap[:, kc, :], xm[3], xm[3].ap[:, kc, :], start=(kc == 0), stop=(kc == 7))
        ckpt("l0", [("gm", gm, gm.ap[:], [128, 8], F32)])
        P.I("act", "activation", [R_r], [lw1], lw1.ap[:], pbank[0][0:64, 0:NT], AF.Tanh)
        ckpt("l1", [("gm", gm, gm.ap[:], [128, 8], F32)])
        for kc in range(8):
            P.mm(R_k, pbank[0][0:64, 256:256 + NT], a1, a1.ap[:, kc, :], xm[4], xm[4].ap[:, kc, :], start=(kc == 0), stop=(kc == 7))
        P.I("act", "copy", [R_k], [la1], la1.ap[:], pbank[0][0:64, 256:256 + NT])
        ckpt("l2", [("gm", gm, gm.ap[:], [128, 8], F32)])
        for kc in range(8):
            P.mm(R_v, R_v.ap[:], g1, g1.ap[:, kc, 0:128], xm[5], xm[5].ap[:, kc, :], start=(kc == 0), stop=(kc == 7))
        P.I("act", "activation", [R_v], [lg1a], lg1a.ap[:], R_v.ap[:], AF.Sigmoid)
        ckpt("l3", [("gm", gm, gm.ap[:], [128, 8], F32)])
        for kc in range(8):
            P.mm(R_zw, R_zw.ap[:], g1, g1.ap[:, kc, 128:256], xm[5], xm[5].ap[:, kc, :], start=(kc == 0), stop=(kc == 7))
        P.I("act", "activation", [R_zw], [lg1b], lg1b.ap[:], R_zw.ap[:], AF.Sigmoid)

        ckpt("lora", [("gm", gm, gm.ap[:], [128, 8], F32)])
        def b_proj(jn):
            jsn = slice(jn * 128, (jn + 1) * 128)
            for (R_, p_) in ((R_r, 0), (R_k, 1), (R_v, 2)):
                for kc in range(8):
                    P.mm(R_, R_.ap[:], wrkv, scratch[:, kc, p_ * D + jn * 128:p_ * D + (jn + 1) * 128], xm[p_], xm[p_].ap[:, kc, :],
                         start=(kc == 0), stop=(kc == 7))
            P.mm(R_zw, R_zw.ap[:], w2, w2.ap[:, jsn], lw1, lw1.ap[:])
            P.mm(R_za, R_za.ap[:], a2, a2.ap[:, jsn], la1, la1.ap[:])
            P.mm(R_g, R_g.ap[:], g2a, g2a.ap[:, jsn], lg1a, lg1a.ap[:], start=True, stop=False)
            P.mm(R_g, R_g.ap[:], g2b, g2b.ap[:, jsn], lg1b, lg1b.ap[:], start=False, stop=True)

        for j in range(8):
            js = slice(j * 128, (j + 1) * 128)
            vj = lambda col: vecs.ap[:, col + j:col + j + 1]
            if j == 0:
                b_proj(0)
            P.I("act", "activation", [R_zw, vecs], [F["lw"]], F["lw"].ap[:], R_zw.ap[:], AF.Sigmoid, bias=vj(C_W0))
            P.I("act", "activation", [R_za, vecs], [F["a"]], F["a"].ap[:], R_za.ap[:], AF.Sigmoid, bias=vj(C_A0))
            P.I("act", "copy", [R_k], [F["k"]], F["k"].ap[:], R_k.ap[:])
            P.I("act", "activation", [R_k, vecs], [kk2], kk2.ap[:], R_k.ap[:], AF.Square, scale=vj(C_KK))
            P.mm(R_st, R_st.ap[:], conb, BO_bf, kk2, kk2.ap[:])
            P.I("dve", "tensor_scalar", [F["lw"]], [F["lw"]], F["lw"].ap[:], F["lw"].ap[:], -EXPM05, None, ALU.mult)
            P.I("dve", "tensor_tensor_scan", [con, F["lw"]], [F["cum"]], F["cum"].ap[:], MC, F["lw"].ap[:], 0.0, ALU.mult, ALU.add)
            P.I("dve", "tensor_tensor", [F["cum"], F["lw"]], [F["egm"]], F["egm"].ap[:], F["cum"].ap[:], F["lw"].ap[:], ALU.subtract)
            P.I("dve", "tensor_scalar", [F["k"], vecs], [F["kk"]], F["kk"].ap[:], F["k"].ap[:], vj(C_KK), None, ALU.mult)
            P.I("act", "activation", [F["cum"]], [F["eg"]], F["eg"].ap[:], F["cum"].ap[:], AF.Exp)
            P.I("act", "activation", [F["cum"]], [F["eneg"]], F["eneg"].ap[:], F["cum"].ap[:], AF.Exp, scale=-1.0)
            P.I("act", "activation", [F["egm"]], [F["egm"]], F["egm"].ap[:], F["egm"].ap[:], AF.Exp)
            P.I("act", "activation", [R_st, eps_t], [F["rn"]], F["rn"].ap[:], R_st.ap[:], AF.Ln, bias=eps_t.ap[:, 1:2])
            P.I("act", "activation", [F["rn"]], [F["rn"]], F["rn"].ap[:], F["rn"].ap[:], AF.Exp, scale=-0.5)
            gL = v3(F["eg"].ap[:])[:, :, 63:64]
            gLb = gL.broadcast_to([128, C, 64])
            P.I("dve", "tensor_tensor", [F["kk"], F["rn"]], [F["kk"]], F["kk"].ap[:], F["kk"].ap[:], F["rn"].ap[:], ALU.mult)
            P.I("dve", "scalar_tensor_tensor", [F["kk"], F["egm"]], [AR], AR.ap[:, :, 0:64], v3(F["kk"].ap[:]), -1.0, v3(F["egm"].ap[:]), ALU.mult, ALU.mult)
            P.I("dve", "tensor_tensor", [R_r, F["eg"]], [AR], AR.ap[:, :, 64:128], v3(R_r.ap[:]), v3(F["eg"].ap[:]), ALU.mult)
            halves("dve", "tensor_copy", [AR], [BD["A_bd"]], lambda hs, hh: BD["A_bd"].ap[hs, :, 64 * hh:64 * hh + 64], [lambda hs, hh: AR.ap[hs, :, 0:64]])
            P.I("dve", "tensor_tensor", [F["kk"], F["a"]], [F["ka"]], F["ka"].ap[:], F["kk"].ap[:], F["a"].ap[:], ALU.mult)
            P.I("dve", "tensor_tensor", [F["ka"], F["eneg"]], [BKr], BKr.ap[:, :, 0:64], v3(F["ka"].ap[:]), v3(F["eneg"].ap[:]), ALU.mult)
            halves("dve", "tensor_copy", [BKr], [BD["B_bd"]], lambda hs, hh: BD["B_bd"].ap[hs, :, 64 * hh:64 * hh + 64], [lambda hs, hh: BKr.ap[hs, :, 0:64]])
            halves("dve", "tensor_tensor", [BKr, F["eg"]], [BD["Bb_bd"]], lambda hs, hh: BD["Bb_bd"].ap[hs, :, 64 * hh:64 * hh + 64],
                   [lambda hs, hh: BKr.ap[hs, :, 0:64], lambda hs, hh: gLb[hs]], ALU.mult)
            P.I("dve", "tensor_scalar", [F["a"], vecs, omka], [F["fac"]], F["fac"].ap[:], F["a"].ap[:], vj(C_KA), omka.ap[:, j:j + 1], ALU.mult, ALU.add)
            P.I("dve", "tensor_tensor", [F["k"], F["fac"]], [F["km"]], F["km"].ap[:], F["k"].ap[:], F["fac"].ap[:], ALU.mult)
            P.I("dve", "tensor_tensor", [F["km"], F["eneg"]], [BKr], BKr.ap[:, :, 64:128], v3(F["km"].ap[:]), v3(F["eneg"].ap[:]), ALU.mult)
            halves("dve", "tensor_copy", [BKr], [BD["K_bd"]], lambda hs, hh: BD["K_bd"].ap[hs, :, 64 * hh:64 * hh + 64], [lambda hs, hh: BKr.ap[hs, :, 64:128]])
            halves("dve", "tensor_tensor", [BKr, F["eg"]], [BD["Kb_bd"]], lambda hs, hh: BD["Kb_bd"].ap[hs, :, 64 * hh:64 * hh + 64],
                   [lambda hs, hh: BKr.ap[hs, :, 64:128], lambda hs, hh: gLb[hs]], ALU.mult)
            P.I("dve", "scalar_tensor_tensor", [R_r, vecs, F["km"]], [rkb], rkb.ap[:], R_r.ap[:], vj(C_RK), F["km"].ap[:], ALU.mult, ALU.mult)
            P.mm(R_st2, R_st2.ap[:], conb, BO_bf, rkb, rkb.ap[:])
            P.I("act", "copy", [R_v], [F["v"]], F["v"].ap[:], R_v.ap[:])
            P.I("dve", "tensor_tensor", [R_st2, F["v"]], [F["bonus"]], F["bonus"].ap[:], R_st2.ap[:], F["v"].ap[:], ALU.mult)
            halves("dve", "tensor_copy", [F["v"]], [BD["V_bd"]], lambda hs, hh: BD["V_bd"].ap[hs, :, 64 * hh:64 * hh + 64], [lambda hs, hh: v3(F["v"].ap[:])[hs]])
            P.I("act", "copy", [R_g], [F["g"]], F["g"].ap[:], R_g.ap[:])
            P.I("dve", "tensor_tensor", [con, F["eg"]], [diagG], diagG.ap[:], IQ.unsqueeze(1).broadcast_to([128, C, 64]), gLb, ALU.mult)

            ckpt("B", [("AR", AR, AR.ap[:], [128, C, 128], BF16), ("BKr", BKr, BKr.ap[:], [128, C, 128], BF16), ("cum", F["cum"], F["cum"].ap[:], [128, NT], F32),
                       ("bonus", F["bonus"], F["bonus"].ap[:], [128, NT], F32), ("Vbd", BD["V_bd"], BD["V_bd"].ap[:], [128, C, 128], BF16),
                       ("Bbbd", BD["Bb_bd"], BD["Bb_bd"].ap[:], [128, C, 128], BF16), ("diagG", diagG, diagG.ap[:], [128, C, 64], F32)])
            tpf = [TV(PB[4], pbank[4][:, :].rearrange("p (c n) -> p c n", c=C), "tpA"), TV(PB[5], pbank[5][:, :].rearrange("p (c n) -> p c n", c=C), "tpB")]

            def do_tp(i_, nm):
                tp = tpf[i_ % 2]
                for c in range(C):
                    P.mm(tp, tp.ap[:, c, :], BD[nm], BD[nm].ap[:, c, :], conb, ID_bf)
                return tp
            tp = do_tp(0, "A_bd")
            ckpt("T0", [("gm", gm, gm.ap[:], [128, 8], F32)])
            halves("act", "copy", [tp], [Xt], lambda hs, hh: Xt.ap[hs, :, 0:64], [lambda hs, hh: tp.ap[hs, :, 64 * hh:64 * hh + 64]])
            ckpt("T1", [("Xt", Xt, Xt.ap[:], [128, C, 128], BF16)])
            tp = do_tp(1, "Bb_bd")
            halves("act", "copy", [tp], [RH["TBr"]], lambda hs, hh: RH["TBr"].ap[hs], [lambda hs, hh: tp.ap[hs, :, 64 * hh:64 * hh + 64]])
            tp = do_tp(2, "Kb_bd")
            halves("act", "copy", [tp], [RH["TKr"]], lambda hs, hh: RH["TKr"].ap[hs], [lambda hs, hh: tp.ap[hs, :, 64 * hh:64 * hh + 64]])
            tp = do_tp(3, "V_bd")
            halves("act", "copy", [tp], [RH["TVr"]], lambda hs, hh: RH["TVr"].ap[hs], [lambda hs, hh: tp.ap[hs, :, 64 * hh:64 * hh + 64]])
            P.I("act", "copy", [tp], [BD["TV_bd"]], BD["TV_bd"].ap[:], tp.ap[:])
            ckpt("T", [("Xt", Xt, Xt.ap[:], [128, C, 128], BF16), ("TVbd", BD["TV_bd"], BD["TV_bd"].ap[:], [128, C, 128], BF16)])
            b6 = B6.ap[:].rearrange("p (c n) -> p c n", c=C)
            b7 = B7.ap[:].rearrange("p (c n) -> p c n", c=C)
            zw3 = v3(R_zw.ap[:])
            for c in range(C):
                P.mm(B6, b6[:, c, :], BD["B_bd"], BD["B_bd"].ap[:, c, :], AR, AR.ap[:, c, :])
            for c in range(C):
                P.mm(R_zw, zw3[:, c, :], BD["K_bd"], BD["K_bd"].ap[:, c, :], AR, AR.ap[:, c, 64:128])
            for c in range(C):
                P.mm(B7, b7[:, c, :], BD["A_bd"], BD["A_bd"].ap[:, c, :], BKr, BKr.ap[:, c, :])
            MSb = MS.unsqueeze(1).broadcast_to([128, C, 64])
            MIb = MI.unsqueeze(1).broadcast_to([128, C, 64])
            MTb = MT.unsqueeze(1).broadcast_to([128, C, 64])
            IQb = IQ.unsqueeze(1).broadcast_to([128, C, 64])
            P.I("dve", "tensor_tensor", [B6, con], [RH["Nr"]], RH["Nr"].ap[:], b6[:, :, 0:64], MSb, ALU.mult)
            P.I("dve", "tensor_tensor", [B6, con], [RH["Arb"]], RH["Arb"].ap[:], b6[:, :, 64:128], MIb, ALU.mult)
            P.I("dve", "tensor_tensor", [R_zw, con], [RH["Ark"]], RH["Ark"].ap[:], zw3, MIb, ALU.mult)
            P.I("dve", "tensor_tensor", [B7, con], [RH["NTr"]], RH["NTr"].ap[:], b7[:, :, 0:64], MTb, ALU.mult)
            P.I("dve", "tensor_tensor", [B7, con], [Xt], Xt.ap[:, :, 64:128], b7[:, :, 64:128], MTb, ALU.mult)
            P.I("dve", "tensor_tensor", [RH["Nr"], con], [RH["Tr"]], RH["Tr"].ap[:], RH["Nr"].ap[:], IQb, ALU.add)
            halves("dve", "tensor_copy", [RH["Nr"]], [BD["N_bd"]], lambda hs, hh: BD["N_bd"].ap[hs, :, 64 * hh:64 * hh + 64], [lambda hs, hh: RH["Nr"].ap[hs]])
            halves("dve", "tensor_copy", [RH["NTr"]], [BD["NT_bd"]], lambda hs, hh: BD["NT_bd"].ap[hs, :, 64 * hh:64 * hh + 64], [lambda hs, hh: RH["NTr"].ap[hs]])
            ckpt("C1", [("Nr", RH["Nr"], RH["Nr"].ap[:], [128, C, 64], BF16), ("NTr", RH["NTr"], RH["NTr"].ap[:], [128, C, 64], BF16),
                        ("Xt", Xt, Xt.ap[:], [128, C, 128], BF16), ("TVbd", BD["TV_bd"], BD["TV_bd"].ap[:], [128, C, 128], BF16),
                        ("Arb", RH["Arb"], RH["Arb"].ap[:], [128, C, 64], BF16), ("Nbd", BD["N_bd"], BD["N_bd"].ap[:], [128, C, 128], BF16)])
            qa = v3(pbank[6][:, 0:256])
            qb = v3(pbank[6][:, 256:512])
            qc = v3(pbank[7][:, 0:256])
            qd = v3(pbank[7][:, 256:512])
            NLEV = 5

            def squarings(do_a):
                if do_a:
                    for c in range(C):
                        P.mm(B6, qa[:, c, :], BD["NT_bd"], BD["NT_bd"].ap[:, c, :], RH["Nr"], RH["Nr"].ap[:, c, :])
                for c in range(C):
                    P.mm(B6, qb[:, c, :], BD["N_bd"], BD["N_bd"].ap[:, c, :], RH["NTr"], RH["NTr"].ap[:, c, :])

            def evac_forms(do_a):
                if do_a:
                    P.I("act", "copy", [B6], [RH["Nr"]], RH["Nr"].ap[:], qa)
                    P.I("act", "copy", [B6], [RH["NTr"]], RH["NTr"].ap[:], qb)
                    halves("dve", "tensor_copy", [RH["Nr"]], [BD["N_bd"]], lambda hs, hh: BD["N_bd"].ap[hs, :, 64 * hh:64 * hh + 64], [lambda hs, hh: RH["Nr"].ap[hs]])
                    halves("dve", "tensor_copy", [RH["NTr"]], [BD["NT_bd"]], lambda hs, hh: BD["NT_bd"].ap[hs, :, 64 * hh:64 * hh + 64], [lambda hs, hh: RH["NTr"].ap[hs]])
                else:
                    halves("act", "copy", [B6], [BD["NT_bd"]], lambda hs, hh: BD["NT_bd"].ap[hs, :, 64 * hh:64 * hh + 64], [lambda hs, hh: qb[hs]])

            squarings(True)
            evac_forms(True)
            for lev in range(1, NLEV + 1):
                for c in range(C):
                    P.mm(B7, qc[:, c, :], BD["NT_bd"], BD["NT_bd"].ap[:, c, :], RH["Tr"], RH["Tr"].ap[:, c, :])
                if lev < NLEV:
                    squarings(lev + 1 < NLEV)
                P.I("dve", "tensor_tensor", [B7, RH["Tr"]], [RH["Tr"]], RH["Tr"].ap[:], qc, RH["Tr"].ap[:], ALU.add)
                if lev < NLEV:
                    evac_forms(lev + 1 < NLEV)
            halves("dve", "tensor_copy", [RH["Tr"]], [BD["T_bd"]], lambda hs, hh: BD["T_bd"].ap[hs, :, 64 * hh:64 * hh + 64], [lambda hs, hh: RH["Tr"].ap[hs]])
            ckpt("inv", [("Tr", RH["Tr"], RH["Tr"].ap[:], [128, C, 64], BF16)])
            for c in range(C):
                P.mm(B6, b6[:, c, :], BD["T_bd"], BD["T_bd"].ap[:, c, :], Xt, Xt.ap[:, c, :])
            halves("act", "copy", [B6], [BD["AhT_bd"]], lambda hs, hh: BD["AhT_bd"].ap[hs, :, 64 * hh:64 * hh + 64], [lambda hs, hh: b6[hs, :, 0:64]])
            halves("act", "copy", [B6], [BD["M1T_bd"]], lambda hs, hh: BD["M1T_bd"].ap[hs, :, 64 * hh:64 * hh + 64], [lambda hs, hh: b6[hs, :, 64:128]])
            for c in range(C):
                P.mm(B7, qc[:, c, :], BD["AhT_bd"], BD["AhT_bd"].ap[:, c, :], RH["Arb"], RH["Arb"].ap[:, c, :])
            for c in range(C):
                P.mm(B7, qd[:, c, :], BD["M1T_bd"], BD["M1T_bd"].ap[:, c, :], RH["Arb"], RH["Arb"].ap[:, c, :])
            P.I("dve", "tensor_tensor", [B7, AR], [RH["Rhat"]], RH["Rhat"].ap[:], qc, AR.ap[:, :, 64:128], ALU.add)
            P.I("dve", "tensor_tensor", [B7, RH["Ark"]], [RH["M2"]], RH["M2"].ap[:], qd, RH["Ark"].ap[:], ALU.add)
            for c in range(C):
                P.mm(B6, qa[:, c, :], BD["AhT_bd"], BD["AhT_bd"].ap[:, c, :], RH["TBr"], RH["TBr"].ap[:, c, :])
            for c in range(C):
                P.mm(B6, qb[:, c, :], BD["M1T_bd"], BD["M1T_bd"].ap[:, c, :], RH["TBr"], RH["TBr"].ap[:, c, :])
            P.I("dve", "tensor_tensor", [B6, diagG], [tmpP], tmpP.ap[:], qa, diagG.ap[:], ALU.add)
            P.I("dve", "tensor_tensor", [B6, RH["TKr"]], [tmpW], tmpW.ap[:], qb, RH["TKr"].ap[:], ALU.add)
            halves("dve", "tensor_copy", [tmpP], [BD["P_bd"]], lambda hs, hh: BD["P_bd"].ap[hs, :, 64 * hh:64 * hh + 64], [lambda hs, hh: tmpP.ap[hs]])
            halves("dve", "tensor_copy", [tmpW], [BD["W2T_bd"]], lambda hs, hh: BD["W2T_bd"].ap[hs, :, 64 * hh:64 * hh + 64], [lambda hs, hh: tmpW.ap[hs]])

            ckpt("C2", [("Rhat", RH["Rhat"], RH["Rhat"].ap[:], [128, C, 64], BF16), ("M2", RH["M2"], RH["M2"].ap[:], [128, C, 64], BF16),
                        ("Pbd", BD["P_bd"], BD["P_bd"].ap[:], [128, C, 128], BF16), ("W2Tbd", BD["W2T_bd"], BD["W2T_bd"].ap[:], [128, C, 128], BF16)])
            for c in range(C):
                P.mm(R_r, R_r.ap[:, c * 64:(c + 1) * 64], St_bd[j], St_bd[j].ap[:], RH["Rhat"], RH["Rhat"].ap[:, c, :], start=True, stop=False)
                P.mm(R_r, R_r.ap[:, c * 64:(c + 1) * 64], BD["TV_bd"], BD["TV_bd"].ap[:, c, :], RH["M2"], RH["M2"].ap[:, c, :], start=False, stop=True)
                P.mm(R_st, R_st.ap[:, 0:64], BD["P_bd"], BD["P_bd"].ap[:, c, :], St_r[j], St_r[j].ap[:], start=True, stop=False)
                P.mm(R_st, R_st.ap[:, 0:64], BD["W2T_bd"], BD["W2T_bd"].ap[:, c, :], RH["TVr"], RH["TVr"].ap[:, c, :], start=False, stop=True)
                P.I("act", "copy", [R_st], [St_r[j]], St_r[j].ap[:], R_st.ap[:, 0:64])
                for hh in range(2):
                    P.I("act", "copy", [R_st], [St_bd[j]], St_bd[j].ap[HS[hh], 64 * hh:64 * hh + 64], R_st.ap[HS[hh], 0:64])

            ckpt("D", [("Stbd", St_bd[j], St_bd[j].ap[:], [128, 128], BF16)])
            P.I("act", "copy", [R_r], [F["Y"]], F["Y"].ap[:], R_r.ap[:])
            if j < 7:
                b_proj(j + 1)
            P.mm(R_st2, R_st2.ap[:], con, BO64, F["Y"], F["Y"].ap[:])
            P.I("dve", "tensor_tensor", [F["Y"], R_st2], [F["yc"]], F["yc"].ap[:], F["Y"].ap[:], R_st2.ap[:], ALU.subtract)
            P.I("act", "activation", [F["yc"]], [F["sq2"]], F["sq2"].ap[:], F["yc"].ap[:], AF.Square)
            P.mm(R_st, R_st.ap[:], con, BO64, F["sq2"], F["sq2"].ap[:])
            P.I("act", "activation", [R_st, eps_t], [F["rs"]], F["rs"].ap[:], R_st.ap[:], AF.Ln, bias=eps_t.ap[:, 2:3])
            P.I("act", "activation", [F["rs"]], [F["rs"]], F["rs"].ap[:], F["rs"].ap[:], AF.Exp, scale=-0.5)
            P.I("dve", "tensor_tensor", [F["yc"], F["rs"]], [F["yc"]], F["yc"].ap[:], F["yc"].ap[:], F["rs"].ap[:], ALU.mult)
            P.I("act", "activation", [F["yc"], vecs], [F["yc"]], F["yc"].ap[:], F["yc"].ap[:], AF.Identity, bias=vj(C_GNB), scale=vj(C_GNW))
            P.I("dve", "tensor_tensor", [F["yc"], F["bonus"]], [F["yc"]], F["yc"].ap[:], F["yc"].ap[:], F["bonus"].ap[:], ALU.add)
            P.I("dve", "tensor_tensor", [F["yc"], F["g"]], [yo], yo.ap[:, j, :], F["yc"].ap[:], F["g"].ap[:], ALU.mult)

        ckpt("E", [("yo", yo, yo.ap[:], [128, 8, NT], BF16)])
        o_ring = Ring([R_v, R_za, R_g])
        for jo in range(8):
            ps = o_ring.next()
            for kc in range(8):
                P.mm(ps, ps.ap[:], wo, wo.ap[:, kc, jo * 128:(jo + 1) * 128], yo, yo.ap[:, kc, :], start=(kc == 0), stop=(kc == 7))
            P.I("dve", "scalar_tensor_tensor", [ps, g1p, x_t], [x_t], x_t.ap[:, jo, :], ps.ap[:], g1p.ap[:, jo:jo + 1], x_t.ap[:, jo, :], ALU.mult, ALU.add)
        P.dma("sp", None, yT_v[:, :, c0:c0 + NT], x_t, x_t.ap[:])


NCORES = 8
RUN_KW = {}
LAST_RES = None
ARENA_BYTES = 211968
_FUSED = []


BLOCKS = ("rw", "f0", "lr", "f1")


def build_fused():
    nc, P = new_prog()
    pbank = [P.ps([128, 512], F32, f"pb{i}") for i in range(8)]
    P.arena_init(ARENA_BYTES)
    VT = 2 * NTOK
    xT_d = nc.dram_tensor("xT", [D, VT], F32, kind="ExternalInput").ap()
    cT_d = nc.dram_tensor("cT", [128, 8], F32, kind="ExternalInput").ap()
    x1_d = nc.dram_tensor("x1_scr", [D, VT], F32, kind="Internal").ap()
    x2_d = nc.dram_tensor("x2_scr", [D, VT], F32, kind="Internal").ap()
    x3_d = nc.dram_tensor("x3_scr", [D, NTOK], F32, kind="Internal").ap()
    yT_d = nc.dram_tensor("yT", [D, NTOK], F32, kind="ExternalOutput").ap()
    fm = lambda ap: ap.rearrange("(j p) t -> p j t", p=128)
    if "rw" in BLOCKS:
        emit_rwkv(P, nc, pbank, fm(xT_d), fm(x1_d), "rw_", cT_d)
        P.barrier()
        P.arena_reset()
    if "f0" in BLOCKS:
        emit_ffn(P, nc, pbank, fm(x1_d if "rw" in BLOCKS else xT_d), fm(x2_d), VT, False, "f0_", cT_d)
        P.barrier()
        P.arena_reset()
    if "lr" in BLOCKS:
        emit_lru(P, nc, pbank, fm(x2_d if "f0" in BLOCKS else xT_d), fm(x3_d), "lr_", cT_d)
        P.barrier()
        P.arena_reset()
    if "f1" in BLOCKS:
        emit_ffn(P, nc, pbank, fm(x3_d if "lr" in BLOCKS else xT_d[:, 0:NTOK]), fm(yT_d), NTOK, True, "f1_", cT_d)
    P.emit()
    P.close()
    return nc, P


IDENT = np.eye(128, dtype=np.float32)


def kernel(x, c, ada_w, ada_b, norm_g, final_g,
           rwkv_mu, rwkv_w_rkv, rwkv_w_o, rwkv_w0, rwkv_w1, rwkv_w2, rwkv_a0, rwkv_a1, rwkv_a2,
           rwkv_g1, rwkv_g2, rwkv_k_k, rwkv_k_a, rwkv_r_k, rwkv_gn_w, rwkv_gn_b,
           lru_w_in, lru_conv_w, lru_conv_b, lru_w_gates, lru_b_gates, lru_lam, lru_w_out,
           ffn_w_gu, ffn_w_d, moe_w_router, moe_b_router, moe_w_gu, moe_w_d):
    f = lambda a: np.ascontiguousarray(np.asarray(a, dtype=np.float32))
    x, c, ada_w, ada_b, norm_g, final_g = f(x), f(c), f(ada_w), f(ada_b), f(norm_g), f(final_g)
    if not _FUSED:
        _FUSED.append(build_fused())
    nc, _ = _FUSED[0]
    B = x.shape[0]
    consts = rwkv_consts()
    bg = f(lru_b_gates)[0]
    cw = f(lru_conv_w)[0]
    shared = {
        "rw_consts": consts, "rw_adaw": f(ada_w[0][:, 0:3 * D]), "rw_wrkv": f(rwkv_w_rkv)[0], "rw_wo": f(rwkv_w_o)[0],
        "rw_w1": f(rwkv_w1)[0], "rw_w2": f(rwkv_w2)[0], "rw_a1": f(rwkv_a1)[0], "rw_a2": f(rwkv_a2)[0],
        "rw_g1": f(rwkv_g1)[0], "rw_g2": f(rwkv_g2)[0],
        "f0_vecs": pack_vecs([("ng", norm_g[0, 1]), ("adab", ada_b[0][3 * D:6 * D])])[0], "f0_adaw": f(ada_w[0][:, 3 * D:6 * D]),
        "f0_wgu": f(ffn_w_gu), "f0_wd": f(ffn_w_d),
        "lr_adaw": f(ada_w[1][:, 0:3 * D]), "lr_win": f(lru_w_in)[0], "lr_wg": f(lru_w_gates)[0], "lr_wout": f(lru_w_out)[0],
        "f1_vecs": pack_vecs([("ng", norm_g[1, 1]), ("adab", ada_b[1][3 * D:6 * D]), ("fg", final_g)])[0],
        "f1_adaw": f(ada_w[1][:, 3 * D:6 * D]), "f1_wgu": f(moe_w_gu)[0], "f1_wd": f(moe_w_d)[0], "f1_ident": IDENT,
        "f1_wr": f(moe_w_router)[0], "f1_br": f(np.broadcast_to(f(moe_b_router)[0].reshape(1, NE), (128, NE))),
    }
    in_maps = []
    for core in range(NCORES):
        b, half = core // 2, core % 2
        flag = np.full((128,), float(half), np.float32)
        xv = np.empty((D, 2 * NTOK), np.float32)
        if half == 1:
            xv[:, :] = x[b].T
        else:
            xv[:, :NTOK] = x[b, 0:NTOK].T
            xv[:, NTOK:] = x[b, 0:NTOK].T
        rw_vecs = pack_vecs([("ng", norm_g[0, 0]), ("adab", ada_b[0][0:3 * D]), ("mu", f(rwkv_mu)[0].reshape(-1)), ("w0", f(rwkv_w0)[0]),
                             ("a0", f(rwkv_a0)[0]), ("kk", f(rwkv_k_k)[0]), ("ka", f(rwkv_k_a)[0]), ("rk", f(rwkv_r_k)[0].reshape(-1)),
                             ("gnw", f(rwkv_gn_w)[0]), ("gnb", f(rwkv_gn_b)[0]), ("flag", flag)])[0]
        lr_vecs = pack_vecs([("ng", norm_g[1, 0]), ("adab", ada_b[1][0:3 * D]), ("cw0", cw[0]), ("cw1", cw[1]), ("cw2", cw[2]), ("cw3", cw[3]),
                             ("cb", f(lru_conv_b)[0]), ("bgr", bg[:, 0:256].reshape(-1)), ("bgi", bg[:, 256:512].reshape(-1)),
                             ("lam", f(lru_lam)[0]), ("flag", flag)])[0]
        m = dict(shared)
        m.update({"xT": xv, "cT": pack_vec(c[b]), "rw_vecs": rw_vecs, "lr_vecs": lr_vecs})
        in_maps.append(m)
    if len(BLOCKS) < 4:
        pre = tuple(b_ + "_" for b_ in BLOCKS)
        in_maps = [{k: v for k, v in m.items() if k in ("xT", "cT") or k.startswith(pre)} for m in in_maps]
    res = run_bass_kernel_spmd(nc, in_maps, core_ids=list(range(NCORES)), **RUN_KW)
    global LAST_RES
    LAST_RES = res
    out = np.empty((B, 2 * NTOK, D), np.float32)
    for core in range(NCORES):
        b, half = core // 2, core % 2
        out[b, half * NTOK:(half + 1) * NTOK, :] = res.results[core]["yT"].T
    return out
```

```python
import contextlib
import numpy as np
import concourse.bass as bass
import concourse.mybir as mybir

F32 = mybir.dt.float32
BF16 = mybir.dt.bfloat16
ALU = mybir.AluOpType
AF = mybir.ActivationFunctionType
AX = mybir.AxisListType

ENGS = ("pe", "act", "dve", "pool", "sp")
DMA_RING = 8


class T:
    __slots__ = ("ap", "w", "r", "name")

    def __init__(self, ap, name=""):
        self.ap = ap
        self.w = None
        self.r = []
        self.name = name

    def __getitem__(self, idx):
        return self.ap[idx]


class TV:
    def __init__(self, parent, ap, name=""):
        self.parent = parent
        self.ap = ap
        self.name = name

    @property
    def w(self):
        return self.parent.w

    @w.setter
    def w(self, v):
        self.parent.w = v

    @property
    def r(self):
        return self.parent.r

    @r.setter
    def r(self, v):
        self.parent.r = v


class Op:
    __slots__ = ("eng", "fn", "deps", "signal", "count", "is_dma", "dma_idx", "idx")

    def __init__(self, eng, fn, is_dma):
        self.eng = eng
        self.fn = fn
        self.deps = []
        self.signal = False
        self.count = 0
        self.is_dma = is_dma
        self.dma_idx = -1
        self.idx = -1


class Prog:
    def __init__(self, nc):
        self.nc = nc
        self.ops = []
        self.stack = contextlib.ExitStack()
        self.n_alloc = 0
        self.arena = None
        self.arena_off = 0
        self.arena_size = 0
        self.fence = {}
        self.last_op = {}
        self.recent_dma = {e: [] for e in ENGS}

    def arena_init(self, nbytes):
        self.arena_size = nbytes
        self.arena = self.stack.enter_context(self.nc.sbuf_tensor("arena", [128, nbytes // 2], BF16))
        self.arena_off = 0

    def arena_reset(self):
        self.arena_off = 0

    def barrier(self):
        deps = [o for o in self.last_op.values()]
        for e in ENGS:
            deps.extend(self.recent_dma[e])
        for e in ENGS:
            self.fence[e] = list(deps)

    def sb(self, shape, dtype, name=None):
        if self.arena is not None:
            esize = 4 if dtype == F32 else 2
            n = 1
            for d_ in shape[1:]:
                n *= d_
            off = (self.arena_off + 63) // 64 * 64
            self.arena_off = off + n * esize
            assert self.arena_off <= self.arena_size, ("SBUF arena overflow", name, self.arena_off)
            ap = self.arena[0:shape[0], off // 2:(off + n * esize) // 2]
            if dtype == F32:
                ap = ap.bitcast(F32)
            if len(shape) == 3:
                ap = ap.rearrange("p (a b) -> p a b", a=shape[1])
            elif len(shape) == 4:
                ap = ap.rearrange("p (a b c) -> p a b c", a=shape[1], b=shape[2])
            return ap
        self.n_alloc += 1
        name = f"sb{self.n_alloc}_{name or ''}"
        h = self.stack.enter_context(self.nc.sbuf_tensor(name, list(shape), dtype))
        return h

    def ps(self, shape, dtype, name=None):
        self.n_alloc += 1
        name = f"ps{self.n_alloc}_{name or ''}"
        h = self.stack.enter_context(self.nc.psum_tensor(name, list(shape), dtype))
        return h

    def tile(self, shape, dtype, name=None):
        h = self.sb(shape, dtype, name)
        return T(h[:] if False else h, name or "")

    def op(self, eng, fn, reads=(), writes=(), dma=False):
        o = Op(eng, fn, dma)
        o.idx = len(self.ops)
        deps = []
        for t in reads:
            if t.w is not None:
                deps.append((t.w, 0))
        for t in writes:
            if t.w is not None:
                deps.append((t.w, 0))
            for r in t.r:
                deps.append((r, 1))
        if self.fence.get(eng):
            for d in self.fence[eng]:
                deps.append((d, 0))
            self.fence[eng] = None
        seen = set()
        for d, war in deps:
            if d.idx in seen:
                continue
            if (not d.is_dma) and d.eng == eng and (eng == "pe" or war):
                continue
            seen.add(d.idx)
            o.deps.append(d)
        for t in reads:
            if not dma:
                t.r = [x for x in t.r if x.is_dma or x.eng != eng]
            t.r.append(o)
        for t in writes:
            t.w = o
            t.r = []
        self.ops.append(o)
        if dma:
            self.recent_dma[eng] = (self.recent_dma[eng] + [o])[-DMA_RING:]
        else:
            self.last_op[eng] = o
        return o

    def emit(self):
        nc = self.nc
        ops = self.ops
        for o in ops:
            for d in o.deps:
                d.signal = True
        cnt = {e: 0 for e in ENGS}
        dcnt = {e: 0 for e in ENGS}
        for o in ops:
            if o.is_dma:
                o.dma_idx = dcnt[o.eng]
                dcnt[o.eng] += 1
            elif o.signal:
                cnt[o.eng] += 1
                o.count = cnt[o.eng]
        sems = {}
        for e in ENGS:
            sems[e] = self.stack.enter_context(nc.semaphore(f"s_{e}"))
        dsems = {}
        for e in ENGS:
            if dcnt[e] > 0:
                dsems[e] = [self.stack.enter_context(nc.semaphore(f"d_{e}{i}")) for i in range(DMA_RING)]
        per_eng = {e: [o for o in ops if o.eng == e] for e in ENGS}
        self.stats = {e: (len(per_eng[e]), cnt[e], dcnt[e]) for e in ENGS}

        def sem_target(d):
            if d.is_dma:
                return dsems[d.eng][d.dma_idx % DMA_RING], 16 * (d.dma_idx // DMA_RING + 1)
            return sems[d.eng], d.count

        def run_engine(e, eng):
            waited = {}
            nwait = 0
            for o in per_eng[e]:
                need = {}
                for d in o.deps:
                    s, v = sem_target(d)
                    key = id(s)
                    if waited.get(key, 0) >= v:
                        continue
                    if key not in need or need[key][1] < v:
                        need[key] = (s, v)
                if o.is_dma and o.dma_idx >= DMA_RING:
                    s = dsems[e][o.dma_idx % DMA_RING]
                    v = 16 * (o.dma_idx // DMA_RING)
                    key = id(s)
                    if waited.get(key, 0) < v and (key not in need or need[key][1] < v):
                        need[key] = (s, v)
                for key, (s, v) in need.items():
                    eng.wait_ge(s, v)
                    waited[key] = v
                    nwait += 1
                ins = o.fn(eng)
                if o.is_dma:
                    s, _ = sem_target(o)
                    ins.then_inc(s, 16)
                elif o.signal:
                    ins.then_inc(sems[e], 1)
            if e == "sp":
                for q in ENGS:
                    for i in range(min(DMA_RING, dcnt[q])):
                        n = (dcnt[q] - 1 - i) // DMA_RING + 1
                        eng.wait_ge(dsems[q][i], 16 * n)
            return nwait

        block = self.stack.enter_context(nc.Block())
        self.nwaits = {}

        if per_eng["pe"]:
            @block.tensor
            def _(eng):
                self.nwaits["pe"] = run_engine("pe", eng)
        if per_eng["act"]:
            @block.scalar
            def _(eng):
                self.nwaits["act"] = run_engine("act", eng)
        if per_eng["dve"]:
            @block.vector
            def _(eng):
                self.nwaits["dve"] = run_engine("dve", eng)
        if per_eng["pool"]:
            @block.gpsimd
            def _(eng):
                self.nwaits["pool"] = run_engine("pool", eng)
        if True:
            @block.sync
            def _(eng):
                self.nwaits["sp"] = run_engine("sp", eng)

    def close(self):
        self.stack.close()

    def I(self, eng, method, reads, writes, *args, **kw):
        return self.op(eng, lambda e: getattr(e, method)(*args, **kw), list(reads), list(writes))

    def dma(self, eng, out_t, out_ap, in_t, in_ap, **kw):
        reads = [in_t] if in_t is not None else []
        writes = [out_t] if out_t is not None else []
        return self.op(eng, lambda e: e.dma_start(out=out_ap, in_=in_ap, **kw), reads, writes, dma=True)

    def mm(self, out_t, out_ap, lhsT_t, lhsT_ap, rhs_t, rhs_ap, start=True, stop=True, extra_reads=(), **kw):
        return self.op("pe", lambda e: e.matmul(out_ap, lhsT_ap, rhs_ap, start=start, stop=stop, **kw),
                       [lhsT_t, rhs_t] + list(extra_reads), [out_t])


from concourse.bass_utils import run_bass_kernel_spmd

D = 1024
JC = 8
NTOK = 2048
FF = 3584
FC = 28
NE = 8
NORM_EPS = 1e-6


def pack_vec(v):
    v = np.asarray(v, np.float32).reshape(-1)
    n = v.shape[0] // 128
    return np.ascontiguousarray(v.reshape(n, 128).T)


def pack_vecs(named):
    cols = {}
    arrs = []
    off = 0
    for k, v in named:
        a = pack_vec(v)
        cols[k] = (off, a.shape[1])
        arrs.append(a)
        off += a.shape[1]
    return np.ascontiguousarray(np.concatenate(arrs, axis=1)), cols


class Ring:
    def __init__(self, tiles):
        self.tiles = tiles
        self.i = 0

    def next(self):
        t = self.tiles[self.i % len(self.tiles)]
        self.i += 1
        return t


def new_prog():
    nc = bass.Bass("TRN2", target_bir_lowering=False)
    return nc, Prog(nc)


def emit_mod(P, nc, cT_d, adaw_d, nvec, vecs, adab_col, scratch_h, ps_bank):
    cT = P.tile([128, 8], F32, "cT")
    P.dma("sp", cT, cT.ap[:], None, cT_d)
    sc_bf = P.tile([128, 8], BF16, "sc_bf")
    P.op("act", lambda e: e.activation(sc_bf.ap[:], cT.ap[:], AF.Silu), [cT], [sc_bf])
    ncols = nvec * D
    aw = T(scratch_h, "adaw_sb")
    src = adaw_d.rearrange("(kc p) n -> p kc n", p=128)
    for v in range(nvec):
        P.dma("pool", aw, aw.ap[:, :, v * D:(v + 1) * D], None, src[:, :, v * D:(v + 1) * D])
    noc = nvec * 8
    for oc in range(noc):
        for kc in range(8):
            P.mm(ps_bank, ps_bank.ap[:, oc:oc + 1], aw, aw.ap[:, kc, oc * 128:(oc + 1) * 128],
                 sc_bf, sc_bf.ap[:, kc:kc + 1], start=(kc == 0), stop=(kc == 7))
    modv = P.tile([128, noc], F32, "modv")
    P.op("dve", lambda e: e.tensor_tensor(modv.ap[:], ps_bank.ap[:, 0:noc], vecs.ap[:, adab_col:adab_col + noc], ALU.add),
         [ps_bank, vecs], [modv])
    return modv


def emit_norm(P, x_t, x_ap, N, gm_t, gm_ap, sh_t, sh_ap, out_t, out_ap, ones_bf, ps_ss, sq_t, rt_t, tmp_t, eps_t, out_extra=()):
    for j in range(8):
        P.op("act", (lambda j: lambda e: e.activation(sq_t.ap[:, j, 0:N], x_ap[:, j, :], AF.Square))(j), [x_t], [sq_t])
    for j in range(8):
        P.mm(ps_ss, ps_ss.ap[:, 0:N], ones_bf, ones_bf.ap[:], sq_t, sq_t.ap[:, j, 0:N], start=(j == 0), stop=(j == 7))
    P.op("act", lambda e: e.activation(rt_t.ap[:, 0:N], ps_ss.ap[:, 0:N], AF.Sqrt, bias=eps_t.ap[:, 0:1], scale=1.0 / D),
         [ps_ss, eps_t], [rt_t])
    P.op("dve", lambda e: e.reciprocal(rt_t.ap[:, 0:N], rt_t.ap[:, 0:N]), [rt_t], [rt_t])
    for j in range(8):
        P.op("dve", (lambda j: lambda e: e.scalar_tensor_tensor(tmp_t.ap[:, j, 0:N], x_ap[:, j, :], gm_ap[:, j:j + 1],
                                                                rt_t.ap[:, 0:N], ALU.mult, ALU.mult))(j),
             [x_t, gm_t, rt_t], [tmp_t])
        if sh_t is not None:
            P.op("act", (lambda j: lambda e: e.activation(out_ap[:, j, :], tmp_t.ap[:, j, 0:N], AF.Identity,
                                                          bias=sh_ap[:, j:j + 1], scale=1.0))(j),
                 [tmp_t, sh_t], [out_t] + list(out_extra))
        else:
            P.op("act", (lambda j: lambda e: e.copy(out_ap[:, j, :], tmp_t.ap[:, j, 0:N]))(j), [tmp_t], [out_t])


DEBUG = False


def emit_ffn(P, nc, pbank, xT_v, yT_v, ntok, moe, pre, cT_d):
    E = NE if moe else 1
    ST = 1024
    NV = 8 + 24 + (8 if moe else 0)
    vecs_d = nc.dram_tensor(pre + "vecs", [128, NV], F32, kind="ExternalInput").ap()
    adaw_d = nc.dram_tensor(pre + "adaw", [D, 3 * D], F32, kind="ExternalInput").ap()
    wgu_d = nc.dram_tensor(pre + "wgu", [E, D, 2 * FF], F32, kind="ExternalInput").ap()
    wd_d = nc.dram_tensor(pre + "wd", [E, FF, D], F32, kind="ExternalInput").ap()
    if moe:
        ident_d = nc.dram_tensor(pre + "ident", [128, 128], F32, kind="ExternalInput").ap()
        wr_d = nc.dram_tensor(pre + "wr", [D, NE], F32, kind="ExternalInput").ap()
        br_d = nc.dram_tensor(pre + "br", [128, NE], F32, kind="ExternalInput").ap()

    vecs = P.tile([128, NV], F32, "vecs")
    P.dma("sp", vecs, vecs.ap[:], None, vecs_d)
    ones_bf = P.tile([128, 128], BF16, "ones")
    P.op("dve", lambda e: e.memset(ones_bf.ap[:], 1.0), [], [ones_bf])
    eps_t = P.tile([128, 1], F32, "eps")
    P.op("dve", lambda e: e.memset(eps_t.ap[:], NORM_EPS), [], [eps_t])
    act_h = P.sb([128, FC, ST], BF16, "act")
    act = [T(act_h[:, fc, :], f"act{fc}") for fc in range(FC)]
    banks = [T(pbank[i], f"bank{i}") for i in range(8)]
    aw_view = act_h[:, 0:24, :].rearrange("p a b -> p (a b)").rearrange("p (k n) -> p k n", k=8)
    modv = emit_mod(P, nc, cT_d, adaw_d, 3, vecs, 8, aw_view, banks[7])
    gm = P.tile([128, 8], F32, "gm")
    P.op("dve", lambda e: e.scalar_tensor_tensor(gm.ap[:], modv.ap[:, 8:16], 1.0, vecs.ap[:, 0:8], ALU.add, ALU.mult),
         [modv, vecs], [gm])
    g2p = P.tile([128, 8], F32, "g2p")
    P.op("dve", lambda e: e.tensor_scalar(g2p.ap[:], modv.ap[:, 16:24], 1.0, None, ALU.add), [modv], [g2p])

    x_h = P.sb([128, 8, ST], F32, "x")
    xall = T(x_h, "xall")
    h_bf = P.tile([128, 8, ST], BF16, "h_bf")
    sq_t = P.tile([128, 8, 512], BF16, "sq")
    rt_t = P.tile([128, 512], F32, "rt")
    tmp_t = P.tile([128, 8, 512], F32, "tmp")
    wgu_ring = Ring([P.tile([128, 8, 2, 256], BF16, f"wgu{i}") for i in range(2)])
    wd_ring = Ring([P.tile([128, FC, 128], BF16, f"wd{i}") for i in range(2)])
    sg_ring = Ring([P.tile([128, 512], F32, f"sg{i}") for i in range(2)])
    psg_ring = Ring([banks[0], banks[1]])
    psu_ring = Ring([banks[2], banks[3]])
    pso_ring = Ring([banks[4], banks[5]])
    ps_ss = banks[6]
    if moe:
        ident = P.tile([128, 128], F32, "ident")
        P.dma("sp", ident, ident.ap[:], None, ident_d)
        wr = P.tile([128, 8, NE], F32, "wr")
        P.dma("sp", wr, wr.ap[:], None, wr_d.rearrange("(kc p) e -> p kc e", p=128))
        br = P.tile([128, NE], F32, "br")
        P.dma("sp", br, br.ap[:], None, br_d)
        comb = P.tile([128, ST // 128, NE], F32, "comb")
        lg = P.tile([128, NE], F32, "lg")
        m1 = P.tile([128, 1], F32, "m1")
        m2 = P.tile([128, 1], F32, "m2")
        eq1 = P.tile([128, NE], F32, "eq1")
        lg2 = P.tile([128, NE], F32, "lg2")
        eq2 = P.tile([128, NE], F32, "eq2")
        p1 = P.tile([128, 1], F32, "p1")
        p2 = P.tile([128, 1], F32, "p2")
        rep_ring = Ring([P.tile([128, 128], F32, f"rep{i}") for i in range(2)])
        cbc_ring = Ring([P.tile([128, ST], F32, f"cbc{i}") for i in range(2)])
        tmp2_ring = Ring([P.tile([128, 512], F32, f"tmp2{i}") for i in range(2)])
        ps_misc = banks[7]

    def loads_wgu(e, g2):
        t = wgu_ring.next()
        src = wgu_d[e].rearrange("(kc p) n -> p kc n", p=128)
        P.dma("pool", t, t.ap[:, :, 0, :], None, src[:, :, g2 * 256:(g2 + 1) * 256])
        P.dma("pool", t, t.ap[:, :, 1, :], None, src[:, :, FF + g2 * 256:FF + (g2 + 1) * 256])
        return t

    def load_wd(e, d):
        t = wd_ring.next()
        src = wd_d[e].rearrange("(fc p) n -> p fc n", p=128)
        P.dma("pool", t, t.ap[:], None, src[:, :, d * 128:(d + 1) * 128])
        return t

    if moe:
        hf = T(act_h[:, 0:16, :].rearrange("p a b -> p (a b)").bitcast(F32).rearrange("p (j n) -> p j n", j=8), "hf")
    for st in range(ntok // ST):
        c0 = st * ST
        P.dma("sp", xall, x_h[:], None, xT_v[:, :, c0:c0 + ST])
        for tt in range(ST // 512):
            cs = slice(tt * 512, (tt + 1) * 512)
            if not moe:
                emit_norm(P, xall, x_h[:, :, cs], 512, gm, gm.ap, modv, modv.ap[:, 0:8], h_bf, h_bf.ap[:, :, cs],
                          ones_bf, ps_ss, sq_t, rt_t, tmp_t, eps_t)
            else:
                emit_norm(P, xall, x_h[:, :, cs], 512, gm, gm.ap, modv, modv.ap[:, 0:8], hf, hf.ap[:, :, 0:512],
                          ones_bf, ps_ss, sq_t, rt_t, tmp_t, eps_t, out_extra=act[0:16])
                for j in range(8):
                    P.I("dve", "tensor_copy", [hf], [h_bf], h_bf.ap[:, j, cs], hf.ap[:, j, 0:512])
                for b in range(4):
                    blk = tt * 4 + b
                    for kc in range(8):
                        P.mm(ps_misc, ps_misc.ap[:, 0:NE], hf, hf.ap[:, kc, b * 128:(b + 1) * 128], wr, wr.ap[:, kc, :],
                             start=(kc == 0), stop=(kc == 7))
                    P.op("dve", lambda e: e.tensor_tensor(lg.ap[:], ps_misc.ap[:, 0:NE], br.ap[:], ALU.add), [ps_misc, br], [lg])
                    P.op("dve", lambda e: e.tensor_reduce(m1.ap[:], lg.ap[:], AX.X, ALU.max), [lg], [m1])
                    P.op("dve", lambda e: e.tensor_scalar(eq1.ap[:], lg.ap[:], m1.ap[:, 0:1], None, ALU.is_equal), [lg, m1], [eq1])
                    P.op("dve", lambda e: e.scalar_tensor_tensor(lg2.ap[:], eq1.ap[:], -1e30, lg.ap[:], ALU.mult, ALU.add),
                         [eq1, lg], [lg2])
                    P.op("dve", lambda e: e.tensor_reduce(m2.ap[:], lg2.ap[:], AX.X, ALU.max), [lg2], [m2])
                    P.op("dve", lambda e: e.tensor_scalar(eq2.ap[:], lg2.ap[:], m2.ap[:, 0:1], None, ALU.is_equal), [lg2, m2], [eq2])
                    P.op("dve", lambda e: e.tensor_tensor(p2.ap[:], m1.ap[:], m2.ap[:], ALU.subtract), [m1, m2], [p2])
                    P.op("act", lambda e: e.activation(p1.ap[:], p2.ap[:], AF.Sigmoid), [p2], [p1])
                    P.op("dve", lambda e: e.tensor_scalar(p2.ap[:], p1.ap[:], -1.0, 1.0, ALU.mult, ALU.add), [p1], [p2])
                    P.op("dve", lambda e: e.tensor_scalar(eq1.ap[:], eq1.ap[:], p1.ap[:, 0:1], None, ALU.mult), [eq1, p1], [eq1])
                    P.op("dve", (lambda blk: lambda e: e.scalar_tensor_tensor(comb.ap[:, blk, :], eq2.ap[:], p2.ap[:, 0:1], eq1.ap[:],
                                                                             ALU.mult, ALU.add))(blk), [eq2, p2, eq1], [comb])
        if moe and DEBUG and st == 0:
            dbg_comb = nc.dram_tensor("dbg_comb", [128, ST // 128, NE], F32, kind="ExternalOutput").ap()
            P.dma("sp", None, dbg_comb, comb, comb.ap[:])
            dbg_h = nc.dram_tensor("dbg_h", [128, 8, ST], BF16, kind="ExternalOutput").ap()
            P.dma("sp", None, dbg_h, h_bf, h_bf.ap[:])
        for e_i in range(E):
            if moe:
                cbc = cbc_ring.next()
                for blk in range(ST // 128):
                    rep = rep_ring.next()
                    P.op("dve", (lambda rep, blk, e_i: lambda e: e.tensor_copy(rep.ap[:], comb.ap[:, blk, e_i:e_i + 1].broadcast_to([128, 128])))(rep, blk, e_i),
                         [comb], [rep])
                    half = blk // 4
                    col = (blk % 4) * 128
                    P.mm(ps_misc, ps_misc.ap[:, col:col + 128], rep, rep.ap[:], ident, ident.ap[:])
                    if blk % 4 == 3:
                        P.op("act", (lambda cbc, half: lambda e: e.copy(cbc.ap[:, half * 512:(half + 1) * 512], ps_misc.ap[:]))(cbc, half),
                             [ps_misc], [cbc])
            if moe and DEBUG and st == 0 and e_i == 0:
                dbg_cbc = nc.dram_tensor("dbg_cbc", [128, ST], F32, kind="ExternalOutput").ap()
                P.dma("sp", None, dbg_cbc, cbc, cbc.ap[:])
            for g2 in range(FC // 2):
                wt = loads_wgu(e_i, g2)
                for f in range(2):
                    fc = g2 * 2 + f
                    for tt in range(ST // 512):
                        cs = slice(tt * 512, (tt + 1) * 512)
                        psg = psg_ring.next()
                        psu = psu_ring.next()
                        for kc in range(8):
                            P.mm(psg, psg.ap[:], wt, wt.ap[:, kc, 0, f * 128:(f + 1) * 128], h_bf, h_bf.ap[:, kc, cs],
                                 start=(kc == 0), stop=(kc == 7))
                        for kc in range(8):
                            P.mm(psu, psu.ap[:], wt, wt.ap[:, kc, 1, f * 128:(f + 1) * 128], h_bf, h_bf.ap[:, kc, cs],
                                 start=(kc == 0), stop=(kc == 7))
                        sg = sg_ring.next()
                        P.op("act", (lambda sg, psg: lambda e: e.activation(sg.ap[:], psg.ap[:], AF.Silu))(sg, psg), [psg], [sg])
                        if not moe:
                            P.op("dve", (lambda sg, psu, fc, cs: lambda e: e.tensor_tensor(act_h[:, fc, cs], sg.ap[:], psu.ap[:], ALU.mult))(sg, psu, fc, cs),
                                 [sg, psu], [act[fc]])
                        else:
                            t2 = tmp2_ring.next()
                            P.op("dve", (lambda sg, psu, t2: lambda e: e.tensor_tensor(t2.ap[:], sg.ap[:], psu.ap[:], ALU.mult))(sg, psu, t2),
                                 [sg, psu], [t2])
                            P.op("dve", (lambda t2, cbc, fc, cs: lambda e: e.tensor_tensor(act_h[:, fc, cs], t2.ap[:], cbc.ap[:, cs], ALU.mult))(t2, cbc, fc, cs),
                                 [t2, cbc], [act[fc]])
            for d in range(8):
                wt = load_wd(e_i, d)
                for tt in range(ST // 512):
                    cs = slice(tt * 512, (tt + 1) * 512)
                    pso = pso_ring.next()
                    for fc in range(FC):
                        P.mm(pso, pso.ap[:], wt, wt.ap[:, fc, :], act[fc], act_h[:, fc, cs], start=(fc == 0), stop=(fc == FC - 1))
                    P.op("dve", (lambda pso, d, cs: lambda e: e.scalar_tensor_tensor(x_h[:, d, cs], pso.ap[:], g2p.ap[:, d:d + 1], x_h[:, d, cs],
                                                                                    ALU.mult, ALU.add))(pso, d, cs),
                         [pso, g2p, xall], [xall])
        if moe:
            fg_col = 32
            for tt in range(ST // 512):
                cs = slice(tt * 512, (tt + 1) * 512)
                emit_final(P, xall, x_h[:, :, cs], vecs, fg_col, ones_bf, ps_ss, sq_t, rt_t, tmp_t, eps_t)
                P.dma("sp", None, yT_v[:, :, c0 + tt * 512:c0 + (tt + 1) * 512], tmp_t, tmp_t.ap[:])
        else:
            P.dma("sp", None, yT_v[:, :, c0:c0 + ST], xall, x_h[:])


def emit_final(P, x_t, x_ap, vecs, fg_col, ones_bf, ps_ss, sq_t, rt_t, tmp_t, eps_t):
    N = 512
    for j in range(8):
        P.op("act", (lambda j: lambda e: e.activation(sq_t.ap[:, j, :], x_ap[:, j, :], AF.Square))(j), [x_t], [sq_t])
    for j in range(8):
        P.mm(ps_ss, ps_ss.ap[:], ones_bf, ones_bf.ap[:], sq_t, sq_t.ap[:, j, :], start=(j == 0), stop=(j == 7))
    P.op("act", lambda e: e.activation(rt_t.ap[:], ps_ss.ap[:], AF.Sqrt, bias=eps_t.ap[:, 0:1], scale=1.0 / D), [ps_ss, eps_t], [rt_t])
    P.op("dve", lambda e: e.reciprocal(rt_t.ap[:], rt_t.ap[:]), [rt_t], [rt_t])
    for j in range(8):
        P.op("dve", (lambda j: lambda e: e.scalar_tensor_tensor(tmp_t.ap[:, j, :], x_ap[:, j, :], vecs.ap[:, fg_col + j:fg_col + j + 1],
                                                                rt_t.ap[:], ALU.mult, ALU.mult))(j),
             [x_t, vecs, rt_t], [tmp_t])


LRU_C = 8.0
LRU_VEC_NAMES = ["ng", "adab", "cw0", "cw1", "cw2", "cw3", "cb", "bgr", "bgi", "lam", "flag"]


def emit_lru(P, nc, pbank, xT_v, yT_v, pre, cT_d):
    NT = 256
    VT = 2 * NTOK
    NV = 8 + 24 + 32 + 8 + 8 + 8 + 8 + 1
    vecs_d = nc.dram_tensor(pre + "vecs", [128, NV], F32, kind="ExternalInput").ap()
    adaw_d = nc.dram_tensor(pre + "adaw", [D, 3 * D], F32, kind="ExternalInput").ap()
    win_d = nc.dram_tensor(pre + "win", [D, 2 * D], F32, kind="ExternalInput").ap()
    wg_d = nc.dram_tensor(pre + "wg", [4, 256, 512], F32, kind="ExternalInput").ap()
    wout_d = nc.dram_tensor(pre + "wout", [D, D], F32, kind="ExternalInput").ap()
    C_NG, C_AB, C_CW, C_CB, C_BGR, C_BGI, C_LAM, C_FLAG = 0, 8, 32, 64, 72, 80, 88, 96

    vecs = P.tile([128, NV], F32, "vecs")
    P.dma("sp", vecs, vecs.ap[:], None, vecs_d)
    ones_bf = P.tile([128, 128], BF16, "ones")
    P.I("dve", "memset", [], [ones_bf], ones_bf.ap[:], 1.0)
    eps_t = P.tile([128, 1], F32, "eps")
    P.I("dve", "memset", [], [eps_t], eps_t.ap[:], NORM_EPS)
    banks = [T(pbank[i], f"bank{i}") for i in range(8)]
    scratch = P.sb([128, 8, 3 * D], BF16, "adaw_sb")
    modv = emit_mod(P, nc, cT_d, adaw_d, 3, vecs, C_AB, scratch, banks[7])
    gm = P.tile([128, 8], F32, "gm")
    P.I("dve", "scalar_tensor_tensor", [modv, vecs], [gm], gm.ap[:], modv.ap[:, 8:16], 1.0, vecs.ap[:, C_NG:C_NG + 8], ALU.add, ALU.mult)
    g1p = P.tile([128, 8], F32, "g1p")
    P.I("dve", "tensor_scalar", [modv], [g1p], g1p.ap[:], modv.ap[:, 16:24], 1.0, None, ALU.add)
    cj = P.tile([128, 8], F32, "cj")
    P.I("act", "activation", [vecs], [cj], cj.ap[:], vecs.ap[:, C_LAM:C_LAM + 8], AF.Exp, scale=-1.0)
    P.I("dve", "tensor_scalar", [cj], [cj], cj.ap[:], cj.ap[:], 1.0, None, ALU.add)
    P.I("act", "activation", [cj], [cj], cj.ap[:], cj.ap[:], AF.Ln)
    P.I("dve", "tensor_scalar", [cj], [cj], cj.ap[:], cj.ap[:], -LRU_C, None, ALU.mult)

    win = T(scratch[:, :, 0:2 * D], "win")
    wout = T(scratch[:, :, 2 * D:3 * D], "wout")
    wg = P.tile([128, 4, 2, 512], BF16, "wg")
    dummy = P.tile([128, 1], F32, "dummy")
    P.I("pool", "tensor_copy", [modv], [win, wout, dummy], dummy.ap[:], modv.ap[:, 0:1])
    src = win_d.rearrange("(kc p) n -> p kc n", p=128)
    for v in range(2):
        P.dma("pool", win, scratch[:, :, v * D:(v + 1) * D], None, src[:, :, v * D:(v + 1) * D])
    P.dma("pool", wout, scratch[:, :, 2 * D:3 * D], None, wout_d.rearrange("(kc p) n -> p kc n", p=128))
    for n in range(4):
        P.dma("pool", wg, wg.ap[:, n, :, :], None, wg_d[n].rearrange("(kc p) n -> p kc n", p=128))

    x_t = P.tile([128, 8, NT], F32, "x")
    h_bf = P.tile([128, 8, NT], BF16, "h_bf")
    sq_t = P.tile([128, 8, NT], BF16, "sq")
    rt_t = P.tile([128, NT], F32, "rt")
    tmp_t = P.tile([128, 8, NT], F32, "tmp")
    xb_sb = P.tile([128, 8, NT + 3], F32, "xb_sb")
    gate = P.tile([128, 8, NT], F32, "gate")
    xbc = P.tile([128, 8, NT], F32, "xbc")
    xbc_bf = P.tile([128, 8, NT], BF16, "xbc_bf")
    hg = P.tile([128, 8, NT], BF16, "hg")
    hprev = P.tile([128, 8], F32, "hprev")
    a_all = P.tile([128, 8, NT], F32, "a_all")
    i_all = P.tile([128, 8, NT], F32, "i_all")
    b_all = P.tile([128, 8, NT], F32, "b_all")
    xo = P.tile([128, 8, NT], F32, "xo")
    w_ring = Ring([P.tile([128, NT], F32, f"wk{i}") for i in range(10)])
    xb_ring = Ring([banks[0], banks[1]])
    gb_ring = Ring([banks[2], banks[3]])
    r_ring = Ring([banks[4], banks[0]])
    i_ring = Ring([banks[5], banks[1]])
    ps_ss = banks[6]
    o_ring = Ring([banks[6], banks[7]])

    P.I("dve", "memset", [], [xb_sb], xb_sb.ap[:, :, NT:NT + 3], 0.0)
    P.I("dve", "memset", [], [hprev], hprev.ap[:], 0.0)
    fl = vecs.ap[:, C_FLAG:C_FLAG + 1]

    for tt in range(VT // NT):
        c0 = tt * NT
        own = tt >= (NTOK // NT)
        P.dma("sp", x_t, x_t.ap[:], None, xT_v[:, :, c0:c0 + NT])
        emit_norm(P, x_t, x_t.ap[:], NT, gm, gm.ap, modv, modv.ap[:, 0:8], h_bf, h_bf.ap[:], ones_bf, ps_ss, sq_t, rt_t, tmp_t, eps_t)
        if tt == NTOK // NT:
            P.I("dve", "tensor_scalar", [xb_sb, vecs], [xb_sb], xb_sb.ap[:, :, 0:3], xb_sb.ap[:, :, NT:NT + 3], fl, None, ALU.mult)
            P.I("dve", "tensor_scalar", [hprev, vecs], [hprev], hprev.ap[:], hprev.ap[:], fl, None, ALU.mult)
        else:
            P.I("dve", "tensor_copy", [xb_sb], [xb_sb], xb_sb.ap[:, :, 0:3], xb_sb.ap[:, :, NT:NT + 3])
        for j in range(8):
            xb_ps = xb_ring.next()
            gb_ps = gb_ring.next() if own else None
            for kc in range(8):
                P.mm(xb_ps, xb_ps.ap[:, 0:NT], win, scratch[:, kc, j * 128:(j + 1) * 128], h_bf, h_bf.ap[:, kc, :], start=(kc == 0), stop=(kc == 7))
            for kc in range(8 if own else 0):
                P.mm(gb_ps, gb_ps.ap[:, 0:NT], win, scratch[:, kc, D + j * 128:D + (j + 1) * 128], h_bf, h_bf.ap[:, kc, :], start=(kc == 0), stop=(kc == 7))
            P.I("act", "copy", [xb_ps], [xb_sb], xb_sb.ap[:, j, 3:NT + 3], xb_ps.ap[:, 0:NT])
            if own:
                t1 = w_ring.next()
                P.I("act", "activation", [gb_ps], [t1], t1.ap[:], gb_ps.ap[:, 0:NT], AF.Square)
                P.I("dve", "tensor_scalar", [t1], [t1], t1.ap[:], t1.ap[:], 0.044715, 1.0, ALU.mult, ALU.add)
                P.I("dve", "tensor_tensor", [t1, gb_ps], [t1], t1.ap[:], t1.ap[:], gb_ps.ap[:, 0:NT], ALU.mult)
                P.I("act", "activation", [t1], [t1], t1.ap[:], t1.ap[:], AF.Sigmoid, scale=1.5957691216057308)
                P.I("dve", "tensor_tensor", [t1, gb_ps], [gate], gate.ap[:, j, :], t1.ap[:], gb_ps.ap[:, 0:NT], ALU.mult)
            P.I("act", "activation", [xb_sb, vecs], [xbc], xbc.ap[:, j, :], xb_sb.ap[:, j, 3:NT + 3], AF.Identity,
                bias=vecs.ap[:, C_CB + j:C_CB + j + 1], scale=vecs.ap[:, C_CW + 24 + j:C_CW + 24 + j + 1])
            for i in (2, 1, 0):
                P.I("dve", "scalar_tensor_tensor", [xb_sb, vecs, xbc], [xbc], xbc.ap[:, j, :], xb_sb.ap[:, j, i:NT + i],
                    vecs.ap[:, C_CW + i * 8 + j:C_CW + i * 8 + j + 1], xbc.ap[:, j, :], ALU.mult, ALU.add)
            P.I("act", "copy", [xbc], [xbc_bf], xbc_bf.ap[:, j, :], xbc.ap[:, j, :])
        for n in range(4):
            for oc in range(2):
                j = 2 * n + oc
                r_ps = r_ring.next()
                i_ps = i_ring.next()
                for kc in range(2):
                    P.mm(r_ps, r_ps.ap[:, 0:NT], wg, wg.ap[:, n, kc, oc * 128:(oc + 1) * 128], xbc_bf, xbc_bf.ap[:, 2 * n + kc, :],
                         start=(kc == 0), stop=(kc == 1))
                for kc in range(2):
                    P.mm(i_ps, i_ps.ap[:, 0:NT], wg, wg.ap[:, n, kc, 256 + oc * 128:256 + (oc + 1) * 128], xbc_bf, xbc_bf.ap[:, 2 * n + kc, :],
                         start=(kc == 0), stop=(kc == 1))
                P.I("act", "activation", [r_ps, vecs], [a_all], a_all.ap[:, j, :], r_ps.ap[:, 0:NT], AF.Sigmoid, bias=vecs.ap[:, C_BGR + j:C_BGR + j + 1])
                P.I("act", "activation", [i_ps, vecs], [i_all], i_all.ap[:, j, :], i_ps.ap[:, 0:NT], AF.Sigmoid, bias=vecs.ap[:, C_BGI + j:C_BGI + j + 1])
        for j in range(8):
            P.I("act", "activation", [a_all, cj], [a_all], a_all.ap[:, j, :], a_all.ap[:, j, :], AF.Exp, scale=cj.ap[:, j:j + 1])
            P.I("dve", "tensor_tensor", [a_all], [b_all], b_all.ap[:, j, :], a_all.ap[:, j, :], a_all.ap[:, j, :], ALU.mult)
            P.I("dve", "tensor_scalar", [b_all], [b_all], b_all.ap[:, j, :], b_all.ap[:, j, :], -1.0, 1.0, ALU.mult, ALU.add)
            P.I("dve", "tensor_tensor", [i_all, xbc], [i_all], i_all.ap[:, j, :], i_all.ap[:, j, :], xbc.ap[:, j, :], ALU.mult)
        for j in range(8):
            P.I("act", "activation", [b_all], [b_all], b_all.ap[:, j, :], b_all.ap[:, j, :], AF.Sqrt)
            P.I("dve", "tensor_tensor", [b_all, i_all], [b_all], b_all.ap[:, j, :], b_all.ap[:, j, :], i_all.ap[:, j, :], ALU.mult)
            hs = w_ring.next()
            P.I("dve", "tensor_tensor_scan", [a_all, b_all, hprev], [hs], hs.ap[:], a_all.ap[:, j, :], b_all.ap[:, j, :], hprev.ap[:, j:j + 1], ALU.mult, ALU.add)
            P.I("dve", "tensor_copy", [hs], [hprev], hprev.ap[:, j:j + 1], hs.ap[:, NT - 1:NT])
            if own:
                P.I("dve", "tensor_tensor", [hs, gate], [hg], hg.ap[:, j, :], hs.ap[:], gate.ap[:, j, :], ALU.mult)
        for jo in range(8 if own else 0):
            ps = o_ring.next()
            for kc in range(8):
                P.mm(ps, ps.ap[:, 0:NT], wout, scratch[:, kc, 2 * D + jo * 128:2 * D + (jo + 1) * 128], hg, hg.ap[:, kc, :], start=(kc == 0), stop=(kc == 7))
            P.I("dve", "scalar_tensor_tensor", [ps, g1p, x_t], [xo], xo.ap[:, jo, :], ps.ap[:, 0:NT], g1p.ap[:, jo:jo + 1], x_t.ap[:, jo, :], ALU.mult, ALU.add)
        if own:
            P.dma("sp", None, yT_v[:, :, c0 - NTOK:c0 - NTOK + NT], xo, xo.ap[:])


GN_EPS = 64e-5
EXPM05 = 0.6065306597126334


def rwkv_consts():
    p = np.arange(128)[:, None]
    q = np.arange(64)[None, :]
    ms = ((p % 64) < q).astype(np.float32)
    mi = ((p % 64) <= q).astype(np.float32)
    mt = (q < (p % 64)).astype(np.float32)
    iq = ((p % 64) == q).astype(np.float32)
    pp = np.arange(128)[None, :]
    bo = ((p // 64) == (pp // 64)).astype(np.float32)
    idn = np.eye(128, dtype=np.float32)
    t = np.arange(256)[None, :]
    mc = np.broadcast_to(((t % 64) != 0).astype(np.float32), (128, 256))
    return np.ascontiguousarray(np.concatenate([ms, mi, mt, iq, bo, bo / 64.0, idn, mc], axis=1))


RW_VECS = ["ng", "adab", "mu", "w0", "a0", "kk", "ka", "rk", "gnw", "gnb", "flag"]


class StopBuild(Exception):
    pass


RW_STOP = None
RW_DUMPS = []


def emit_rwkv(P, nc, pbank, xT_v, yT_v, pre, cT_d):
    def ckpt(name, dumps):
        return
    VT = 2 * NTOK
    NT = 256
    C = NT // 64
    NV = 8 + 24 + 48 + 8 * 7 + 1
    vecs_d = nc.dram_tensor(pre + "vecs", [128, NV], F32, kind="ExternalInput").ap()
    NCON = 64 * 4 + 128 * 3 + 256
    con_d = nc.dram_tensor(pre + "consts", [128, NCON], F32, kind="ExternalInput").ap()
    adaw_d = nc.dram_tensor(pre + "adaw", [D, 3 * D], F32, kind="ExternalInput").ap()
    wrkv_d = nc.dram_tensor(pre + "wrkv", [3, D, D], F32, kind="ExternalInput").ap()
    wo_d = nc.dram_tensor(pre + "wo", [D, D], F32, kind="ExternalInput").ap()
    w1_d = nc.dram_tensor(pre + "w1", [D, 64], F32, kind="ExternalInput").ap()
    w2_d = nc.dram_tensor(pre + "w2", [64, D], F32, kind="ExternalInput").ap()
    a1_d = nc.dram_tensor(pre + "a1", [D, 64], F32, kind="ExternalInput").ap()
    a2_d = nc.dram_tensor(pre + "a2", [64, D], F32, kind="ExternalInput").ap()
    g1_d = nc.dram_tensor(pre + "g1", [D, 160], F32, kind="ExternalInput").ap()
    g2_d = nc.dram_tensor(pre + "g2", [160, D], F32, kind="ExternalInput").ap()
    C_NG, C_AB, C_MU, C_W0, C_A0, C_KK, C_KA, C_RK, C_GNW, C_GNB, C_FLAG = 0, 8, 32, 80, 88, 96, 104, 112, 120, 128, 136

    vecs = P.tile([128, NV], F32, "vecs")
    P.dma("sp", vecs, vecs.ap[:], None, vecs_d)
    con = P.tile([128, NCON], F32, "con")
    P.dma("sp", con, con.ap[:], None, con_d)
    MS, MI, MT, IQ = (con.ap[:, 64 * i:64 * (i + 1)] for i in range(4))
    BO = con.ap[:, 256:384]
    BO64 = con.ap[:, 384:512]
    IDN = con.ap[:, 512:640]
    MC = con.ap[:, 640:896]
    conb = P.tile([128, 384], BF16, "conb")
    P.I("dve", "tensor_copy", [con], [conb], conb.ap[:, 0:128], BO)
    P.I("dve", "tensor_copy", [con], [conb], conb.ap[:, 128:256], IDN)
    P.I("dve", "memset", [], [conb], conb.ap[:, 256:384], 1.0)
    BO_bf = conb.ap[:, 0:128]
    ID_bf = conb.ap[:, 128:256]
    ones_bf = T(conb.ap[:, 256:384], "ones_v")
    ones_bf.w = conb.w
    eps_t = P.tile([128, 3], F32, "eps")
    P.I("dve", "memset", [], [eps_t], eps_t.ap[:, 0:1], NORM_EPS)
    P.I("dve", "memset", [], [eps_t], eps_t.ap[:, 1:2], 1e-24)
    P.I("dve", "memset", [], [eps_t], eps_t.ap[:, 2:3], GN_EPS)


    PB = [T(pbank[i], f"PB{i}") for i in range(8)]

    def reg(i, name):
        return TV(PB[i // 2], pbank[i // 2][:, 256 * (i % 2):256 * (i % 2 + 1)], name)
    R_r, R_k, R_v, R_zw, R_za, R_g, R_st, R_st2 = (reg(i, f"R{i}") for i in range(8))
    B6 = TV(PB[6], pbank[6][:, :], "B6")
    B7 = TV(PB[7], pbank[7][:, :], "B7")

    scratch = P.sb([128, 8, 3 * D], BF16, "wrkv_sb")
    modv = emit_mod(P, nc, cT_d, adaw_d, 3, vecs, C_AB, scratch, R_g)
    gm = P.tile([128, 8], F32, "gm")
    P.I("dve", "scalar_tensor_tensor", [modv, vecs], [gm], gm.ap[:], modv.ap[:, 8:16], 1.0, vecs.ap[:, C_NG:C_NG + 8], ALU.add, ALU.mult)
    g1p = P.tile([128, 8], F32, "g1p")
    P.I("dve", "tensor_scalar", [modv], [g1p], g1p.ap[:], modv.ap[:, 16:24], 1.0, None, ALU.add)
    omka = P.tile([128, 8], F32, "omka")
    P.I("dve", "tensor_scalar", [vecs], [omka], omka.ap[:], vecs.ap[:, C_KA:C_KA + 8], -1.0, 1.0, ALU.mult, ALU.add)

    wrkv = T(scratch, "wrkv")
    dummy = P.tile([128, 1], F32, "dummy")
    P.I("pool", "tensor_copy", [modv], [wrkv, dummy], dummy.ap[:], modv.ap[:, 0:1])
    for p_ in range(3):
        P.dma("pool", wrkv, scratch[:, :, p_ * D:(p_ + 1) * D], None, wrkv_d[p_].rearrange("(kc p) n -> p kc n", p=128))
    wo = P.tile([128, 8, D], BF16, "wo")
    P.dma("pool", wo, wo.ap[:], None, wo_d.rearrange("(kc p) n -> p kc n", p=128))
    w1 = P.tile([128, 8, 64], BF16, "w1")
    P.dma("pool", w1, w1.ap[:], None, w1_d.rearrange("(kc p) n -> p kc n", p=128))
    a1 = P.tile([128, 8, 64], BF16, "a1")
    P.dma("pool", a1, a1.ap[:], None, a1_d.rearrange("(kc p) n -> p kc n", p=128))
    g1 = P.tile([128, 8, 256], BF16, "g1")
    P.I("pool", "memset", [], [g1], g1.ap[:], 0.0)
    P.dma("pool", g1, g1.ap[:, :, 0:160], None, g1_d.rearrange("(kc p) n -> p kc n", p=128))
    w2 = P.tile([64, D], BF16, "w2")
    P.dma("pool", w2, w2.ap[:], None, w2_d)
    a2 = P.tile([64, D], BF16, "a2")
    P.dma("pool", a2, a2.ap[:], None, a2_d)
    g2a = P.tile([128, D], BF16, "g2a")
    P.dma("pool", g2a, g2a.ap[:], None, g2_d[0:128, :])
    g2b = P.tile([128, D], BF16, "g2b")
    P.I("pool", "memset", [], [g2b], g2b.ap[:], 0.0)
    P.dma("pool", g2b, g2b.ap[0:32, :], None, g2_d[128:160, :])

    ckpt("setup", [("gm", gm, gm.ap[:], [128, 8], F32)])
    x_t = P.tile([128, 8, NT], F32, "x")
    h_t = P.tile([128, 8, NT + 1], F32, "h")
    sq_t = P.tile([128, 8, NT], BF16, "sq")
    rt_t = P.tile([128, NT], F32, "rt")
    tmp_t = P.tile([128, 8, NT], F32, "tmp")
    xm = [P.tile([128, 8, NT], BF16, f"xm{p_}") for p_ in range(6)]
    lw1 = P.tile([64, NT], BF16, "lw1")
    la1 = P.tile([64, NT], BF16, "la1")
    lg1a = P.tile([128, NT], BF16, "lg1a")
    lg1b = P.tile([128, NT], BF16, "lg1b")
    yo = P.tile([128, 8, NT], BF16, "yo")
    F = {}
    for nm in ["lw", "cum", "eg", "egm", "eneg", "a", "k", "kk", "rn", "ka", "fac", "km", "v", "bonus", "g", "Y", "yc", "sq2", "rs"]:
        F[nm] = P.tile([128, NT], F32, "f_" + nm)
    kk2 = P.tile([128, NT], BF16, "kk2")
    rkb = P.tile([128, NT], BF16, "rkb")
    AR = P.tile([128, C, 128], BF16, "AR")
    BKr = P.tile([128, C, 128], BF16, "BKr")
    bd_names = ["A_bd", "B_bd", "K_bd", "Bb_bd", "Kb_bd", "V_bd", "N_bd", "NT_bd", "T_bd", "AhT_bd", "M1T_bd", "P_bd", "W2T_bd", "TV_bd"]
    BD = {}
    for nm in bd_names:
        BD[nm] = P.tile([128, C, 128], BF16, nm)
        P.I("pool", "memset", [], [BD[nm]], BD[nm].ap[:], 0.0)
    Xt = P.tile([128, C, 128], BF16, "Xt")
    RH = {}
    for nm in ["TBr", "TKr", "TVr", "Nr", "NTr", "Tr", "Arb", "Ark", "Rhat", "M2"]:
        RH[nm] = P.tile([128, C, 64], BF16, nm)
    diagG = P.tile([128, C, 64], F32, "diagG")
    tmpP = P.tile([128, C, 64], BF16, "tmpP")
    tmpW = P.tile([128, C, 64], BF16, "tmpW")
    St_r = [P.tile([128, 64], BF16, f"St_r{j}") for j in range(8)]
    St_bd = [P.tile([128, 128], BF16, f"St_bd{j}") for j in range(8)]
    HS = (slice(0, 64), slice(64, 128))

    def halves(eng, method, reads, writes, out_fn, in_fns, *extra):
        for hh in range(2):
            hs = HS[hh]
            e_, m_ = eng, method
            if eng == "dve" and method == "tensor_copy" and hh == 1:
                e_, m_ = "act", "copy"
            P.I(e_, m_, reads, writes, out_fn(hs, hh), *[f(hs, hh) for f in in_fns], *extra)

    for j in range(8):
        P.I("pool", "memset", [], [St_bd[j]], St_bd[j].ap[:], 0.0)
        P.I("pool", "memset", [], [St_r[j]], St_r[j].ap[:], 0.0)
    P.I("dve", "memset", [], [h_t], h_t.ap[:, :, NT:NT + 1], 0.0)
    fl = vecs.ap[:, C_FLAG:C_FLAG + 1]

    v3 = lambda ap: ap.rearrange("p (c n) -> p c n", c=C)
    ckpt("init", [("hfull", h_t, h_t.ap[:], [128, 8, NT + 1], F32), ("Stbd0", St_bd[0], St_bd[0].ap[:], [128, 128], BF16)])

    for tt in range(VT // NT):
        c0 = tt * NT
        P.dma("sp", x_t, x_t.ap[:], None, xT_v[:, :, c0:c0 + NT])
        if tt == NTOK // NT:
            P.I("dve", "tensor_scalar", [h_t, vecs], [h_t], h_t.ap[:, :, 0:1], h_t.ap[:, :, NT:NT + 1], fl, None, ALU.mult)
            for j in range(8):
                P.I("dve", "tensor_scalar", [St_r[j], vecs], [St_r[j]], St_r[j].ap[:], St_r[j].ap[:], fl, None, ALU.mult)
                P.I("dve", "tensor_scalar", [St_bd[j], vecs], [St_bd[j]], St_bd[j].ap[:], St_bd[j].ap[:], fl, None, ALU.mult)
        else:
            P.I("dve", "tensor_copy", [h_t], [h_t], h_t.ap[:, :, 0:1], h_t.ap[:, :, NT:NT + 1])
        emit_norm(P, x_t, x_t.ap[:], NT, gm, gm.ap, modv, modv.ap[:, 0:8], h_t, h_t.ap[:, :, 1:NT + 1], ones_bf, R_st, sq_t, rt_t, tmp_t, eps_t)
        P.I("dve", "tensor_tensor", [h_t], [tmp_t], tmp_t.ap[:], h_t.ap[:, :, 0:NT], h_t.ap[:, :, 1:NT + 1], ALU.subtract)
        ckpt("norm", [("hfull", h_t, h_t.ap[:], [128, 8, NT + 1], F32), ("xx", tmp_t, tmp_t.ap[:], [128, 8, NT], F32)])
        for p_ in range(6):
            for j in range(8):
                eng = "dve"
                P.I(eng, "scalar_tensor_tensor", [tmp_t, vecs, h_t], [xm[p_]], xm[p_].ap[:, j, :], tmp_t.ap[:, j, :],
                    vecs.ap[:, C_MU + p_ * 8 + j:C_MU + p_ * 8 + j + 1], h_t.ap[:, j, 1:NT + 1], ALU.mult, ALU.add)
        ckpt("xm", [("xm0", xm[0], xm[0].ap[:], [128, 8, NT], BF16), ("xm5", xm[5], xm[5].ap[:], [128, 8, NT], BF16)])
        for kc in range(8):
            P.mm(R_r, pbank[0][0:64, 0:NT], w1, w1.ap[:, kc, :], xm[3], xm[3].ap[:, kc, :], start=(kc == 0), stop=(kc == 7))
        ckpt("l0", [("gm", gm, gm.ap[:], [128, 8], F32)])
        P.I("act", "activation", [R_r], [lw1], lw1.ap[:], pbank[0][0:64, 0:NT], AF.Tanh)
        ckpt("l1", [("gm", gm, gm.ap[:], [128, 8], F32)])
        for kc in range(8):
            P.mm(R_k, pbank[0][0:64, 256:256 + NT], a1, a1.ap[:, kc, :], xm[4], xm[4].ap[:, kc, :], start=(kc == 0), stop=(kc == 7))
        P.I("act", "copy", [R_k], [la1], la1.ap[:], pbank[0][0:64, 256:256 + NT])
        ckpt("l2", [("gm", gm, gm.ap[:], [128, 8], F32)])
        for kc in range(8):
            P.mm(R_v, R_v.ap[:], g1, g1.ap[:, kc, 0:128], xm[5], xm[5].ap[:, kc, :], start=(kc == 0), stop=(kc == 7))
        P.I("act", "activation", [R_v], [lg1a], lg1a.ap[:], R_v.ap[:], AF.Sigmoid)
        ckpt("l3", [("gm", gm, gm.ap[:], [128, 8], F32)])
        for kc in range(8):
            P.mm(R_zw, R_zw.ap[:], g1, g1.ap[:, kc, 128:256], xm[5], xm[5].ap[:, kc, :], start=(kc == 0), stop=(kc == 7))
        P.I("act", "activation", [R_zw], [lg1b], lg1b.ap[:], R_zw.ap[:], AF.Sigmoid)

        ckpt("lora", [("gm", gm, gm.ap[:], [128, 8], F32)])
        for j in range(8):
            js = slice(j * 128, (j + 1) * 128)
            vj = lambda col: vecs.ap[:, col + j:col + j + 1]
            for (R_, p_) in ((R_r, 0), (R_k, 1), (R_v, 2)):
                for kc in range(8):
                    P.mm(R_, R_.ap[:], wrkv, scratch[:, kc, p_ * D + j * 128:p_ * D + (j + 1) * 128], xm[p_], xm[p_].ap[:, kc, :],
                         start=(kc == 0), stop=(kc == 7))
            P.mm(R_zw, R_zw.ap[:], w2, w2.ap[:, js], lw1, lw1.ap[:])
            P.mm(R_za, R_za.ap[:], a2, a2.ap[:, js], la1, la1.ap[:])
            P.mm(R_g, R_g.ap[:], g2a, g2a.ap[:, js], lg1a, lg1a.ap[:], start=True, stop=False)
            P.mm(R_g, R_g.ap[:], g2b, g2b.ap[:, js], lg1b, lg1b.ap[:], start=False, stop=True)
            P.I("act", "activation", [R_zw, vecs], [F["lw"]], F["lw"].ap[:], R_zw.ap[:], AF.Sigmoid, bias=vj(C_W0))
            P.I("act", "activation", [R_za, vecs], [F["a"]], F["a"].ap[:], R_za.ap[:], AF.Sigmoid, bias=vj(C_A0))
            P.I("act", "copy", [R_k], [F["k"]], F["k"].ap[:], R_k.ap[:])
            P.I("act", "activation", [R_k, vecs], [kk2], kk2.ap[:], R_k.ap[:], AF.Square, scale=vj(C_KK))
            P.mm(R_st, R_st.ap[:], conb, BO_bf, kk2, kk2.ap[:])
            P.I("dve", "tensor_scalar", [F["lw"]], [F["lw"]], F["lw"].ap[:], F["lw"].ap[:], -EXPM05, None, ALU.mult)
            P.I("dve", "tensor_tensor_scan", [con, F["lw"]], [F["cum"]], F["cum"].ap[:], MC, F["lw"].ap[:], 0.0, ALU.mult, ALU.add)
            P.I("dve", "tensor_tensor", [F["cum"], F["lw"]], [F["egm"]], F["egm"].ap[:], F["cum"].ap[:], F["lw"].ap[:], ALU.subtract)
            P.I("dve", "tensor_scalar", [F["k"], vecs], [F["kk"]], F["kk"].ap[:], F["k"].ap[:], vj(C_KK), None, ALU.mult)
            P.I("act", "activation", [F["cum"]], [F["eg"]], F["eg"].ap[:], F["cum"].ap[:], AF.Exp)
            P.I("act", "activation", [F["cum"]], [F["eneg"]], F["eneg"].ap[:], F["cum"].ap[:], AF.Exp, scale=-1.0)
            P.I("act", "activation", [F["egm"]], [F["egm"]], F["egm"].ap[:], F["egm"].ap[:], AF.Exp)
            P.I("act", "activation", [R_st, eps_t], [F["rn"]], F["rn"].ap[:], R_st.ap[:], AF.Ln, bias=eps_t.ap[:, 1:2])
            P.I("act", "activation", [F["rn"]], [F["rn"]], F["rn"].ap[:], F["rn"].ap[:], AF.Exp, scale=-0.5)
            gL = v3(F["eg"].ap[:])[:, :, 63:64]
            gLb = gL.broadcast_to([128, C, 64])
            P.I("dve", "tensor_tensor", [F["kk"], F["rn"]], [F["kk"]], F["kk"].ap[:], F["kk"].ap[:], F["rn"].ap[:], ALU.mult)
            P.I("dve", "scalar_tensor_tensor", [F["kk"], F["egm"]], [AR], AR.ap[:, :, 0:64], v3(F["kk"].ap[:]), -1.0, v3(F["egm"].ap[:]), ALU.mult, ALU.mult)
            P.I("dve", "tensor_tensor", [R_r, F["eg"]], [AR], AR.ap[:, :, 64:128], v3(R_r.ap[:]), v3(F["eg"].ap[:]), ALU.mult)
            halves("dve", "tensor_copy", [AR], [BD["A_bd"]], lambda hs, hh: BD["A_bd"].ap[hs, :, 64 * hh:64 * hh + 64], [lambda hs, hh: AR.ap[hs, :, 0:64]])
            P.I("dve", "tensor_tensor", [F["kk"], F["a"]], [F["ka"]], F["ka"].ap[:], F["kk"].ap[:], F["a"].ap[:], ALU.mult)
            P.I("dve", "tensor_tensor", [F["ka"], F["eneg"]], [BKr], BKr.ap[:, :, 0:64], v3(F["ka"].ap[:]), v3(F["eneg"].ap[:]), ALU.mult)
            halves("dve", "tensor_copy", [BKr], [BD["B_bd"]], lambda hs, hh: BD["B_bd"].ap[hs, :, 64 * hh:64 * hh + 64], [lambda hs, hh: BKr.ap[hs, :, 0:64]])
            halves("dve", "tensor_tensor", [BKr, F["eg"]], [BD["Bb_bd"]], lambda hs, hh: BD["Bb_bd"].ap[hs, :, 64 * hh:64 * hh + 64],
                   [lambda hs, hh: BKr.ap[hs, :, 0:64], lambda hs, hh: gLb[hs]], ALU.mult)
            P.I("dve", "tensor_scalar", [F["a"], vecs, omka], [F["fac"]], F["fac"].ap[:], F["a"].ap[:], vj(C_KA), omka.ap[:, j:j + 1], ALU.mult, ALU.add)
            P.I("dve", "tensor_tensor", [F["k"], F["fac"]], [F["km"]], F["km"].ap[:], F["k"].ap[:], F["fac"].ap[:], ALU.mult)
            P.I("dve", "tensor_tensor", [F["km"], F["eneg"]], [BKr], BKr.ap[:, :, 64:128], v3(F["km"].ap[:]), v3(F["eneg"].ap[:]), ALU.mult)
            halves("dve", "tensor_copy", [BKr], [BD["K_bd"]], lambda hs, hh: BD["K_bd"].ap[hs, :, 64 * hh:64 * hh + 64], [lambda hs, hh: BKr.ap[hs, :, 64:128]])
            halves("dve", "tensor_tensor", [BKr, F["eg"]], [BD["Kb_bd"]], lambda hs, hh: BD["Kb_bd"].ap[hs, :, 64 * hh:64 * hh + 64],
                   [lambda hs, hh: BKr.ap[hs, :, 64:128], lambda hs, hh: gLb[hs]], ALU.mult)
            P.I("dve", "scalar_tensor_tensor", [R_r, vecs, F["km"]], [rkb], rkb.ap[:], R_r.ap[:], vj(C_RK), F["km"].ap[:], ALU.mult, ALU.mult)
            P.mm(R_st2, R_st2.ap[:], conb, BO_bf, rkb, rkb.ap[:])
            P.I("act", "copy", [R_v], [F["v"]], F["v"].ap[:], R_v.ap[:])
            P.I("dve", "tensor_tensor", [R_st2, F["v"]], [F["bonus"]], F["bonus"].ap[:], R_st2.ap[:], F["v"].ap[:], ALU.mult)
            halves("dve", "tensor_copy", [F["v"]], [BD["V_bd"]], lambda hs, hh: BD["V_bd"].ap[hs, :, 64 * hh:64 * hh + 64], [lambda hs, hh: v3(F["v"].ap[:])[hs]])
            P.I("act", "copy", [R_g], [F["g"]], F["g"].ap[:], R_g.ap[:])
            P.I("dve", "tensor_tensor", [con, F["eg"]], [diagG], diagG.ap[:], IQ.unsqueeze(1).broadcast_to([128, C, 64]), gLb, ALU.mult)

            ckpt("B", [("AR", AR, AR.ap[:], [128, C, 128], BF16), ("BKr", BKr, BKr.ap[:], [128, C, 128], BF16), ("cum", F["cum"], F["cum"].ap[:], [128, NT], F32),
                       ("bonus", F["bonus"], F["bonus"].ap[:], [128, NT], F32), ("Vbd", BD["V_bd"], BD["V_bd"].ap[:], [128, C, 128], BF16),
                       ("Bbbd", BD["Bb_bd"], BD["Bb_bd"].ap[:], [128, C, 128], BF16), ("diagG", diagG, diagG.ap[:], [128, C, 64], F32)])
            tpf = [TV(PB[4], pbank[4][:, :].rearrange("p (c n) -> p c n", c=C), "tpA"), TV(PB[5], pbank[5][:, :].rearrange("p (c n) -> p c n", c=C), "tpB")]

            def do_tp(i_, nm):
                tp = tpf[i_ % 2]
                for c in range(C):
                    P.mm(tp, tp.ap[:, c, :], BD[nm], BD[nm].ap[:, c, :], conb, ID_bf)
                return tp
            tp = do_tp(0, "A_bd")
            ckpt("T0", [("gm", gm, gm.ap[:], [128, 8], F32)])
            halves("act", "copy", [tp], [Xt], lambda hs, hh: Xt.ap[hs, :, 0:64], [lambda hs, hh: tp.ap[hs, :, 64 * hh:64 * hh + 64]])
            ckpt("T1", [("Xt", Xt, Xt.ap[:], [128, C, 128], BF16)])
            tp = do_tp(1, "Bb_bd")
            halves("act", "copy", [tp], [RH["TBr"]], lambda hs, hh: RH["TBr"].ap[hs], [lambda hs, hh: tp.ap[hs, :, 64 * hh:64 * hh + 64]])
            tp = do_tp(2, "Kb_bd")
            halves("act", "copy", [tp], [RH["TKr"]], lambda hs, hh: RH["TKr"].ap[hs], [lambda hs, hh: tp.ap[hs, :, 64 * hh:64 * hh + 64]])
            tp = do_tp(3, "V_bd")
            halves("act", "copy", [tp], [RH["TVr"]], lambda hs, hh: RH["TVr"].ap[hs], [lambda hs, hh: tp.ap[hs, :, 64 * hh:64 * hh + 64]])
            P.I("act", "copy", [tp], [BD["TV_bd"]], BD["TV_bd"].ap[:], tp.ap[:])
            ckpt("T", [("Xt", Xt, Xt.ap[:], [128, C, 128], BF16), ("TVbd", BD["TV_bd"], BD["TV_bd"].ap[:], [128, C, 128], BF16)])
            b6 = B6.ap[:].rearrange("p (c n) -> p c n", c=C)
            b7 = B7.ap[:].rearrange("p (c n) -> p c n", c=C)
            zw3 = v3(R_zw.ap[:])
            for c in range(C):
                P.mm(B6, b6[:, c, :], BD["B_bd"], BD["B_bd"].ap[:, c, :], AR, AR.ap[:, c, :])
            for c in range(C):
                P.mm(R_zw, zw3[:, c, :], BD["K_bd"], BD["K_bd"].ap[:, c, :], AR, AR.ap[:, c, 64:128])
            for c in range(C):
                P.mm(B7, b7[:, c, :], BD["A_bd"], BD["A_bd"].ap[:, c, :], BKr, BKr.ap[:, c, :])
            MSb = MS.unsqueeze(1).broadcast_to([128, C, 64])
            MIb = MI.unsqueeze(1).broadcast_to([128, C, 64])
            MTb = MT.unsqueeze(1).broadcast_to([128, C, 64])
            IQb = IQ.unsqueeze(1).broadcast_to([128, C, 64])
            P.I("dve", "tensor_tensor", [B6, con], [RH["Nr"]], RH["Nr"].ap[:], b6[:, :, 0:64], MSb, ALU.mult)
            P.I("dve", "tensor_tensor", [B6, con], [RH["Arb"]], RH["Arb"].ap[:], b6[:, :, 64:128], MIb, ALU.mult)
            P.I("dve", "tensor_tensor", [R_zw, con], [RH["Ark"]], RH["Ark"].ap[:], zw3, MIb, ALU.mult)
            P.I("dve", "tensor_tensor", [B7, con], [RH["NTr"]], RH["NTr"].ap[:], b7[:, :, 0:64], MTb, ALU.mult)
            P.I("dve", "tensor_tensor", [B7, con], [Xt], Xt.ap[:, :, 64:128], b7[:, :, 64:128], MTb, ALU.mult)
            P.I("dve", "tensor_tensor", [RH["Nr"], con], [RH["Tr"]], RH["Tr"].ap[:], RH["Nr"].ap[:], IQb, ALU.add)
            halves("dve", "tensor_copy", [RH["Nr"]], [BD["N_bd"]], lambda hs, hh: BD["N_bd"].ap[hs, :, 64 * hh:64 * hh + 64], [lambda hs, hh: RH["Nr"].ap[hs]])
            halves("dve", "tensor_copy", [RH["NTr"]], [BD["NT_bd"]], lambda hs, hh: BD["NT_bd"].ap[hs, :, 64 * hh:64 * hh + 64], [lambda hs, hh: RH["NTr"].ap[hs]])
            ckpt("C1", [("Nr", RH["Nr"], RH["Nr"].ap[:], [128, C, 64], BF16), ("NTr", RH["NTr"], RH["NTr"].ap[:], [128, C, 64], BF16),
                        ("Xt", Xt, Xt.ap[:], [128, C, 128], BF16), ("TVbd", BD["TV_bd"], BD["TV_bd"].ap[:], [128, C, 128], BF16),
                        ("Arb", RH["Arb"], RH["Arb"].ap[:], [128, C, 64], BF16), ("Nbd", BD["N_bd"], BD["N_bd"].ap[:], [128, C, 128], BF16)])
            qa = v3(pbank[6][:, 0:256])
            qb = v3(pbank[6][:, 256:512])
            qc = v3(pbank[7][:, 0:256])
            qd = v3(pbank[7][:, 256:512])
            NLEV = 5

            def squarings(do_a):
                if do_a:
                    for c in range(C):
                        P.mm(B6, qa[:, c, :], BD["NT_bd"], BD["NT_bd"].ap[:, c, :], RH["Nr"], RH["Nr"].ap[:, c, :])
                for c in range(C):
                    P.mm(B6, qb[:, c, :], BD["N_bd"], BD["N_bd"].ap[:, c, :], RH["NTr"], RH["NTr"].ap[:, c, :])

            def evac_forms(do_a):
                if do_a:
                    P.I("act", "copy", [B6], [RH["Nr"]], RH["Nr"].ap[:], qa)
                    P.I("act", "copy", [B6], [RH["NTr"]], RH["NTr"].ap[:], qb)
                    halves("dve", "tensor_copy", [RH["Nr"]], [BD["N_bd"]], lambda hs, hh: BD["N_bd"].ap[hs, :, 64 * hh:64 * hh + 64], [lambda hs, hh: RH["Nr"].ap[hs]])
                    halves("dve", "tensor_copy", [RH["NTr"]], [BD["NT_bd"]], lambda hs, hh: BD["NT_bd"].ap[hs, :, 64 * hh:64 * hh + 64], [lambda hs, hh: RH["NTr"].ap[hs]])
                else:
                    halves("act", "copy", [B6], [BD["NT_bd"]], lambda hs, hh: BD["NT_bd"].ap[hs, :, 64 * hh:64 * hh + 64], [lambda hs, hh: qb[hs]])

            squarings(True)
            evac_forms(True)
            for lev in range(1, NLEV + 1):
                for c in range(C):
                    P.mm(B7, qc[:, c, :], BD["NT_bd"], BD["NT_bd"].ap[:, c, :], RH["Tr"], RH["Tr"].ap[:, c, :])
                if lev < NLEV:
                    squarings(lev + 1 < NLEV)
                P.I("dve", "tensor_tensor", [B7, RH["Tr"]], [RH["Tr"]], RH["Tr"].ap[:], qc, RH["Tr"].ap[:], ALU.add)
                if lev < NLEV:
                    evac_forms(lev + 1 < NLEV)
            halves("dve", "tensor_copy", [RH["Tr"]], [BD["T_bd"]], lambda hs, hh: BD["T_bd"].ap[hs, :, 64 * hh:64 * hh + 64], [lambda hs, hh: RH["Tr"].ap[hs]])
            ckpt("inv", [("Tr", RH["Tr"], RH["Tr"].ap[:], [128, C, 64], BF16)])
            for c in range(C):
                P.mm(B6, b6[:, c, :], BD["T_bd"], BD["T_bd"].ap[:, c, :], Xt, Xt.ap[:, c, :])
            halves("act", "copy", [B6], [BD["AhT_bd"]], lambda hs, hh: BD["AhT_bd"].ap[hs, :, 64 * hh:64 * hh + 64], [lambda hs, hh: b6[hs, :, 0:64]])
            halves("act", "copy", [B6], [BD["M1T_bd"]], lambda hs, hh: BD["M1T_bd"].ap[hs, :, 64 * hh:64 * hh + 64], [lambda hs, hh: b6[hs, :, 64:128]])
            for c in range(C):
                P.mm(B7, qc[:, c, :], BD["AhT_bd"], BD["AhT_bd"].ap[:, c, :], RH["Arb"], RH["Arb"].ap[:, c, :])
            for c in range(C):
                P.mm(B7, qd[:, c, :], BD["M1T_bd"], BD["M1T_bd"].ap[:, c, :], RH["Arb"], RH["Arb"].ap[:, c, :])
            P.I("dve", "tensor_tensor", [B7, AR], [RH["Rhat"]], RH["Rhat"].ap[:], qc, AR.ap[:, :, 64:128], ALU.add)
            P.I("dve", "tensor_tensor", [B7, RH["Ark"]], [RH["M2"]], RH["M2"].ap[:], qd, RH["Ark"].ap[:], ALU.add)
            for c in range(C):
                P.mm(B6, qa[:, c, :], BD["AhT_bd"], BD["AhT_bd"].ap[:, c, :], RH["TBr"], RH["TBr"].ap[:, c, :])
            for c in range(C):
                P.mm(B6, qb[:, c, :], BD["M1T_bd"], BD["M1T_bd"].ap[:, c, :], RH["TBr"], RH["TBr"].ap[:, c, :])
            P.I("dve", "tensor_tensor", [B6, diagG], [tmpP], tmpP.ap[:], qa, diagG.ap[:], ALU.add)
            P.I("dve", "tensor_tensor", [B6, RH["TKr"]], [tmpW], tmpW.ap[:], qb, RH["TKr"].ap[:], ALU.add)
            halves("dve", "tensor_copy", [tmpP], [BD["P_bd"]], lambda hs, hh: BD["P_bd"].ap[hs, :, 64 * hh:64 * hh + 64], [lambda hs, hh: tmpP.ap[hs]])
            halves("dve", "tensor_copy", [tmpW], [BD["W2T_bd"]], lambda hs, hh: BD["W2T_bd"].ap[hs, :, 64 * hh:64 * hh + 64], [lambda hs, hh: tmpW.ap[hs]])

            ckpt("C2", [("Rhat", RH["Rhat"], RH["Rhat"].ap[:], [128, C, 64], BF16), ("M2", RH["M2"], RH["M2"].ap[:], [128, C, 64], BF16),
                        ("Pbd", BD["P_bd"], BD["P_bd"].ap[:], [128, C, 128], BF16), ("W2Tbd", BD["W2T_bd"], BD["W2T_bd"].ap[:], [128, C, 128], BF16)])
            for c in range(C):
                P.mm(R_r, R_r.ap[:, c * 64:(c + 1) * 64], St_bd[j], St_bd[j].ap[:], RH["Rhat"], RH["Rhat"].ap[:, c, :], start=True, stop=False)
                P.mm(R_r, R_r.ap[:, c * 64:(c + 1) * 64], BD["TV_bd"], BD["TV_bd"].ap[:, c, :], RH["M2"], RH["M2"].ap[:, c, :], start=False, stop=True)
                P.mm(R_st, R_st.ap[:, 0:64], BD["P_bd"], BD["P_bd"].ap[:, c, :], St_r[j], St_r[j].ap[:], start=True, stop=False)
                P.mm(R_st, R_st.ap[:, 0:64], BD["W2T_bd"], BD["W2T_bd"].ap[:, c, :], RH["TVr"], RH["TVr"].ap[:, c, :], start=False, stop=True)
                P.I("act", "copy", [R_st], [St_r[j]], St_r[j].ap[:], R_st.ap[:, 0:64])
                for hh in range(2):
                    P.I("act", "copy", [R_st], [St_bd[j]], St_bd[j].ap[HS[hh], 64 * hh:64 * hh + 64], R_st.ap[HS[hh], 0:64])

            ckpt("D", [("Stbd", St_bd[j], St_bd[j].ap[:], [128, 128], BF16)])
            P.I("act", "copy", [R_r], [F["Y"]], F["Y"].ap[:], R_r.ap[:])
            P.mm(R_st2, R_st2.ap[:], con, BO64, F["Y"], F["Y"].ap[:])
            P.I("dve", "tensor_tensor", [F["Y"], R_st2], [F["yc"]], F["yc"].ap[:], F["Y"].ap[:], R_st2.ap[:], ALU.subtract)
            P.I("act", "activation", [F["yc"]], [F["sq2"]], F["sq2"].ap[:], F["yc"].ap[:], AF.Square)
            P.mm(R_k, R_k.ap[:], con, BO64, F["sq2"], F["sq2"].ap[:])
            P.I("act", "activation", [R_k, eps_t], [F["rs"]], F["rs"].ap[:], R_k.ap[:], AF.Ln, bias=eps_t.ap[:, 2:3])
            P.I("act", "activation", [F["rs"]], [F["rs"]], F["rs"].ap[:], F["rs"].ap[:], AF.Exp, scale=-0.5)
            P.I("dve", "tensor_tensor", [F["yc"], F["rs"]], [F["yc"]], F["yc"].ap[:], F["yc"].ap[:], F["rs"].ap[:], ALU.mult)
            P.I("act", "activation", [F["yc"], vecs], [F["yc"]], F["yc"].ap[:], F["yc"].ap[:], AF.Identity, bias=vj(C_GNB), scale=vj(C_GNW))
            P.I("dve", "tensor_tensor", [F["yc"], F["bonus"]], [F["yc"]], F["yc"].ap[:], F["yc"].ap[:], F["bonus"].ap[:], ALU.add)
            P.I("dve", "tensor_tensor", [F["yc"], F["g"]], [yo], yo.ap[:, j, :], F["yc"].ap[:], F["g"].ap[:], ALU.mult)

        ckpt("E", [("yo", yo, yo.ap[:], [128, 8, NT], BF16)])
        o_ring = Ring([R_v, R_za, R_g])
        for jo in range(8):
            ps = o_ring.next()
            for kc in range(8):
                P.mm(ps, ps.ap[:], wo, wo.ap[:, kc, jo * 128:(jo + 1) * 128], yo, yo.ap[:, kc, :], start=(kc == 0), stop=(kc == 7))
            P.I("dve", "scalar_tensor_tensor", [ps, g1p, x_t], [x_t], x_t.ap[:, jo, :], ps.ap[:], g1p.ap[:, jo:jo + 1], x_t.ap[:, jo, :], ALU.mult, ALU.add)
        P.dma("sp", None, yT_v[:, :, c0:c0 + NT], x_t, x_t.ap[:])


NCORES = 8
RUN_KW = {}
LAST_RES = None
ARENA_BYTES = 211968
_FUSED = []


BLOCKS = ("rw", "f0", "lr", "f1")


def build_fused():
    nc, P = new_prog()
    pbank = [P.ps([128, 512], F32, f"pb{i}") for i in range(8)]
    P.arena_init(ARENA_BYTES)
    VT = 2 * NTOK
    xT_d = nc.dram_tensor("xT", [D, VT], F32, kind="ExternalInput").ap()
    cT_d = nc.dram_tensor("cT", [128, 8], F32, kind="ExternalInput").ap()
    x1_d = nc.dram_tensor("x1_scr", [D, VT], F32, kind="Internal").ap()
    x2_d = nc.dram_tensor("x2_scr", [D, VT], F32, kind="Internal").ap()
    x3_d = nc.dram_tensor("x3_scr", [D, NTOK], F32, kind="Internal").ap()
    yT_d = nc.dram_tensor("yT", [D, NTOK], F32, kind="ExternalOutput").ap()
    fm = lambda ap: ap.rearrange("(j p) t -> p j t", p=128)
    if "rw" in BLOCKS:
        emit_rwkv(P, nc, pbank, fm(xT_d), fm(x1_d), "rw_", cT_d)
        P.barrier()
        P.arena_reset()
    if "f0" in BLOCKS:
        emit_ffn(P, nc, pbank, fm(x1_d if "rw" in BLOCKS else xT_d), fm(x2_d), VT, False, "f0_", cT_d)
        P.barrier()
        P.arena_reset()
    if "lr" in BLOCKS:
        emit_lru(P, nc, pbank, fm(x2_d if "f0" in BLOCKS else xT_d), fm(x3_d), "lr_", cT_d)
        P.barrier()
        P.arena_reset()
    if "f1" in BLOCKS:
        emit_ffn(P, nc, pbank, fm(x3_d if "lr" in BLOCKS else xT_d[:, 0:NTOK]), fm(yT_d), NTOK, True, "f1_", cT_d)
    P.emit()
    P.close()
    return nc, P


IDENT = np.eye(128, dtype=np.float32)


def kernel(x, c, ada_w, ada_b, norm_g, final_g,
           rwkv_mu, rwkv_w_rkv, rwkv_w_o, rwkv_w0, rwkv_w1, rwkv_w2, rwkv_a0, rwkv_a1, rwkv_a2,
           rwkv_g1, rwkv_g2, rwkv_k_k, rwkv_k_a, rwkv_r_k, rwkv_gn_w, rwkv_gn_b,
           lru_w_in, lru_conv_w, lru_conv_b, lru_w_gates, lru_b_gates, lru_lam, lru_w_out,
           ffn_w_gu, ffn_w_d, moe_w_router, moe_b_router, moe_w_gu, moe_w_d):
    f = lambda a: np.ascontiguousarray(np.asarray(a, dtype=np.float32))
    x, c, ada_w, ada_b, norm_g, final_g = f(x), f(c), f(ada_w), f(ada_b), f(norm_g), f(final_g)
    if not _FUSED:
        _FUSED.append(build_fused())
    nc, _ = _FUSED[0]
    B = x.shape[0]
    consts = rwkv_consts()
    bg = f(lru_b_gates)[0]
    cw = f(lru_conv_w)[0]
    shared = {
        "rw_consts": consts, "rw_adaw": f(ada_w[0][:, 0:3 * D]), "rw_wrkv": f(rwkv_w_rkv)[0], "rw_wo": f(rwkv_w_o)[0],
        "rw_w1": f(rwkv_w1)[0], "rw_w2": f(rwkv_w2)[0], "rw_a1": f(rwkv_a1)[0], "rw_a2": f(rwkv_a2)[0],
        "rw_g1": f(rwkv_g1)[0], "rw_g2": f(rwkv_g2)[0],
        "f0_vecs": pack_vecs([("ng", norm_g[0, 1]), ("adab", ada_b[0][3 * D:6 * D])])[0], "f0_adaw": f(ada_w[0][:, 3 * D:6 * D]),
        "f0_wgu": f(ffn_w_gu), "f0_wd": f(ffn_w_d),
        "lr_adaw": f(ada_w[1][:, 0:3 * D]), "lr_win": f(lru_w_in)[0], "lr_wg": f(lru_w_gates)[0], "lr_wout": f(lru_w_out)[0],
        "f1_vecs": pack_vecs([("ng", norm_g[1, 1]), ("adab", ada_b[1][3 * D:6 * D]), ("fg", final_g)])[0],
        "f1_adaw": f(ada_w[1][:, 3 * D:6 * D]), "f1_wgu": f(moe_w_gu)[0], "f1_wd": f(moe_w_d)[0], "f1_ident": IDENT,
        "f1_wr": f(moe_w_router)[0], "f1_br": f(np.broadcast_to(f(moe_b_router)[0].reshape(1, NE), (128, NE))),
    }
    in_maps = []
    for core in range(NCORES):
        b, half = core // 2, core % 2
        flag = np.full((128,), float(half), np.float32)
        xv = np.empty((D, 2 * NTOK), np.float32)
        if half == 1:
            xv[:, :] = x[b].T
        else:
            xv[:, :NTOK] = x[b, 0:NTOK].T
            xv[:, NTOK:] = x[b, 0:NTOK].T
        rw_vecs = pack_vecs([("ng", norm_g[0, 0]), ("adab", ada_b[0][0:3 * D]), ("mu", f(rwkv_mu)[0].reshape(-1)), ("w0", f(rwkv_w0)[0]),
                             ("a0", f(rwkv_a0)[0]), ("kk", f(rwkv_k_k)[0]), ("ka", f(rwkv_k_a)[0]), ("rk", f(rwkv_r_k)[0].reshape(-1)),
                             ("gnw", f(rwkv_gn_w)[0]), ("gnb", f(rwkv_gn_b)[0]), ("flag", flag)])[0]
        lr_vecs = pack_vecs([("ng", norm_g[1, 0]), ("adab", ada_b[1][0:3 * D]), ("cw0", cw[0]), ("cw1", cw[1]), ("cw2", cw[2]), ("cw3", cw[3]),
                             ("cb", f(lru_conv_b)[0]), ("bgr", bg[:, 0:256].reshape(-1)), ("bgi", bg[:, 256:512].reshape(-1)),
                             ("lam", f(lru_lam)[0]), ("flag", flag)])[0]
        m = dict(shared)
        m.update({"xT": xv, "cT": pack_vec(c[b]), "rw_vecs": rw_vecs, "lr_vecs": lr_vecs})
        in_maps.append(m)
    if len(BLOCKS) < 4:
        pre = tuple(b_ + "_" for b_ in BLOCKS)
        in_maps = [{k: v for k, v in m.items() if k in ("xT", "cT") or k.startswith(pre)} for m in in_maps]
    res = run_bass_kernel_spmd(nc, in_maps, core_ids=list(range(NCORES)), **RUN_KW)
    global LAST_RES
    LAST_RES = res
    out = np.empty((B, 2 * NTOK, D), np.float32)
    for core in range(NCORES):
        b, half = core // 2, core % 2
        out[b, half * NTOK:(half + 1) * NTOK, :] = res.results[core]["yT"].T
    return out
```
